# Optimizing a Trainium2 kernel written in Bass

```python
import jax, jax.numpy as jnp
from jax import lax
import numpy as np

D_MODEL = 2048
BATCH = 8
SEQ = 4096
DEPTH = 1

HEAD_DIM = 64
N_Q_HEADS = 16
N_KV_HEADS = 4
ATTN_WIDTH = N_Q_HEADS * HEAD_DIM
KV_WIDTH = N_KV_HEADS * HEAD_DIM
CONV_WIDTH = D_MODEL - ATTN_WIDTH
CONV_GROUPS = 16
CONV_GROUP_DIM = CONV_WIDTH // CONV_GROUPS
CONV_K = 3
IN_COLS = ATTN_WIDTH + 2 * KV_WIDTH + 3 * CONV_WIDTH

WINDOW = 128
ROPE_THETA = 500000.0
ROPE_DIM = HEAD_DIM // 4
ATTN_SCALE = HEAD_DIM ** -0.5
NEG_INF = -1e30

PEER_HEADS = 8
PEER_NKEYS = 128
PEER_N = PEER_NKEYS * PEER_NKEYS
PEER_DKEY = 256
PEER_HALF = PEER_DKEY // 2
PEER_TOPK = 16
PEER_CHUNK = 128

EPS = 1e-6

kernel_name = "hymba_swa_shortconv_peer_adaln"


def rmsnorm(x, gain):
    xf = x.astype(jnp.float32)
    y = xf * lax.rsqrt(jnp.mean(xf * xf, axis=-1, keepdims=True) + EPS)
    return (y * gain.astype(jnp.float32)).astype(x.dtype)


def head_rmsnorm(y, gain, n_heads):
    b, s, w = y.shape
    yf = y.reshape(b, s, n_heads, w // n_heads).astype(jnp.float32)
    yf = yf * lax.rsqrt(jnp.mean(yf * yf, axis=-1, keepdims=True) + EPS)
    return (yf.reshape(b, s, w) * gain.astype(jnp.float32)).astype(y.dtype)


def partial_rope(t, cos, sin):
    half = ROPE_DIM // 2
    t1 = t[..., :half]
    t2 = t[..., half:ROPE_DIM]
    rot = jnp.concatenate([t1 * cos - t2 * sin, t2 * cos + t1 * sin], axis=-1)
    return jnp.concatenate([rot.astype(t.dtype), t[..., ROPE_DIM:]], axis=-1)


def sliding_window_attention(q, k, v, sinks):
    b, s = q.shape[:2]
    nb = s // WINDOW
    g = N_Q_HEADS // N_KV_HEADS
    qb = q.reshape(b, nb, WINDOW, N_KV_HEADS, g, HEAD_DIM).transpose(1, 0, 2, 3, 4, 5)

    def band(t):
        tb = t.reshape(b, nb, WINDOW, N_KV_HEADS, HEAD_DIM)
        prev = jnp.concatenate([jnp.zeros_like(tb[:, :1]), tb[:, :-1]], axis=1)
        return jnp.concatenate([prev, tb], axis=2).transpose(1, 0, 2, 3, 4)

    kb, vb = band(k), band(v)
    sink = sinks.astype(jnp.float32).reshape(1, N_KV_HEADS, g, 1, 1)
    qi = jnp.arange(WINDOW)[:, None]
    kj = jnp.arange(2 * WINDOW)[None, :]
    diff = WINDOW + qi - kj
    band_mask = (diff >= 0) & (diff < WINDOW)

    def one_block(args):
        qn, kn, vn, n = args
        sc = jnp.einsum('bqhgd,bkhd->bhgqk', qn, kn,
                        preferred_element_type=jnp.float32) * ATTN_SCALE
        key_pos = (n - 1) * WINDOW + kj
        mask = band_mask & (key_pos >= 0)
        sc = jnp.where(mask, sc, NEG_INF)
        m = jnp.maximum(jnp.max(sc, axis=-1, keepdims=True), sink)
        p = jnp.exp(sc - m)
        denom = jnp.sum(p, axis=-1, keepdims=True) + jnp.exp(sink - m)
        probs = (p / denom).astype(vn.dtype)
        return jnp.einsum('bhgqk,bkhd->bqhgd', probs, vn)

    out = lax.map(one_block, (qb, kb, vb, jnp.arange(nb)))
    return out.transpose(1, 0, 2, 3, 4, 5).reshape(b, s, ATTN_WIDTH)


def short_conv(bg, cg, hc, conv_w):
    s = hc.shape[1]
    u = cg * hc
    upad = jnp.pad(u, ((0, 0), (CONV_K - 1, 0), (0, 0)))
    acc = conv_w[0] * upad[:, 0:s]
    for tap in range(1, CONV_K):
        acc = acc + conv_w[tap] * upad[:, tap:tap + s]
    return bg * acc


def peer(h, w_pq, peer_subkeys, peer_u, peer_v):
    b, s, d = h.shape
    q = jnp.einsum('bsd,de->bse', h, w_pq).reshape(b, s, PEER_HEADS, PEER_DKEY)
    q1, q2 = q[..., :PEER_HALF], q[..., PEER_HALF:]
    s1 = jnp.einsum('bshd,hkd->bshk', q1, peer_subkeys[:, 0], preferred_element_type=jnp.float32)
    s2 = jnp.einsum('bshd,hkd->bshk', q2, peer_subkeys[:, 1], preferred_element_type=jnp.float32)
    t1, i1 = lax.top_k(s1, PEER_TOPK)
    t2, i2 = lax.top_k(s2, PEER_TOPK)
    cand = (t1[..., :, None] + t2[..., None, :]).reshape(b, s, PEER_HEADS, PEER_TOPK * PEER_TOPK)
    ts, ci = lax.top_k(cand, PEER_TOPK)
    e1 = jnp.take_along_axis(i1, ci // PEER_TOPK, axis=-1)
    e2 = jnp.take_along_axis(i2, ci % PEER_TOPK, axis=-1)
    expert = e1 * PEER_NKEYS + e2
    gates = jax.nn.softmax(ts, axis=-1).astype(h.dtype)

    n_tok = b * s
    n_chunk = n_tok // PEER_CHUNK
    xt = h.reshape(n_chunk, PEER_CHUNK, d)
    idx = expert.reshape(n_chunk, PEER_CHUNK, PEER_HEADS * PEER_TOPK)
    gw = gates.reshape(n_chunk, PEER_CHUNK, PEER_HEADS * PEER_TOPK)

    def chunk_fn(args):
        xc, ic, gc = args
        u = peer_u[ic]
        a = jax.nn.gelu(jnp.einsum('cd,ced->ce', xc, u), approximate=False)
        vv = peer_v[ic]
        return jnp.einsum('ce,ced->cd', gc * a, vv)

    y = lax.map(chunk_fn, (xt, idx, gw))
    return y.reshape(b, s, d)


def setup_inputs(seed: int = 0) -> dict:
    key = jax.random.key(seed)
    ks = jax.random.split(key, 20)
    f32 = jnp.float32
    nrm = lambda k, shape, scale: jax.random.normal(k, shape, f32) * scale
    return {
        "x": nrm(ks[0], (BATCH, SEQ, D_MODEL), 1.0),
        "c": nrm(ks[1], (BATCH, D_MODEL), 1.0),
        "w_ada": nrm(ks[2], (D_MODEL, 6 * D_MODEL), 0.5 * D_MODEL ** -0.5),
        "b_ada": nrm(ks[3], (6 * D_MODEL,), 0.01),
        "g_norm1": 1.0 + nrm(ks[4], (D_MODEL,), 0.02),
        "w_in": nrm(ks[5], (D_MODEL, IN_COLS), D_MODEL ** -0.5),
        "g_q": 1.0 + nrm(ks[6], (HEAD_DIM,), 0.02),
        "g_k": 1.0 + nrm(ks[7], (HEAD_DIM,), 0.02),
        "sinks": nrm(ks[8], (N_Q_HEADS,), 0.5),
        "conv_w": nrm(ks[9], (CONV_K, CONV_WIDTH), CONV_K ** -0.5),
        "g_out_attn": 1.0 + nrm(ks[10], (ATTN_WIDTH,), 0.02),
        "g_out_conv": 1.0 + nrm(ks[11], (CONV_WIDTH,), 0.02),
        "w_out": nrm(ks[12], (D_MODEL, D_MODEL), D_MODEL ** -0.5),
        "g_norm2": 1.0 + nrm(ks[13], (D_MODEL,), 0.02),
        "w_pq": nrm(ks[14], (D_MODEL, PEER_HEADS * PEER_DKEY), D_MODEL ** -0.5),
        "peer_subkeys": nrm(ks[15], (PEER_HEADS, 2, PEER_NKEYS, PEER_HALF), PEER_HALF ** -0.5),
        "peer_u": nrm(ks[16], (PEER_N, D_MODEL), D_MODEL ** -0.5),
        "peer_v": nrm(ks[17], (PEER_N, D_MODEL), 0.5),
    }


def reference(x, c, w_ada, b_ada, g_norm1, w_in, g_q, g_k, sinks, conv_w,
              g_out_attn, g_out_conv, w_out, g_norm2, w_pq, peer_subkeys,
              peer_u, peer_v):
    b, s, d = x.shape
    pos = jnp.arange(s, dtype=jnp.float32)
    inv_freq = ROPE_THETA ** (-jnp.arange(0, ROPE_DIM, 2, dtype=jnp.float32) / ROPE_DIM)
    ang = pos[:, None] * inv_freq[None, :]
    cos = jnp.cos(ang)[None, :, None, :].astype(x.dtype)
    sin = jnp.sin(ang)[None, :, None, :].astype(x.dtype)

    for _ in range(DEPTH):
        mod = jnp.einsum('bd,de->be', jax.nn.silu(c), w_ada) + b_ada
        sh1, sc1, gt1, sh2, sc2, gt2 = jnp.split(mod[:, None, :], 6, axis=-1)

        h1 = rmsnorm(x, g_norm1) * (1.0 + sc1) + sh1
        proj = jnp.einsum('bsd,de->bse', h1, w_in)
        o = 0
        q = proj[..., o:o + ATTN_WIDTH]; o += ATTN_WIDTH
        k = proj[..., o:o + KV_WIDTH]; o += KV_WIDTH
        v = proj[..., o:o + KV_WIDTH]; o += KV_WIDTH
        bg = proj[..., o:o + CONV_WIDTH]; o += CONV_WIDTH
        cg = proj[..., o:o + CONV_WIDTH]; o += CONV_WIDTH
        hc = proj[..., o:o + CONV_WIDTH]

        q = rmsnorm(q.reshape(b, s, N_Q_HEADS, HEAD_DIM), g_q)
        k = rmsnorm(k.reshape(b, s, N_KV_HEADS, HEAD_DIM), g_k)
        v = v.reshape(b, s, N_KV_HEADS, HEAD_DIM)
        q = partial_rope(q, cos, sin)
        k = partial_rope(k, cos, sin)
        attn = sliding_window_attention(q, k, v, sinks)
        conv = short_conv(bg, cg, hc, conv_w)

        mixed = jnp.concatenate([head_rmsnorm(attn, g_out_attn, N_Q_HEADS),
                                 head_rmsnorm(conv, g_out_conv, CONV_GROUPS)], axis=-1)
        x = x + gt1 * jnp.einsum('bse,ed->bsd', mixed, w_out)

        h2 = rmsnorm(x, g_norm2) * (1.0 + sc2) + sh2
        x = x + gt2 * peer(h2, w_pq, peer_subkeys, peer_u, peer_v)
    return x
```

```python
import numpy as np
from contextlib import ExitStack
import concourse.bass as bass
import concourse.mybir as mybir
from concourse.bass_utils import run_bass_kernel_spmd

F32 = mybir.dt.float32
BF16 = mybir.dt.bfloat16
U32 = mybir.dt.uint32
I32 = mybir.dt.int32
ALU = mybir.AluOpType
AF = mybir.ActivationFunctionType
AX = mybir.AxisListType

S = 4096
D = 2048
TT = 256
NST = S // TT
NI = TT // 128
EPS = 1e-6
NGRP = 18
INC = 5120

DEBUG_STAGE = None
N_SUPER = NST


class Buf:
    __slots__ = ("name", "w", "r", "dsem", "dcnt")

    def __init__(self, name):
        self.name = name
        self.w = None
        self.r = []
        self.dsem = None
        self.dcnt = 0


class Tracker:
    def __init__(self, nc, es):
        self.nc = nc
        self.es = es
        self.eng = {}
        for name, h in (("pe", nc.tensor), ("act", nc.scalar), ("dve", nc.vector),
                        ("pool", nc.gpsimd), ("sp", nc.sync)):
            sem = es.enter_context(nc.semaphore("sem_" + name))
            self.eng[name] = {"h": h, "sem": sem, "cnt": 0, "seen": {}, "name": name}
        self.bufs = {}
        self.dsems = {}
        self.ninst = 0

    def buf(self, name):
        b = Buf(name)
        self.bufs[name] = b
        return b

    def _wait(self, e, toks):
        best = {}
        for t in toks:
            if t is None:
                continue
            sem, val = t
            k = id(sem)
            if e["seen"].get(k, 0) >= val:
                continue
            if k not in best or best[k][1] < val:
                best[k] = (sem, val)
        for k, (sem, val) in best.items():
            if e["name"] == "pe" and sem is e["sem"]:
                continue
            e["h"].wait_ge(sem, val)
            e["seen"][k] = val
            self.ninst += 1

    @staticmethod
    def _deps(reads, writes):
        toks = []
        for b in reads:
            toks.append(b.w)
        for b in writes:
            toks.append(b.w)
            toks.extend(b.r)
        return toks

    def op(self, en, fn, reads=(), writes=(), inc=True):
        e = self.eng[en]
        self._wait(e, self._deps(reads, writes))
        ins = fn(e["h"])
        self.ninst += 1
        if inc:
            e["cnt"] += 1
            ins.then_inc(e["sem"], 1)
            tok = (e["sem"], e["cnt"])
        else:
            tok = (e["sem"], e["cnt"] + 1)
        for b in reads:
            b.r.append(tok)
        for b in writes:
            b.w = tok
            b.r = []
        return tok

    def dma(self, en, fn, reads=(), writes=(), sembuf=None):
        e = self.eng[en]
        sb = sembuf if sembuf is not None else (writes[0] if writes else reads[0])
        if sb.dsem is None:
            if sb.name not in self.dsems:
                self.dsems[sb.name] = [self.es.enter_context(self.nc.semaphore("ds_" + sb.name)), 0]
            sb.dsem = self.dsems[sb.name][0]
            sb.dcnt = self.dsems[sb.name][1]
        toks = [t for t in self._deps(reads, writes) if not (t is not None and t[0] is sb.dsem)]
        self._wait(e, toks)
        ins = fn(e["h"])
        self.ninst += 1
        sb.dcnt += 16
        self.dsems[sb.name][1] = sb.dcnt
        ins.then_inc(sb.dsem, 16)
        tok = (sb.dsem, sb.dcnt)
        for b in reads:
            b.r.append(tok)
        for b in writes:
            b.w = tok
            b.r = []
        return tok

    def wait_all(self, en, bufs):
        e = self.eng[en]
        toks = []
        for b in bufs:
            toks.append(b.w)
            toks.extend(b.r)
        self._wait(e, toks)

    def barrier(self):
        sp = self.eng["sp"]
        toks = []
        for n, e in self.eng.items():
            if n != "sp" and e["cnt"] > 0:
                toks.append((e["sem"], e["cnt"]))
        for nm, (dsem, dcnt) in self.dsems.items():
            if dcnt > 0:
                toks.append((dsem, dcnt))
        self._wait(sp, toks)
        sp["cnt"] += 1
        sp["h"].nop().then_inc(sp["sem"], 1)
        self.ninst += 1
        tok = (sp["sem"], sp["cnt"])
        for n, e in self.eng.items():
            if n != "sp":
                self._wait(e, [tok])
        for b in self.bufs.values():
            b.w = None
            b.r = []


def _ap(src, offset, dims):
    return bass.AP(src.tensor, src.offset + offset, [list(src.ap[0])] + [list(d) for d in dims])


def build_program():
    nc = bass.Bass("TRN2", target_bir_lowering=False)

    def din(name, shape, dt=F32):
        return nc.dram_tensor(name, list(shape), dt, kind="ExternalInput")

    x_d = din("x", [S, D])
    ccol_d = din("ccol", [128, 16])
    wada_d = din("w_ada", [D, 6 * D])
    bada_d = din("b_ada", [1, 6 * D])
    g1col_d = din("g1col", [128, 16])
    g2col_d = din("g2col", [128, 16])
    win_d = din("w_in2", [D, INC])
    gq_d = din("gqcol", [128, 1])
    gk_d = din("gkcol", [128, 1])
    sinks_d = din("sinks", [1, 16])
    convw_d = din("convw", [128, 24])
    gattn_d = din("gattn", [128, 8])
    gconv_d = din("gconv", [128, 8])
    wout_d = din("w_out", [D, D])
    wpq_d = din("w_pq", [D, D])
    subk_d = din("subkT", [128, 16 * 128])
    put_d = din("peer_uT", [D, 16384])
    pv_d = din("peer_v", [16384, D])
    identf_d = din("identf", [128, 128])
    bones_d = din("bones", [128, 128])
    ropep_d = din("ropepT", [128, 128])
    mprev_d = din("mprev", [128, 128])
    mcur_d = din("mcur", [128, 128])
    cos_d = din("cosT", [128, S])
    sin_d = din("sinT", [128, S])
    iota_d = din("iota128", [128, 128])
    out_d = nc.dram_tensor("out", [S, D], F32, kind="ExternalOutput")
    wsc_d = nc.dram_tensor("wsc", [NGRP, 128, 16 * 512], BF16)
    us_d = nc.dram_tensor("us", [32, 128, 8192], BF16)
    gt1_d = nc.dram_tensor("gt1s", [128, D], F32)
    vs_d = nc.dram_tensor("vs", [32, 128, 8192], BF16)

    with ExitStack() as es:
        def sb(name, shape, dt=F32):
            return es.enter_context(nc.sbuf_tensor(name, list(shape), dt))

        tr = Tracker(nc, es)
        B = tr.buf

        xres2 = sb("xres", [128, 2 * NI, D])
        b_xres2 = [[B("xres%d_%d" % (p_, i)) for i in range(NI)] for p_ in range(2)]
        xres = xres2[:, 0:NI, :]; b_xres = b_xres2[0]
        b_rows = B("rows")
        cols = sb("cols", [128, 4, 16]); b_cols = B("cols")
        identb = sb("identb", [128, 128], BF16); bonesb = sb("bonesb", [128, 128], BF16)
        ropepb = sb("ropepb", [128, 128], BF16); mprevb = sb("mprevb", [128, 128], BF16)
        mcurb = sb("mcurb", [128, 128], BF16); onesb = sb("onesb", [128, 128], BF16)
        onesf = sb("onesf", [1, 128]); iota16 = sb("iota16s", [128, 16]); iotab = sb("iotab", [128, 128], BF16); identf = sb("identf_s", [128, 128])
        b_const = B("const")
        gqk = sb("gqk", [128, 2]); convw = sb("convw_s", [128, 24]); gattn = sb("gattn_s", [128, 8])
        gconv = sb("gconv_s", [128, 8]); esink = sb("esink", [128, 16]); subkb = sb("subkb", [128, 16, 128], BF16)
        b_par = B("par")
        kT = sb("kT", [128, 4, 2, 128 + TT], BF16); b_kT = B("kT")
        vtok = sb("vtok", [128, NI + 1, 512], BF16); b_vtok = B("vtok")
        ubuf = sb("ubuf", [128, 8, 2 + TT]); b_u = B("ubuf")
        stat = sb("stat", [128, 8]); b_stat = B("stat")
        epsc = sb("epsc", [128, 1])

        pbank = [es.enter_context(nc.psum_tensor("pb%d" % i, [128, 512], F32)) for i in range(8)]
        b_pb = [B("pb%d" % i) for i in range(8)]
        rr = {"i": 0}

        def nextbank(lo=0, hi=8):
            i = lo + (rr["i"] % (hi - lo))
            rr["i"] += 1
            return pbank[i], b_pb[i]

        es.enter_context(nc.Block())
        b_out = B("out")
        b_wsc1 = B("wsc"); b_wsc = [b_wsc1] * NGRP
        b_uvs = B("uvs")

        with ExitStack() as ps:
            def sbp(name, shape, dt=F32):
                return ps.enter_context(nc.sbuf_tensor(name, list(shape), dt))
            NSTG = 3
            stage = [sbp("stage%d" % i, [128, 8, 512]) for i in range(NSTG)]
            b_stage = [B("stage%d" % i) for i in range(NSTG)]
            cvt = [sbp("cvt%d" % i, [128, 8 * 512], BF16) for i in range(2)]
            b_cvt = [B("cvt%d" % i) for i in range(2)]
            ccol = sbp("ccol_s", [128, 16]); b_ccol = B("ccol")
            ctmp = sbp("ctmp", [128, 128]); b_ctmp = B("ctmp")
            g12 = sbp("g12", [128, 2, 16]); b_g12 = B("g12")
            subkf = sbp("subkf", [128, 16 * 128]); b_subkf = B("subkf")
            gt2b = sbp("gt2b", [128, D]); b_gt2 = B("gt2b")
            gt1b = sbp("gt1b_p", [128, D]); b_gt1s = B("gt1s")
            pm_ = ExitStack()
            modrow = pm_.enter_context(nc.sbuf_tensor("modrow", [1, 6 * D], F32)); b_mod = B("modrow")

            sp_loads = [
                (ccol[:], ccol_d.ap(), b_ccol), (modrow[:], bada_d.ap(), b_mod),
                (g12[:, 0, :], g1col_d.ap(), b_g12), (g12[:, 1, :], g2col_d.ap(), b_g12),
                (gqk[:, 0:1], gq_d.ap(), b_par), (gqk[:, 1:2], gk_d.ap(), b_par),
                (convw[:], convw_d.ap(), b_par), (gattn[:], gattn_d.ap(), b_par),
                (gconv[:], gconv_d.ap(), b_par), (iota16[:], iota_d[:, 0:16], b_const), (identf[:], identf_d.ap(), b_const),
                (esink[:], bass.AP(sinks_d, 0, [[0, 128], [1, 16]]), b_par), (subkf[:], subk_d.ap(), b_subkf),
            ]
            for o, i_, bb in sp_loads:
                tr.dma("sp", lambda h, o=o, i_=i_: h.dma_start(out=o, in_=i_), writes=[bb])
            for k, (src_d, dst) in enumerate([(identf_d, identb), (bones_d, bonesb), (ropep_d, ropepb),
                                              (mprev_d, mprevb), (mcur_d, mcurb)]):
                tr.dma("sp", lambda h, s=src_d: h.dma_start(out=ctmp[:], in_=s.ap()), writes=[b_ctmp])
                tr.op("dve", lambda h, d=dst: h.tensor_copy(out=d[:], in_=ctmp[:]), reads=[b_ctmp], writes=[b_const])
            tr.op("dve", lambda h: h.memset(onesb[:], 1.0), writes=[b_const])
            tr.op("dve", lambda h: h.memset(epsc[:], EPS), writes=[b_const])
            tr.dma("sp", lambda h: h.dma_start(out=ctmp[:], in_=iota_d.ap()), writes=[b_ctmp])
            tr.op("dve", lambda h: h.tensor_copy(out=iotab[:], in_=ctmp[:]), reads=[b_ctmp], writes=[b_const])
            tr.op("dve", lambda h: h.memset(onesf[:], 1.0), writes=[b_const])
            tr.op("dve", lambda h: h.tensor_copy(out=subkb[:].rearrange("p a b -> p (a b)"), in_=subkf[:]),
                  reads=[b_subkf], writes=[b_par])
            tr.op("act", lambda h: h.activation(out=esink[:], in_=esink[:], func=AF.Exp), reads=[b_par], writes=[b_par])
            tr.op("dve", lambda h: h.tensor_scalar(out=esink[:], in0=esink[:], scalar1=1e-3, scalar2=None, op0=ALU.mult), reads=[b_par], writes=[b_par])
            tr.op("act", lambda h: h.activation(out=ccol[:], in_=ccol[:], func=AF.Silu), reads=[b_ccol], writes=[b_ccol])

            if DEBUG_STAGE == "p1":
                tr.barrier()
                return nc, tr.ninst
            ntile = 0
            for t in range(24):
                pb, bpb = nextbank()
                for hh in range(2):
                    sl = ntile % NSTG
                    ntile += 1
                    src = wada_d.ap().rearrange("(c p) n -> p c n", p=128)[:, hh * 8:(hh + 1) * 8, t * 512:(t + 1) * 512]
                    tr.dma("sp", lambda h, sl=sl, src=src: h.dma_start(out=stage[sl][:], in_=src), writes=[b_stage[sl]])
                    for c8 in range(8):
                        c = hh * 8 + c8
                        tr.op("pe", lambda h, c=c, c8=c8, pb=pb, sl=sl: h.matmul(pb[0:1, :], lhsT=ccol[:, c:c + 1], rhs=stage[sl][:, c8, :],
                                                                                 start=(c == 0), stop=(c == 15)),
                              reads=[b_ccol, b_stage[sl]], writes=[bpb], inc=(c8 == 7))
                tr.op("dve", lambda h, pb=pb, t=t: h.tensor_tensor(out=modrow[0:1, t * 512:(t + 1) * 512], in0=pb[0:1, :],
                                                                   in1=modrow[0:1, t * 512:(t + 1) * 512], op=ALU.add),
                      reads=[bpb, b_mod], writes=[b_mod])
            if DEBUG_STAGE == "p2":
                tr.barrier()
                return nc, tr.ninst
            for dst, sec, bdst in ((gt1b, 2, b_rows), (gt2b, 5, b_gt2)):
                for dg in range(4):
                    pb, bpb = nextbank()
                    tr.op("pe", lambda h, pb=pb, sec=sec, dg=dg: h.matmul(pb[:, :], lhsT=onesf[0:1, :],
                                                                          rhs=modrow[0:1, sec * D + dg * 512: sec * D + (dg + 1) * 512],
                                                                          start=True, stop=True),
                          reads=[b_mod, b_const], writes=[bpb])
                    tr.op("act", lambda h, pb=pb, dst=dst, dg=dg: h.activation(out=dst[:, dg * 512:(dg + 1) * 512], in_=pb[:, :], func=AF.Copy),
                          reads=[bpb], writes=[bdst])
            tr.dma("sp", lambda h: h.dma_start(out=gt1_d.ap(), in_=gt1b[:]), reads=[b_rows], writes=[b_gt1s], sembuf=b_rows)
            pb, bpb = nextbank()
            for k, sec in enumerate((0, 1, 3, 4)):
                for c in range(16):
                    tr.op("pe", lambda h, pb=pb, k=k, c=c, sec=sec: h.matmul(
                        pb[:, (k * 16 + c) * 2:(k * 16 + c) * 2 + 2], lhsT=modrow[0:1, sec * D + c * 128: sec * D + (c + 1) * 128],
                        rhs=onesf[0:1, 0:2], start=True, stop=True), reads=[b_mod, b_const], writes=[bpb],
                        inc=(k == 3 and c == 15))
            pbv = pb[:, 0:128].rearrange("p (k c two) -> p k c two", k=4, c=16, two=2)
            tr.op("dve", lambda h: h.tensor_copy(out=cols[:], in_=pbv[:, :, :, 0]), reads=[bpb], writes=[b_cols])
            for k, gi in ((1, 0), (3, 1)):
                tr.op("dve", lambda h, k=k, gi=gi: h.scalar_tensor_tensor(out=cols[:, k, :], in0=cols[:, k, :], scalar=1.0,
                                                                         in1=g12[:, gi, :], op0=ALU.add, op1=ALU.mult),
                      reads=[b_cols, b_g12], writes=[b_cols])

            if DEBUG_STAGE == "p3":
                tr.barrier()
                return nc, tr.ninst
            tr.barrier()
            pm_.close()
            for k_ in range(3):
                stage.append(sbp("stage%d" % (NSTG + k_), [128, 8, 512])); b_stage.append(B("stage%d" % (NSTG + k_)))
            NSTG = 6
            gsrc = []
            for g in range(10):
                gsrc.append(win_d.ap().rearrange("(c p) n -> p c n", p=128)[:, :, g * 512:(g + 1) * 512])
            for g in range(4):
                gsrc.append(wout_d.ap().rearrange("(c p) n -> p c n", p=128)[:, :, g * 512:(g + 1) * 512])
            for g in range(4):
                gsrc.append(wpq_d.ap().rearrange("(c p) n -> p c n", p=128)[:, :, g * 512:(g + 1) * 512])
            utv = put_d.ap().rearrange("(c p) n -> p c n", p=128)
            vv = pv_d.ap().rearrange("(g c p) d -> g p c d", c=4, p=128)
            jobs = []
            for g in range(NGRP):
                for hh in range(2):
                    jobs.append((gsrc[g][:, hh * 8:(hh + 1) * 8, :], False, wsc_d[g][:, hh * 4096:(hh + 1) * 4096], b_wsc[g]))
            for g in range(32):
                for hh in range(2):
                    jobs.append((utv[:, hh * 8:(hh + 1) * 8, g * 512:(g + 1) * 512], False, us_d[g][:, hh * 4096:(hh + 1) * 4096], b_uvs))
            for g in range(32):
                for hh in range(2):
                    jobs.append((vv[g][:, hh * 2:(hh + 1) * 2, :], True, vs_d[g][:, hh * 4096:(hh + 1) * 4096], b_uvs))
            base = ntile

            def job_in(k):
                src, is_v, dst, bd = jobs[k]
                sl = (base + k) % NSTG
                o = stage[sl][:].rearrange("p c n -> p (c n)").rearrange("p (c d) -> p c d", c=2) if is_v else stage[sl][:]
                tr.dma("sp", lambda h: h.dma_start(out=o, in_=src), writes=[b_stage[sl]])

            for k in range(min(NSTG, len(jobs))):
                job_in(k)
            for k in range(len(jobs)):
                src, is_v, dst, bd = jobs[k]
                sl = (base + k) % NSTG
                cs = k % 2
                stf = stage[sl][:].rearrange("p c n -> p (c n)")
                if is_v:
                    tr.op("dve", lambda h: h.tensor_tensor(out=cvt[cs][:, 0:2048], in0=stf[:, 0:2048], in1=gt2b[:], op=ALU.mult),
                          reads=[b_stage[sl], b_gt2], writes=[b_cvt[cs]])
                    tr.op("pool", lambda h: h.tensor_tensor(out=cvt[cs][:, 2048:4096], in0=stf[:, 2048:4096], in1=gt2b[:], op=ALU.mult),
                          reads=[b_stage[sl], b_gt2], writes=[b_cvt[cs]])
                else:
                    tr.op("act", lambda h: h.activation(out=cvt[cs][:, 0:2048], in_=stf[:, 0:2048], func=AF.Copy),
                          reads=[b_stage[sl]], writes=[b_cvt[cs]])
                    tr.op("dve", lambda h: h.tensor_copy(out=cvt[cs][:, 2048:4096], in_=stf[:, 2048:4096]),
                          reads=[b_stage[sl]], writes=[b_cvt[cs]])
                if k + NSTG < len(jobs):
                    job_in(k + NSTG)
                tr.dma("sp", lambda h: h.dma_start(out=dst, in_=cvt[cs][:]), reads=[b_cvt[cs]], writes=[bd], sembuf=b_cvt[cs])
            tr.op("dve", lambda h: h.memset(kT[:], 0.0), writes=[b_kT])
            tr.op("dve", lambda h: h.memset(vtok[:], 0.0), writes=[b_vtok])
            tr.op("dve", lambda h: h.memset(ubuf[:], 0.0), writes=[b_u])
            tr.barrier()

        def load_w(g, wb, bwb):
            for hh in range(2):
                tr.dma("sp", lambda h, hh=hh: h.dma_start(out=wb[:, hh * 4096:(hh + 1) * 4096], in_=wsc_d[g][:, hh * 4096:(hh + 1) * 4096]),
                       reads=[b_wsc[g]], writes=[bwb])

        def rstd_of(ssq_ap, out_ap, scale, bufs_r, bufs_w):
            tr.op("act", lambda h: h.activation(out=out_ap, in_=ssq_ap, func=AF.Ln, scale=scale, bias=epsc[:]),
                  reads=list(bufs_r) + [b_const], writes=bufs_w)
            tr.op("act", lambda h: h.activation(out=out_ap, in_=out_ap, func=AF.Exp, scale=-0.5), reads=bufs_w, writes=bufs_w)

        def norm_transpose(xn, b_xn, hT, b_hT, kcol_shift, kcol_scale, stat_off):
            for i in range(NI):
                tr.op("act", lambda h, i=i: h.activation(out=xn[:, i, :], in_=xres[:, i, :], func=AF.Square,
                                                         accum_out=stat[:, stat_off + i:stat_off + i + 1]),
                      reads=[b_xres[i]], writes=[b_xn, b_stat])
            rstd_of(stat[:, stat_off:stat_off + NI], stat[:, stat_off + 2:stat_off + 2 + NI], 1.0 / D, [b_stat], [b_stat])
            for i in range(NI):
                tr.op("act", lambda h, i=i: h.activation(out=xn[:, i, :], in_=xres[:, i, :], func=AF.Copy,
                                                         scale=stat[:, stat_off + 2 + i:stat_off + 3 + i]),
                      reads=[b_xres[i], b_stat], writes=[b_xn])
            for c in range(16):
                pb, bpb = nextbank()
                pbb = pb[:].bitcast(BF16)
                for i in range(NI):
                    tr.op("pe", lambda h, i=i, c=c, pbb=pbb: h.transpose(out=pbb[:, i * 128:(i + 1) * 128],
                                                                         in_=xn[:, i, c * 128:(c + 1) * 128], identity=identb[:]),
                          reads=[b_xn, b_const], writes=[bpb], inc=(i == NI - 1))
                tr.op("dve", lambda h, c=c, pbb=pbb: h.tensor_scalar(out=hT[:, c, :], in0=pbb[:, 0:TT],
                                                                     scalar1=cols[:, kcol_scale, c:c + 1], scalar2=cols[:, kcol_shift, c:c + 1],
                                                                     op0=ALU.mult, op1=ALU.add),
                      reads=[bpb, b_cols], writes=[b_hT])

        for st in range(N_SUPER):
            t0 = st * TT
            with ExitStack() as pa:
                def sba(name, shape, dt=F32):
                    return pa.enter_context(nc.sbuf_tensor("%s_a%d" % (name, st), list(shape), dt))
                xn = sba("xn", [128, NI, D], BF16); b_xn = B("xn")
                hT = sba("hT", [128, 16, TT], BF16); b_hT = B("hT")
                wbuf = [sba("wbuf%d" % i, [128, 8192], BF16) for i in range(2)]
                b_wbuf = [B("wbuf%d" % i) for i in range(2)]
                cst = sba("cst", [128, 2, TT]); b_cst = B("cst")
                qT = sba("qT", [128, 8, TT], BF16); b_qT = B("qT")
                cgs = sba("cgs", [128, 8, TT]); b_cgs = B("cgs")
                mixT = sba("mixT", [128, 16, TT], BF16); b_mixT = B("mixT")
                Pt = [sba("Pt%d" % i, [128, 512], BF16) for i in range(4)]
                gt1b = sba("gt1b", [128, D])
                b_Pt = [B("Pt%d" % i) for i in range(4)]

                par_ = st % 2
                xres = xres2[:, par_ * NI:(par_ + 1) * NI, :]; b_xres = b_xres2[par_]
                if st == 0 or DEBUG_STAGE == "A":
                    for i in range(NI):
                        tr.dma("sp", lambda h, i=i: h.dma_start(out=xres[:, i, :], in_=x_d[t0 + i * 128: t0 + (i + 1) * 128, :]),
                               writes=[b_xres[i]])
                tr.dma("sp", lambda h: h.dma_start(out=cst[:, 0, :], in_=cos_d[:, t0:t0 + TT]), writes=[b_cst])
                tr.dma("sp", lambda h: h.dma_start(out=cst[:, 1, :], in_=sin_d[:, t0:t0 + TT]), writes=[b_cst])
                load_w(0, wbuf[0], b_wbuf[0])
                tr.dma("sp", lambda h: h.dma_start(out=gt1b[:], in_=gt1_d.ap()), reads=[b_gt1s], writes=[b_rows])
                load_w(1, wbuf[1], b_wbuf[1])
                norm_transpose(xn, b_xn, hT, b_hT, 0, 1, 0)

                if DEBUG_STAGE == "a1":
                    tr.barrier()
                    return nc, tr.ninst
                tmpc = {"i": 0}
                T2 = {}
                for nm, shp, dt_ in (("sq", [128, 512], BF16), ("r", [128, 512], F32), ("a", [128, 512], F32), ("b", [128, 512], F32), ("qn", [128, TT], BF16)):
                    T2[nm] = [(sba("t2%s%d" % (nm, k), shp, dt_), B("t2%s%d" % (nm, k))) for k in range(4)]

                def tmps():
                    k = tmpc["i"] % 4
                    tmpc["i"] += 1
                    return {nm: T2[nm][k] for nm in T2}

                pgc = {"i": 0}

                def proj_group(g):
                    wb, bwb = wbuf[g % 2], b_wbuf[g % 2]
                    bs = 2 * (pgc["i"] % 2)
                    pgc["i"] += 1
                    outs = []
                    for ch in range(4):
                        pbk, bpbk = pbank[bs + ch // 2], b_pb[bs + ch // 2]
                        pv = pbk[:, (ch % 2) * TT:(ch % 2 + 1) * TT]
                        for c in range(16):
                            tr.op("pe", lambda h, c=c, pv=pv, ch=ch: h.matmul(pv, lhsT=wb[:, c * 512 + ch * 128: c * 512 + (ch + 1) * 128],
                                                                             rhs=hT[:, c, :], start=(c == 0), stop=(c == 15)),
                                  reads=[bwb, b_hT], writes=[bpbk], inc=(c == 15))
                        outs.append((pv, bpbk))
                    if g + 2 < 14:
                        load_w(g + 2, wbuf[g % 2], b_wbuf[g % 2])
                    return outs

                def halfbank(k, ch):
                    pbk, bpbk = pbank[k + ch // 2], b_pb[k + ch // 2]
                    return pbk[:, (ch % 2) * TT:(ch % 2 + 1) * TT], bpbk

                def rstd_stages(srcs, Ts, n):
                    for (src_ap, b_src), T in zip(srcs, Ts):
                        (tsq, btsq) = T["sq"]
                        tr.op("act", lambda h, tsq=tsq, src_ap=src_ap: h.activation(out=tsq[:, 0:n], in_=src_ap, func=AF.Square),
                              reads=[b_src], writes=[btsq])
                    pbs = []
                    for ch, T in enumerate(Ts):
                        (tsq, btsq) = T["sq"]
                        pv2, bpb2 = halfbank(4, ch)
                        tr.op("pe", lambda h, tsq=tsq, pv2=pv2: h.matmul(pv2, lhsT=bonesb[:], rhs=tsq[:, 0:n], start=True, stop=True),
                              reads=[btsq, b_const], writes=[bpb2])
                        pbs.append((pv2, bpb2))
                    for (pv2, bpb2), T in zip(pbs, Ts):
                        (tr_, btr) = T["r"]
                        tr.op("act", lambda h, tr_=tr_, pv2=pv2: h.activation(out=tr_[:, 0:n], in_=pv2, func=AF.Ln, bias=epsc[:]),
                              reads=[bpb2, b_const], writes=[btr])
                    for T in Ts:
                        (tr_, btr) = T["r"]
                        tr.op("act", lambda h, tr_=tr_: h.activation(out=tr_[:, 0:n], in_=tr_[:, 0:n], func=AF.Exp, scale=-0.5), reads=[btr], writes=[btr])

                def post_qk(g, outs):
                    Ts = [tmps() for _ in range(4)]
                    rstd_stages(outs, Ts, TT)
                    gcol = gqk[:, 0:1] if g < 2 else gqk[:, 1:2]
                    for (pv, bpb), T in zip(outs, Ts):
                        (tr_, btr), (tqn, btqn) = T["r"], T["qn"]
                        tr.op("dve", lambda h, pv=pv, tr_=tr_, tqn=tqn: h.scalar_tensor_tensor(out=tqn[:], in0=pv, scalar=gcol, in1=tr_[:, 0:TT],
                                                                                             op0=ALU.mult, op1=ALU.mult),
                              reads=[bpb, btr, b_par], writes=[btqn])
                    rps = []
                    for ch, T in enumerate(Ts):
                        (tqn, btqn) = T["qn"]
                        pv3, bpb3 = halfbank(6, ch)
                        tr.op("pe", lambda h, tqn=tqn, pv3=pv3: h.matmul(pv3, lhsT=ropepb[:], rhs=tqn[:], start=True, stop=True),
                              reads=[btqn, b_const], writes=[bpb3])
                        rps.append((pv3, bpb3))
                    for T in Ts:
                        (ta, bta), (tqn, btqn) = T["a"], T["qn"]
                        tr.op("pool", lambda h, ta=ta, tqn=tqn: h.tensor_tensor(out=ta[:, 0:TT], in0=tqn[:], in1=cst[:, 0, :], op=ALU.mult),
                              reads=[btqn, b_cst], writes=[bta])
                    for (pv3, bpb3), T in zip(rps, Ts):
                        (tb, btb) = T["b"]
                        tr.op("dve", lambda h, tb=tb, pv3=pv3: h.tensor_tensor(out=tb[:, 0:TT], in0=pv3, in1=cst[:, 1, :], op=ALU.mult),
                              reads=[bpb3, b_cst], writes=[btb])
                    for ch, T in enumerate(Ts):
                        (ta, bta), (tb, btb) = T["a"], T["b"]
                        if g < 2:
                            tr.op("dve", lambda h, ch=ch, ta=ta, tb=tb: h.tensor_tensor(out=qT[:, g * 4 + ch, :], in0=ta[:, 0:TT], in1=tb[:, 0:TT], op=ALU.add),
                                  reads=[bta, btb], writes=[b_qT])
                        else:
                            for half in range(2):
                                ps_ = slice(half * 64, (half + 1) * 64)
                                tr.op("dve", lambda h, ch=ch, half=half, ps_=ps_, ta=ta, tb=tb: h.tensor_tensor(
                                    out=kT[ps_, ch, half, 128:128 + TT], in0=ta[ps_, 0:TT], in1=tb[ps_, 0:TT], op=ALU.add),
                                    reads=[bta, btb], writes=[b_kT])

                def post_conv(g, outs):
                    if g in (4, 5):
                        for ch in range(4):
                            pv, bpb = outs[ch]
                            cc = (g - 4) * 4 + ch
                            tr.op("act", lambda h, cc=cc, pv=pv: h.activation(out=cgs[:, cc, :], in_=pv, func=AF.Copy), reads=[bpb], writes=[b_cgs])
                    elif g in (6, 7):
                        for ch in range(4):
                            pv, bpb = outs[ch]
                            cc = (g - 6) * 4 + ch
                            tr.op("dve", lambda h, cc=cc, pv=pv: h.tensor_tensor(out=ubuf[:, cc, 2:2 + TT], in0=pv, in1=cgs[:, cc, :], op=ALU.mult),
                                  reads=[bpb, b_cgs], writes=[b_u])
                            tr.op("dve", lambda h, cc=cc: h.tensor_scalar(out=cgs[:, cc, :], in0=ubuf[:, cc, 0:TT], scalar1=convw[:, cc * 3:cc * 3 + 1],
                                                                           scalar2=None, op0=ALU.mult), reads=[b_u, b_par], writes=[b_cgs])
                            for tap in (1, 2):
                                tr.op("dve", lambda h, cc=cc, tap=tap: h.scalar_tensor_tensor(
                                    out=cgs[:, cc, :], in0=ubuf[:, cc, tap:tap + TT], scalar=convw[:, cc * 3 + tap:cc * 3 + tap + 1],
                                    in1=cgs[:, cc, :], op0=ALU.mult, op1=ALU.add), reads=[b_u, b_par, b_cgs], writes=[b_cgs])
                            tr.op("pool", lambda h, cc=cc: h.tensor_copy(out=ubuf[:, cc, 0:2], in_=ubuf[:, cc, TT:TT + 2]), reads=[b_u], writes=[b_u])
                    else:
                        Ts = [tmps() for _ in range(4)]
                        for ch, T in enumerate(Ts):
                            pv, bpb = outs[ch]
                            cc = (g - 8) * 4 + ch
                            (ta, bta) = T["a"]
                            tr.op("dve", lambda h, cc=cc, pv=pv, ta=ta: h.tensor_tensor(out=ta[:, 0:TT], in0=pv, in1=cgs[:, cc, :], op=ALU.mult),
                                  reads=[bpb, b_cgs], writes=[bta])
                        rstd_stages([(T["a"][0][:, 0:TT], T["a"][1]) for T in Ts], Ts, TT)
                        for ch, T in enumerate(Ts):
                            cc = (g - 8) * 4 + ch
                            (tr_, btr), (ta, bta) = T["r"], T["a"]
                            tr.op("dve", lambda h, cc=cc, ta=ta, tr_=tr_: h.scalar_tensor_tensor(out=mixT[:, 8 + cc, :], in0=ta[:, 0:TT], scalar=gconv[:, cc:cc + 1],
                                                                                                in1=tr_[:, 0:TT], op0=ALU.mult, op1=ALU.mult),
                                  reads=[bta, btr, b_par], writes=[b_mixT])

                def v_proj():
                    wb, bwb = wbuf[3 % 2], b_wbuf[3 % 2]
                    for i in range(NI):
                        pb, bpb = nextbank(4, 8)
                        for c in range(16):
                            tr.op("pe", lambda h, c=c, i=i, pb=pb: h.matmul(pb[:, :], lhsT=hT[:, c, i * 128:(i + 1) * 128],
                                                                            rhs=wb[:, c * 512:(c + 1) * 512], start=(c == 0), stop=(c == 15)),
                                  reads=[bwb, b_hT], writes=[bpb], inc=(c == 15))
                        tr.op("act", lambda h, i=i, pb=pb: h.activation(out=vtok[:, 1 + i, :], in_=pb[:, :], func=AF.Copy),
                              reads=[bpb], writes=[b_vtok])
                    load_w(5, wbuf[1], b_wbuf[1])

                def attn_pair(i, hks):
                    n = st * NI + i
                    kbs = ([] if n == 0 else [("prev", i * 128, i, mprevb)]) + [("cur", (i + 1) * 128, i + 1, mcurb)]
                    nk = len(kbs)
                    Ts = [tmps() for _ in hks]
                    sc = {}
                    for a_, hk in enumerate(hks):
                        for kbi, (nm, kc0, vblk, msk) in enumerate(kbs):
                            pb, bpb = pbank[a_ * 2 + kbi], b_pb[a_ * 2 + kbi]
                            for j in range(4):
                                half = j % 2
                                qc = 2 * hk + j // 2
                                tr.op("pe", lambda h, pb=pb, j=j, half=half, qc=qc, kc0=kc0, hk=hk: h.matmul(
                                    pb[:, j * 128:(j + 1) * 128], lhsT=kT[:, hk, half, kc0:kc0 + 128],
                                    rhs=qT[:, qc, i * 128:(i + 1) * 128], start=True, stop=True),
                                    reads=[b_kT, b_qT], writes=[bpb], inc=(j == 3))
                            sc[(a_, kbi)] = (pb, bpb)
                    for a_, hk in enumerate(hks):
                        for kbi in range(nk):
                            pb, bpb = sc[(a_, kbi)]
                            P_, bP = Pt[a_ * 2 + kbi], b_Pt[a_ * 2 + kbi]
                            tr.op("act", lambda h, pb=pb, P_=P_: h.activation(out=P_[:], in_=pb[:, :], func=AF.Exp, scale=0.125),
                                  reads=[bpb], writes=[bP])
                    for a_, hk in enumerate(hks):
                        for kbi, (nm, kc0, vblk, msk) in enumerate(kbs):
                            P_, bP = Pt[a_ * 2 + kbi], b_Pt[a_ * 2 + kbi]
                            tr.op("pool", lambda h, P_=P_, msk=msk: h.tensor_tensor(
                                out=P_[:].rearrange("p (j q) -> p j q", j=4), in0=P_[:].rearrange("p (j q) -> p j q", j=4),
                                in1=msk[:].unsqueeze(1).broadcast_to([128, 4, 128]), op=ALU.mult),
                                reads=[bP, b_const], writes=[bP])
                    pos, pds = [], []
                    for a_, hk in enumerate(hks):
                        po, bpo = pbank[4 + a_], b_pb[4 + a_]
                        pd, bpd = pbank[6 + a_], b_pb[6 + a_]
                        for kbi, (nm, kc0, vblk, msk) in enumerate(kbs):
                            P_, bP = Pt[a_ * 2 + kbi], b_Pt[a_ * 2 + kbi]
                            tr.op("pe", lambda h, P_=P_, vblk=vblk, kbi=kbi, po=po, hk=hk: h.matmul(
                                po[:, :], lhsT=vtok[:, vblk, hk * 128:(hk + 1) * 128], rhs=P_[:],
                                start=(kbi == 0), stop=(kbi == nk - 1)), reads=[bP, b_vtok], writes=[bpo], inc=(kbi == nk - 1))
                        for kbi in range(nk):
                            P_, bP = Pt[a_ * 2 + kbi], b_Pt[a_ * 2 + kbi]
                            tr.op("pe", lambda h, P_=P_, kbi=kbi, pd=pd: h.matmul(pd[:, :], lhsT=onesb[:], rhs=P_[:],
                                                                                 start=(kbi == 0), stop=(kbi == nk - 1)),
                                  reads=[bP, b_const], writes=[bpd], inc=(kbi == nk - 1))
                        pos.append((po, bpo))
                        pds.append((pd, bpd))
                    for a_, T in enumerate(Ts):
                        (tsq, btsq) = T["sq"]
                        po, bpo = pos[a_]
                        tr.op("act", lambda h, tsq=tsq, po=po: h.activation(out=tsq[:], in_=po[:, :], func=AF.Square), reads=[bpo], writes=[btsq])
                    pms = []
                    for a_, T in enumerate(Ts):
                        (tsq, btsq) = T["sq"]
                        pm, bpm = pbank[a_ * 2], b_pb[a_ * 2]
                        tr.op("pe", lambda h, tsq=tsq, pm=pm: h.matmul(pm[:, :], lhsT=bonesb[:], rhs=tsq[:], start=True, stop=True),
                              reads=[btsq, b_const], writes=[bpm])
                        pms.append((pm, bpm))
                    for a_, hk in enumerate(hks):
                        (ta, bta) = Ts[a_]["a"]
                        pd, bpd = pds[a_]
                        for j in range(4):
                            tr.op("act", lambda h, j=j, ta=ta, pd=pd, hk=hk: h.activation(out=ta[:, j * 128:(j + 1) * 128], in_=pd[:, j * 128:(j + 1) * 128],
                                                                                         func=AF.Square, scale=1e-3, bias=esink[:, hk * 4 + j:hk * 4 + j + 1]),
                                  reads=[bpd, b_par], writes=[bta])
                    for a_, T in enumerate(Ts):
                        (tr_, btr), (ta, bta) = T["r"], T["a"]
                        pm, bpm = pms[a_]
                        tr.op("dve", lambda h, tr_=tr_, pm=pm, ta=ta: h.tensor_tensor(out=tr_[:], in0=pm[:, :], in1=ta[:], op=ALU.add),
                              reads=[bpm, bta], writes=[btr])
                    for T in Ts:
                        (tr_, btr) = T["r"]
                        tr.op("act", lambda h, tr_=tr_: h.activation(out=tr_[:], in_=tr_[:], func=AF.Ln), reads=[btr], writes=[btr])
                    for T in Ts:
                        (tr_, btr) = T["r"]
                        tr.op("act", lambda h, tr_=tr_: h.activation(out=tr_[:], in_=tr_[:], func=AF.Exp, scale=-0.5), reads=[btr], writes=[btr])
                    for a_, hk in enumerate(hks):
                        (tr_, btr) = Ts[a_]["r"]
                        po, bpo = pos[a_]
                        for j in range(4):
                            half = j % 2
                            mc = 2 * hk + j // 2
                            ps_ = slice(half * 64, (half + 1) * 64)
                            tr.op("dve", lambda h, j=j, mc=mc, ps_=ps_, po=po, tr_=tr_: h.scalar_tensor_tensor(
                                out=mixT[ps_, mc, i * 128:(i + 1) * 128], in0=po[ps_, j * 128:(j + 1) * 128],
                                scalar=gattn[ps_, mc:mc + 1], in1=tr_[ps_, j * 128:(j + 1) * 128], op0=ALU.mult, op1=ALU.mult),
                                reads=[bpo, btr, b_par], writes=[b_mixT])

                o0 = proj_group(0)
                o1 = proj_group(1)
                post_qk(0, o0)
                o2 = proj_group(2)
                post_qk(1, o1)
                v_proj()
                post_qk(2, o2)
                for i in range(NI):
                    for hp in range(2):
                        attn_pair(i, (2 * hp, 2 * hp + 1))
                tr.op("pool", lambda h: h.tensor_copy(out=kT[:, :, :, 0:128], in_=kT[:, :, :, TT:TT + 128]), reads=[b_kT], writes=[b_kT])
                tr.op("pool", lambda h: h.tensor_copy(out=vtok[:, 0, :], in_=vtok[:, NI, :]), reads=[b_vtok], writes=[b_vtok])
                pend = None
                for g in range(4, 10):
                    o = proj_group(g)
                    if pend is not None:
                        post_conv(*pend)
                    pend = (g, o)
                post_conv(*pend)
                for g in range(10, 14):
                    wb, bwb = wbuf[g % 2], b_wbuf[g % 2]
                    dg = g - 10
                    for i in range(NI):
                        pb, bpb = nextbank()
                        T = tmps()
                        ta, bta = T["a"]
                        for mc in range(16):
                            tr.op("pe", lambda h, mc=mc, i=i, pb=pb: h.matmul(pb[:, :], lhsT=mixT[:, mc, i * 128:(i + 1) * 128],
                                                                             rhs=wb[:, mc * 512:(mc + 1) * 512], start=(mc == 0), stop=(mc == 15)),
                                  reads=[bwb, b_mixT], writes=[bpb], inc=(mc == 15))
                        tr.op("dve", lambda h, pb=pb, dg=dg, ta=ta: h.tensor_tensor(out=ta[:], in0=pb[:, :], in1=gt1b[:, dg * 512:(dg + 1) * 512], op=ALU.mult),
                              reads=[bpb, b_rows], writes=[bta])
                        tr.op("pool", lambda h, i=i, dg=dg, ta=ta: h.tensor_tensor(out=xres[:, i, dg * 512:(dg + 1) * 512], in0=xres[:, i, dg * 512:(dg + 1) * 512],
                                                                                in1=ta[:], op=ALU.add), reads=[bta, b_xres[i]], writes=[b_xres[i]])
                    if g + 2 < 14:
                        load_w(g + 2, wbuf[g % 2], b_wbuf[g % 2])
                tr.barrier()

            if DEBUG_STAGE == "A":
                for i in range(NI):
                    tr.dma("sp", lambda h, i=i: h.dma_start(out=out_d[t0 + i * 128: t0 + (i + 1) * 128, :], in_=xres[:, i, :]),
                           reads=[b_xres[i]], writes=[b_out], sembuf=b_xres[i])
                tr.barrier()
                continue

            with ExitStack() as pp:
                def sbq(name, shape, dt=F32):
                    return pp.enter_context(nc.sbuf_tensor("%s_p%d" % (name, st), list(shape), dt))
                hT = sbq("h2T", [128, 16, TT], BF16); b_hT = B("h2T")
                GT = sbq("GT", [128, TT, 128], BF16); b_GT = B("GT")
                mid_ = ExitStack()
                pqT = mid_.enter_context(nc.sbuf_tensor("pqT_p%d" % st, [128, 16, TT], BF16)); b_pqT = B("pqT")
                with ExitStack() as pq_:
                    def sbq1(name, shape, dt=F32):
                        return pq_.enter_context(nc.sbuf_tensor("%s_q%d" % (name, st), list(shape), dt))
                    xn = sbq1("xn2", [128, NI, D], BF16); b_xn = B("xn2")
                    wbuf = [sbq1("wbq%d" % i, [128, 8192], BF16) for i in range(2)]
                    b_wbuf = [B("wbq%d" % i) for i in range(2)]
                    load_w(14, wbuf[0], b_wbuf[0])
                    load_w(15, wbuf[1], b_wbuf[1])
                    norm_transpose(xn, b_xn, hT, b_hT, 2, 3, 4)
                    for g in range(4):
                        wb, bwb = wbuf[g % 2], b_wbuf[g % 2]
                        for ch in range(4):
                            pb, bpb = nextbank()
                            for c in range(16):
                                tr.op("pe", lambda h, c=c, pb=pb, ch=ch: h.matmul(pb[:, 0:TT], lhsT=wb[:, c * 512 + ch * 128: c * 512 + (ch + 1) * 128],
                                                                                 rhs=hT[:, c, :], start=(c == 0), stop=(c == 15)),
                                      reads=[bwb, b_hT], writes=[bpb], inc=(c == 15))
                            tr.op("act", lambda h, pb=pb, g=g, ch=ch: h.activation(out=pqT[:, g * 4 + ch, :], in_=pb[:, 0:TT], func=AF.Copy),
                                  reads=[bpb], writes=[b_pqT])
                        if g + 2 < 4:
                            load_w(14 + g + 2, wbuf[g % 2], b_wbuf[g % 2])
                    tr.barrier()
                with ExitStack() as p2_:
                    def sbq2(name, shape, dt=F32):
                        return p2_.enter_context(nc.sbuf_tensor("%s_r%d" % (name, st), list(shape), dt))
                    Ssb = sbq2("Ssb", [128, 16, 128]); b_S = B("Ssb")
                    S2s = [sbq2("S2_%d" % k, [128, 128]) for k in range(4)]; b_S2s = [B("S2_%d" % k) for k in range(4)]
                    T16 = sbq2("T16", [128, 16, 16]); b_T16s = [B("T16_%d" % k) for k in range(16)]
                    I16 = sbq2("I16", [128, 16, 16], U32); b_I16s = [B("I16_%d" % k) for k in range(16)]
                    I16f = sbq2("I16f", [128, 16, 16]); b_I16f = B("I16f")
                    cand = sbq2("cand", [128, 8, 256]); b_cand = B("cand")
                    cand2s = [sbq2("cand2_%d" % k, [128, 256]) for k in range(4)]; b_cand2s = [B("cand2_%d" % k) for k in range(4)]
                    C16 = sbq2("C16", [128, 8, 16]); b_C16s = [B("C16_%d" % k) for k in range(8)]
                    CI = sbq2("CI", [128, 8, 16], U32); b_CIs = [B("CI_%d" % k) for k in range(8)]
                    rc = sbq2("rc", [128, 2, 128], U32); b_rc = B("rc")
                    rcf = sbq2("rcf", [128, 2, 128]); b_rcf = B("rcf")
                    oh_flat = cand[:].rearrange("p a b -> p (a b)"); b_oh = b_cand
                    E12 = sbq2("E12", [128, 3, 128]); b_E12 = B("E12")
                    zz = sbq2("zz", [128, 16]); b_zz = B("zz")
                    ejT = sbq2("ejT", [128, 3, 128], BF16); b_ejT = B("ejT")
                    Xb = [sbq2("Xb%d" % k, [128, 32, 128], BF16) for k in range(2)]; b_Xb = [B("Xb%d" % k) for k in range(2)]
                    Yb = [sbq2("Yb%d" % k, [128, 32, 128], BF16) for k in range(2)]; b_Yb = [B("Yb%d" % k) for k in range(2)]
                    gate3 = E12[:, 2, :].rearrange("p (a b) -> p a b", a=8)
                    xyc = {"i": 0}

                    def topk_gen(i):
                        for q4 in range(4):
                            pb, bpb = nextbank()
                            for k4 in range(4):
                                hh = q4 * 4 + k4
                                tr.op("pe", lambda h, pb=pb, k4=k4, hh=hh: h.matmul(pb[:, k4 * 128:(k4 + 1) * 128], lhsT=pqT[:, hh, i * 128:(i + 1) * 128],
                                                                                    rhs=subkb[:, hh, :], start=True, stop=True),
                                      reads=[b_pqT, b_par], writes=[bpb], inc=(k4 == 3))
                            tr.op("act", lambda h, pb=pb, q4=q4: h.activation(out=Ssb[:, q4 * 4:(q4 + 1) * 4, :].rearrange("p a b -> p (a b)"),
                                                                              in_=pb[:, :], func=AF.Copy), reads=[bpb], writes=[b_S])
                            yield
                        for h4 in range(4):
                            hhs = [h4 * 4 + k for k in range(4)]
                            for k, hh in enumerate(hhs):
                                tr.op("dve", lambda h, hh=hh: h.max(out=T16[:, hh, 0:8], in_=Ssb[:, hh, :]), reads=[b_S], writes=[b_T16s[hh]])
                            for k, hh in enumerate(hhs):
                                tr.op("dve", lambda h, hh=hh, k=k: h.match_replace(out=S2s[k][:], in_to_replace=T16[:, hh, 0:8], in_values=Ssb[:, hh, :], imm_value=-1e30),
                                      reads=[b_S, b_T16s[hh]], writes=[b_S2s[k]])
                            for k, hh in enumerate(hhs):
                                tr.op("dve", lambda h, hh=hh, k=k: h.max(out=T16[:, hh, 8:16], in_=S2s[k][:]), reads=[b_S2s[k]], writes=[b_T16s[hh]])
                            for k, hh in enumerate(hhs):
                                tr.op("dve", lambda h, hh=hh: h.max_index(out=I16[:, hh, 0:8], in_max=T16[:, hh, 0:8], in_values=Ssb[:, hh, :]),
                                      reads=[b_S, b_T16s[hh]], writes=[b_I16s[hh]])
                            for k, hh in enumerate(hhs):
                                tr.op("dve", lambda h, hh=hh, k=k: h.max_index(out=I16[:, hh, 8:16], in_max=T16[:, hh, 8:16], in_values=S2s[k][:]),
                                      reads=[b_S2s[k], b_T16s[hh]], writes=[b_I16s[hh]])
                            yield
                        tr.op("dve", lambda h: h.tensor_copy(out=I16f[:], in_=I16[:]), reads=b_I16s, writes=[b_I16f])
                        t16 = T16[:]
                        in0 = _ap(t16, 0, [[32, 8], [1, 16], [0, 16]])
                        in1 = _ap(t16, 16, [[32, 8], [0, 16], [1, 16]])
                        tr.op("dve", lambda h: h.tensor_tensor(out=cand[:].rearrange("p a (b c) -> p a b c", b=16), in0=in0, in1=in1, op=ALU.add),
                              reads=b_T16s, writes=[b_cand])
                        for h4 in range(2):
                            hds = [h4 * 4 + k for k in range(4)]
                            for k, hd in enumerate(hds):
                                tr.op("dve", lambda h, hd=hd: h.max(out=C16[:, hd, 0:8], in_=cand[:, hd, :]), reads=[b_cand], writes=[b_C16s[hd]])
                            for k, hd in enumerate(hds):
                                tr.op("dve", lambda h, hd=hd, k=k: h.match_replace(out=cand2s[k][:], in_to_replace=C16[:, hd, 0:8], in_values=cand[:, hd, :], imm_value=-1e30),
                                      reads=[b_cand, b_C16s[hd]], writes=[b_cand2s[k]])
                            for k, hd in enumerate(hds):
                                tr.op("dve", lambda h, hd=hd, k=k: h.max(out=C16[:, hd, 8:16], in_=cand2s[k][:]), reads=[b_cand2s[k]], writes=[b_C16s[hd]])
                            for k, hd in enumerate(hds):
                                tr.op("dve", lambda h, hd=hd: h.max_index(out=CI[:, hd, 0:8], in_max=C16[:, hd, 0:8], in_values=cand[:, hd, :]),
                                      reads=[b_cand, b_C16s[hd]], writes=[b_CIs[hd]])
                            for k, hd in enumerate(hds):
                                tr.op("dve", lambda h, hd=hd, k=k: h.max_index(out=CI[:, hd, 8:16], in_max=C16[:, hd, 8:16], in_values=cand2s[k][:]),
                                      reads=[b_cand2s[k], b_C16s[hd]], writes=[b_CIs[hd]])
                            yield
                        c16 = C16[:]
                        tr.op("dve", lambda h: h.tensor_tensor(out=gate3, in0=c16, in1=_ap(c16, 0, [[16, 8], [0, 16]]), op=ALU.subtract),
                              reads=b_C16s, writes=[b_E12])
                        tr.op("act", lambda h: h.activation(out=E12[:, 2, :], in_=E12[:, 2, :], func=AF.Exp), reads=[b_E12], writes=[b_E12])
                        tr.op("dve", lambda h: h.tensor_reduce(out=zz[:, 0:8], in_=gate3, axis=AX.X, op=ALU.add), reads=[b_E12], writes=[b_zz])
                        tr.op("dve", lambda h: h.reciprocal(out=zz[:, 8:16], in_=zz[:, 0:8]), reads=[b_zz], writes=[b_zz])
                        tr.op("dve", lambda h: h.tensor_tensor(out=gate3, in0=gate3, in1=_ap(zz[:], 8, [[1, 8], [0, 16]]), op=ALU.mult),
                              reads=[b_E12, b_zz], writes=[b_E12])
                        cif = CI[:].rearrange("p a b -> p (a b)")
                        tr.op("dve", lambda h: h.tensor_single_scalar(out=rc[:, 0, :], in_=cif, scalar=4, op=ALU.logical_shift_right),
                              reads=b_CIs, writes=[b_rc])
                        tr.op("dve", lambda h: h.tensor_single_scalar(out=rc[:, 1, :], in_=cif, scalar=15, op=ALU.bitwise_and),
                              reads=b_CIs, writes=[b_rc])
                        tr.op("dve", lambda h: h.tensor_copy(out=rcf[:], in_=rc[:]), reads=[b_rc], writes=[b_rcf])
                        i16f = I16f[:]
                        for w in range(2):
                            tr.op("dve", lambda h, w=w: h.tensor_tensor(out=oh_flat.rearrange("p (a b) -> p a b", b=16),
                                                                        in0=_ap(rcf[:], w * 128, [[1, 128], [0, 16]]),
                                                                        in1=_ap(iota16[:], 0, [[0, 128], [1, 16]]), op=ALU.is_equal),
                                  reads=[b_rcf, b_const], writes=[b_oh])
                            tr.op("dve", lambda h, w=w: h.tensor_tensor(out=oh_flat.rearrange("p (a k b) -> p a k b", a=8, k=16),
                                                                        in0=oh_flat.rearrange("p (a k b) -> p a k b", a=8, k=16),
                                                                        in1=_ap(i16f, w * 16, [[32, 8], [0, 16], [1, 16]]), op=ALU.mult),
                                  reads=[b_oh, b_I16f], writes=[b_oh])
                            tr.op("dve", lambda h, w=w: h.tensor_reduce(out=E12[:, w, :], in_=oh_flat.rearrange("p (a b) -> p a b", b=16), axis=AX.X, op=ALU.add),
                                  reads=[b_oh], writes=[b_E12])
                        yield

                    def ggen_gen(i):
                        pb, bpb = nextbank()
                        for k in range(3):
                            tr.op("pe", lambda h, pb=pb, k=k: h.transpose(out=pb[:, k * 128:(k + 1) * 128], in_=E12[:, k, :], identity=identf[:]),
                                  reads=[b_E12, b_const], writes=[bpb], inc=(k == 2))
                        tr.op("act", lambda h, pb=pb: h.activation(out=ejT[:].rearrange("p a b -> p (a b)"), in_=pb[:, 0:384], func=AF.Copy),
                              reads=[bpb], writes=[b_ejT])
                        yield
                        iob = _ap(iotab[:], 0, [[0, 32], [1, 128]])
                        for tg in range(4):
                            k_ = xyc["i"] % 2
                            xyc["i"] += 1
                            X_, bX, Y_, bY = Xb[k_], b_Xb[k_], Yb[k_], b_Yb[k_]
                            tr.op("dve", lambda h, X_=X_, tg=tg: h.tensor_tensor(out=X_[:], in0=iob, in1=_ap(ejT[:], 0 * 128 + tg * 32, [[1, 32], [0, 128]]),
                                                                                 op=ALU.is_equal), reads=[b_const, b_ejT], writes=[bX])
                            tr.op("pool", lambda h, X_=X_, tg=tg: h.tensor_tensor(out=X_[:], in0=X_[:], in1=_ap(ejT[:], 2 * 128 + tg * 32, [[1, 32], [0, 128]]),
                                                                                  op=ALU.mult), reads=[bX, b_ejT], writes=[bX])
                            tr.op("dve", lambda h, Y_=Y_, tg=tg: h.tensor_tensor(out=Y_[:], in0=iob, in1=_ap(ejT[:], 1 * 128 + tg * 32, [[1, 32], [0, 128]]),
                                                                                 op=ALU.is_equal), reads=[b_const, b_ejT], writes=[bY])
                            for t4 in range(8):
                                pg, bpg = nextbank()
                                for tt in range(4):
                                    tl = t4 * 4 + tt
                                    tr.op("pe", lambda h, pg=pg, tt=tt, tl=tl, X_=X_, Y_=Y_: h.matmul(pg[:, tt * 128:(tt + 1) * 128], lhsT=Y_[:, tl, :], rhs=X_[:, tl, :],
                                                                                                    start=True, stop=True),
                                          reads=[bX, bY], writes=[bpg], inc=(tt == 3))
                                tok0 = i * 128 + tg * 32 + t4 * 4
                                tr.op("act", lambda h, pg=pg, tok0=tok0: h.activation(out=GT[:, tok0:tok0 + 4, :].rearrange("p t e -> p (t e)"),
                                                                                      in_=pg[:, :], func=AF.Copy),
                                      reads=[bpg], writes=[b_GT])
                            yield
                        yield

                    def drain(*gw):
                        gw = [list(x) for x in gw]
                        while gw:
                            for item in list(gw):
                                for _ in range(item[1]):
                                    try:
                                        next(item[0])
                                    except StopIteration:
                                        gw.remove(item)
                                        break

                    drain((topk_gen(0), 1))
                    drain((ggen_gen(0), 1), (topk_gen(1), 3))
                    drain((ggen_gen(1), 1))
                    tr.barrier()
                mid_.close()
                with ExitStack() as p3_:
                    def sbq3(name, shape, dt=F32):
                        return p3_.enter_context(nc.sbuf_tensor("%s_s%d" % (name, st), list(shape), dt))
                    UTg = [sbq3("UTg%d" % k, [128, 8192], BF16) for k in range(2)]; b_UTg = [B("UTg%d" % k) for k in range(2)]
                    Vgr = [sbq3("Vgr%d" % k, [128, 8192], BF16) for k in range(2)]; b_Vgr = [B("Vgr%d" % k) for k in range(2)]
                    gl = [sbq3("gl%d" % k, [128, TT], BF16) for k in range(2)]; b_gl = [B("gl%d" % k) for k in range(2)]
                    GA = [sbq3("GA%d" % k, [128, 4, TT], BF16) for k in range(2)]; b_GA = [B("GA%d" % k) for k in range(2)]
                    ytmp = [sbq3("ytmp%d" % k, [128, 512]) for k in range(2)]; b_ytmp = [B("ytmp%d" % k) for k in range(2)]

                    def load_u(g):
                        k_ = g % 2
                        for hh in range(2):
                            tr.dma("sp", lambda h, hh=hh: h.dma_start(out=UTg[k_][:, hh * 4096:(hh + 1) * 4096], in_=us_d[g][:, hh * 4096:(hh + 1) * 4096]),
                                   reads=[b_uvs], writes=[b_UTg[k_]])

                    def load_v(g):
                        k_ = g % 2
                        for hh in range(2):
                            tr.dma("sp", lambda h, hh=hh: h.dma_start(out=Vgr[k_][:, hh * 4096:(hh + 1) * 4096], in_=vs_d[g][:, hh * 4096:(hh + 1) * 4096]),
                                   reads=[b_uvs], writes=[b_Vgr[k_]])

                    glc = {"i": 0}
                    ycnt = {"i": 0}

                    def a_stage(g):
                        k_ = g % 2
                        ut, but, ga_, bga = UTg[k_], b_UTg[k_], GA[k_], b_GA[k_]
                        for c4 in range(4):
                            c = g * 4 + c4
                            pa, bpa = nextbank(0, 4)
                            for dc in range(16):
                                tr.op("pe", lambda h, dc=dc, pa=pa, c4=c4: h.matmul(pa[:, 0:TT], lhsT=ut[:, dc * 512 + c4 * 128: dc * 512 + (c4 + 1) * 128],
                                                                                   rhs=hT[:, dc, :], start=(dc == 0), stop=(dc == 15)),
                                      reads=[but, b_hT], writes=[bpa], inc=(dc == 15))
                            gl_, bgl = gl[glc["i"] % 2], b_gl[glc["i"] % 2]
                            glc["i"] += 1
                            tr.op("act", lambda h, pa=pa, gl_=gl_: h.activation(out=gl_[:], in_=pa[:, 0:TT], func=AF.Gelu), reads=[bpa], writes=[bgl])
                            tr.op("dve", lambda h, gl_=gl_, c4=c4, c=c: h.tensor_tensor(out=ga_[:, c4, :], in0=gl_[:], in1=GT[:, :, c], op=ALU.mult),
                                  reads=[bgl, b_GT], writes=[bga])
                        if g + 2 < 32:
                            load_u(g + 2)

                    def y_stage(g):
                        k_ = g % 2
                        vg, bvg, ga_, bga = Vgr[k_], b_Vgr[k_], GA[k_], b_GA[k_]
                        for i in range(NI):
                            for dt_ in range(4):
                                py, bpy = nextbank(4, 8)
                                for c4 in range(4):
                                    tr.op("pe", lambda h, py=py, c4=c4, i=i, dt_=dt_: h.matmul(py[:, :], lhsT=ga_[:, c4, i * 128:(i + 1) * 128],
                                                                                              rhs=vg[:, c4 * 2048 + dt_ * 512: c4 * 2048 + (dt_ + 1) * 512],
                                                                                              start=(c4 == 0), stop=(c4 == 3)),
                                          reads=[bga, bvg], writes=[bpy], inc=(c4 == 3))
                                xs = xres[:, i, dt_ * 512:(dt_ + 1) * 512]
                                yc = ycnt["i"]
                                ycnt["i"] += 1
                                if yc % 2 == 0:
                                    tr.op("dve", lambda h, py=py, xs=xs: h.tensor_tensor(out=xs, in0=py[:, :], in1=xs, op=ALU.add),
                                          reads=[bpy, b_xres[i]], writes=[b_xres[i]])
                                else:
                                    yt, byt = ytmp[(yc // 2) % 2], b_ytmp[(yc // 2) % 2]
                                    tr.op("act", lambda h, py=py, yt=yt: h.activation(out=yt[:], in_=py[:, :], func=AF.Copy), reads=[bpy], writes=[byt])
                                    tr.op("pool", lambda h, yt=yt, xs=xs: h.tensor_tensor(out=xs, in0=yt[:], in1=xs, op=ALU.add),
                                          reads=[byt, b_xres[i]], writes=[b_xres[i]])
                        if g + 2 < 32:
                            load_v(g + 2)

                    load_u(0)
                    load_v(0)
                    load_u(1)
                    load_v(1)
                    if st + 1 < N_SUPER:
                        for i in range(NI):
                            tr.dma("sp", lambda h, i=i: h.dma_start(out=xres2[:, (1 - par_) * NI + i, :],
                                                                    in_=x_d[t0 + TT + i * 128: t0 + TT + (i + 1) * 128, :]),
                                   writes=[b_xres2[1 - par_][i]])
                    a_stage(0)
                    for g in range(32):
                        if g + 1 < 32:
                            a_stage(g + 1)
                        y_stage(g)
                    for i in range(NI):
                        tr.dma("sp", lambda h, i=i: h.dma_start(out=out_d[t0 + i * 128: t0 + (i + 1) * 128, :], in_=xres[:, i, :]),
                               reads=[b_xres[i]], writes=[b_out], sembuf=b_xres[i])
                    tr.barrier()
        tr.wait_all("sp", [b_out])
    return nc, tr.ninst


def _consts():
    identf = np.eye(128, dtype=np.float32)
    blk = np.arange(128) // 64
    bones = (blk[:, None] == blk[None, :]).astype(np.float32) / 64.0
    P = np.zeros((128, 128), np.float32)
    for hb in (0, 64):
        for i in range(8):
            P[hb + i, hb + i + 8] = -1.0
            P[hb + i + 8, hb + i] = 1.0
    ropepT = np.ascontiguousarray(P.T)
    kk = np.arange(128)[:, None]
    qq = np.arange(128)[None, :]
    mprev = (kk > qq).astype(np.float32)
    mcur = (kk <= qq).astype(np.float32)
    pos = np.arange(S, dtype=np.float32)
    inv_freq = (np.float32(500000.0) ** (-np.arange(0, 16, 2, dtype=np.float32) / np.float32(16))).astype(np.float32)
    ang = (pos[:, None] * inv_freq[None, :]).astype(np.float32)
    cos8 = np.cos(ang).astype(np.float32).T
    sin8 = np.sin(ang).astype(np.float32).T
    cosT = np.ones((128, S), np.float32)
    sinT = np.zeros((128, S), np.float32)
    for hb in (0, 64):
        cosT[hb:hb + 8] = cos8
        cosT[hb + 8:hb + 16] = cos8
        sinT[hb:hb + 8] = sin8
        sinT[hb + 8:hb + 16] = sin8
    iota128 = np.tile(np.arange(128, dtype=np.float32)[None, :], (128, 1))
    return dict(identf=identf, bones=bones, ropepT=ropepT, mprev=mprev, mcur=mcur, cosT=cosT, sinT=sinT, iota128=iota128)


def _layout_shared(w_ada, b_ada, g_norm1, w_in, g_q, g_k, sinks, conv_w, g_out_attn, g_out_conv,
                   w_out, g_norm2, w_pq, peer_subkeys, peer_u, peer_v):
    f = lambda a: np.ascontiguousarray(np.asarray(a, dtype=np.float32))
    col16 = lambda v: f(np.asarray(v).reshape(16, 128).T)
    w_in = np.asarray(w_in)
    q = w_in[:, 0:1024]
    k = w_in[:, 1024:1280]
    v = w_in[:, 1280:1536]
    bg = w_in[:, 1536:2560]
    cg = w_in[:, 2560:3584]
    hc = w_in[:, 3584:4608]
    kd = np.concatenate([k[:, h * 64:(h + 1) * 64] for h in range(4) for _ in range(2)], axis=1)
    vd = np.concatenate([v[:, h * 64:(h + 1) * 64] for h in range(4) for _ in range(2)], axis=1)
    w_in2 = f(np.concatenate([q, kd, vd, cg, hc, bg], axis=1))
    subkT = f(np.asarray(peer_subkeys).reshape(16, 128, 128).transpose(2, 0, 1).reshape(128, 16 * 128))
    convw = f(np.asarray(conv_w).reshape(3, 8, 128).transpose(2, 1, 0).reshape(128, 24))
    d = dict(
        w_ada=f(w_ada), b_ada=f(np.asarray(b_ada).reshape(1, -1)), g1col=col16(g_norm1), g2col=col16(g_norm2),
        w_in2=w_in2,
        gqcol=f(np.tile(np.asarray(g_q), 2).reshape(128, 1)), gkcol=f(np.tile(np.asarray(g_k), 2).reshape(128, 1)),
        sinks=f(np.asarray(sinks).reshape(1, 16)), convw=convw,
        gattn=f(np.asarray(g_out_attn).reshape(8, 128).T), gconv=f(np.asarray(g_out_conv).reshape(8, 128).T),
        w_out=f(w_out), w_pq=f(w_pq), subkT=subkT, peer_uT=f(np.asarray(peer_u).T), peer_v=f(peer_v),
    )
    d.update(_consts())
    return d


def kernel(x, c, w_ada, b_ada, g_norm1, w_in, g_q, g_k, sinks, conv_w, g_out_attn, g_out_conv,
           w_out, g_norm2, w_pq, peer_subkeys, peer_u, peer_v, _cores=None):
    x = np.asarray(x, dtype=np.float32)
    c = np.asarray(c, dtype=np.float32)
    shared = _layout_shared(w_ada, b_ada, g_norm1, w_in, g_q, g_k, sinks, conv_w, g_out_attn, g_out_conv,
                            w_out, g_norm2, w_pq, peer_subkeys, peer_u, peer_v)
    cores = list(range(8)) if _cores is None else list(_cores)
    nc, _ = build_program()
    in_maps = []
    for b in cores:
        m = dict(shared)
        m["x"] = np.ascontiguousarray(x[b])
        m["ccol"] = np.ascontiguousarray(c[b].reshape(16, 128).T)
        in_maps.append(m)
    res = run_bass_kernel_spmd(nc, in_maps, core_ids=list(range(len(cores))))
    outs = [np.asarray(r["out"], dtype=np.float32) for r in res.results]
    if _cores is not None:
        return outs
    return np.stack(outs, axis=0)
```

```python
import numpy as np
from contextlib import ExitStack
import concourse.bass as bass
import concourse.mybir as mybir
from concourse.bass_utils import run_bass_kernel_spmd

F32 = mybir.dt.float32
BF16 = mybir.dt.bfloat16
U32 = mybir.dt.uint32
I32 = mybir.dt.int32
ALU = mybir.AluOpType
AF = mybir.ActivationFunctionType
AX = mybir.AxisListType

S = 4096
D = 2048
TT = 256
NST = S // TT
NI = TT // 128
EPS = 1e-6
NGRP = 18
INC = 5120

DEBUG_STAGE = None
N_SUPER = NST


class Buf:
    __slots__ = ("name", "w", "r", "dsem", "dcnt")

    def __init__(self, name):
        self.name = name
        self.w = None
        self.r = []
        self.dsem = None
        self.dcnt = 0


class Tracker:
    def __init__(self, nc, es):
        self.nc = nc
        self.es = es
        self.eng = {}
        for name, h in (("pe", nc.tensor), ("act", nc.scalar), ("dve", nc.vector),
                        ("pool", nc.gpsimd), ("sp", nc.sync)):
            sem = es.enter_context(nc.semaphore("sem_" + name))
            self.eng[name] = {"h": h, "sem": sem, "cnt": 0, "seen": {}, "name": name}
        self.bufs = {}
        self.dsems = {}
        self.ninst = 0

    def buf(self, name):
        b = Buf(name)
        self.bufs[name] = b
        return b

    def _wait(self, e, toks):
        best = {}
        for t in toks:
            if t is None:
                continue
            sem, val = t
            k = id(sem)
            if e["seen"].get(k, 0) >= val:
                continue
            if k not in best or best[k][1] < val:
                best[k] = (sem, val)
        for k, (sem, val) in best.items():
            if e["name"] == "pe" and sem is e["sem"]:
                continue
            e["h"].wait_ge(sem, val)
            e["seen"][k] = val
            self.ninst += 1

    @staticmethod
    def _deps(reads, writes):
        toks = []
        for b in reads:
            toks.append(b.w)
        for b in writes:
            toks.append(b.w)
            toks.extend(b.r)
        return toks

    def op(self, en, fn, reads=(), writes=(), inc=True):
        e = self.eng[en]
        self._wait(e, self._deps(reads, writes))
        ins = fn(e["h"])
        self.ninst += 1
        if inc:
            e["cnt"] += 1
            ins.then_inc(e["sem"], 1)
            tok = (e["sem"], e["cnt"])
        else:
            tok = (e["sem"], e["cnt"] + 1)
        for b in reads:
            b.r.append(tok)
        for b in writes:
            b.w = tok
            b.r = []
        return tok

    def dma(self, en, fn, reads=(), writes=(), sembuf=None):
        e = self.eng[en]
        sb = sembuf if sembuf is not None else (writes[0] if writes else reads[0])
        if sb.dsem is None:
            if sb.name not in self.dsems:
                self.dsems[sb.name] = [self.es.enter_context(self.nc.semaphore("ds_" + sb.name)), 0]
            sb.dsem = self.dsems[sb.name][0]
            sb.dcnt = self.dsems[sb.name][1]
        toks = [t for t in self._deps(reads, writes) if not (t is not None and t[0] is sb.dsem)]
        self._wait(e, toks)
        ins = fn(e["h"])
        self.ninst += 1
        sb.dcnt += 16
        self.dsems[sb.name][1] = sb.dcnt
        ins.then_inc(sb.dsem, 16)
        tok = (sb.dsem, sb.dcnt)
        for b in reads:
            b.r.append(tok)
        for b in writes:
            b.w = tok
            b.r = []
        return tok

    def wait_all(self, en, bufs):
        e = self.eng[en]
        toks = []
        for b in bufs:
            toks.append(b.w)
            toks.extend(b.r)
        self._wait(e, toks)

    def barrier(self):
        sp = self.eng["sp"]
        toks = []
        for n, e in self.eng.items():
            if n != "sp" and e["cnt"] > 0:
                toks.append((e["sem"], e["cnt"]))
        for nm, (dsem, dcnt) in self.dsems.items():
            if dcnt > 0:
                toks.append((dsem, dcnt))
        self._wait(sp, toks)
        sp["cnt"] += 1
        sp["h"].nop().then_inc(sp["sem"], 1)
        self.ninst += 1
        tok = (sp["sem"], sp["cnt"])
        for n, e in self.eng.items():
            if n != "sp":
                self._wait(e, [tok])
        for b in self.bufs.values():
            b.w = None
            b.r = []


def _ap(src, offset, dims):
    return bass.AP(src.tensor, src.offset + offset, [list(src.ap[0])] + [list(d) for d in dims])


def build_program():
    nc = bass.Bass("TRN2", target_bir_lowering=False)

    def din(name, shape, dt=F32):
        return nc.dram_tensor(name, list(shape), dt, kind="ExternalInput")

    x_d = din("x", [S, D])
    ccol_d = din("ccol", [128, 16])
    wada_d = din("w_ada", [D, 6 * D])
    bada_d = din("b_ada", [1, 6 * D])
    g1col_d = din("g1col", [128, 16])
    g2col_d = din("g2col", [128, 16])
    win_d = din("w_in2", [D, INC])
    gq_d = din("gqcol", [128, 1])
    gk_d = din("gkcol", [128, 1])
    sinks_d = din("sinks", [1, 16])
    convw_d = din("convw", [128, 24])
    gattn_d = din("gattn", [128, 8])
    gconv_d = din("gconv", [128, 8])
    wout_d = din("w_out", [D, D])
    wpq_d = din("w_pq", [D, D])
    subk_d = din("subkT", [128, 16 * 128])
    put_d = din("peer_uT", [D, 16384])
    pv_d = din("peer_v", [16384, D])
    identf_d = din("identf", [128, 128])
    bones_d = din("bones", [128, 128])
    ropep_d = din("ropepT", [128, 128])
    mprev_d = din("mprev", [128, 128])
    mcur_d = din("mcur", [128, 128])
    cos_d = din("cosT", [128, S])
    sin_d = din("sinT", [128, S])
    iota_d = din("iota128", [128, 128])
    out_d = nc.dram_tensor("out", [S, D], F32, kind="ExternalOutput")
    wsc_d = nc.dram_tensor("wsc", [NGRP, 128, 16 * 512], BF16)
    us_d = nc.dram_tensor("us", [32, 128, 8192], BF16)
    gt1_d = nc.dram_tensor("gt1s", [128, D], F32)
    vs_d = nc.dram_tensor("vs", [32, 128, 8192], BF16)

    with ExitStack() as es:
        def sb(name, shape, dt=F32):
            return es.enter_context(nc.sbuf_tensor(name, list(shape), dt))

        tr = Tracker(nc, es)
        B = tr.buf

        xres2 = sb("xres", [128, 2 * NI, D])
        b_xres2 = [[B("xres%d_%d" % (p_, i)) for i in range(NI)] for p_ in range(2)]
        xres = xres2[:, 0:NI, :]; b_xres = b_xres2[0]
        b_rows = B("rows")
        cols = sb("cols", [128, 4, 16]); b_cols = B("cols")
        identb = sb("identb", [128, 128], BF16); bonesb = sb("bonesb", [128, 128], BF16)
        ropepb = sb("ropepb", [128, 128], BF16); mprevb = sb("mprevb", [128, 128], BF16)
        mcurb = sb("mcurb", [128, 128], BF16); onesb = sb("onesb", [128, 128], BF16)
        onesf = sb("onesf", [1, 128]); iota16 = sb("iota16s", [128, 16]); iotab = sb("iotab", [128, 128], BF16); identf = sb("identf_s", [128, 128])
        b_const = B("const")
        gqk = sb("gqk", [128, 2]); convw = sb("convw_s", [128, 24]); gattn = sb("gattn_s", [128, 8])
        gconv = sb("gconv_s", [128, 8]); esink = sb("esink", [128, 16]); subkb = sb("subkb", [128, 16, 128], BF16)
        b_par = B("par")
        kT = sb("kT", [128, 4, 2, 128 + TT], BF16); b_kT = B("kT")
        vtok = sb("vtok", [128, NI + 1, 512], BF16); b_vtok = B("vtok")
        ubuf = sb("ubuf", [128, 8, 2 + TT]); b_u = B("ubuf")
        stat = sb("stat", [128, 8]); b_stat = B("stat")
        epsc = sb("epsc", [128, 1])

        pbank = [es.enter_context(nc.psum_tensor("pb%d" % i, [128, 512], F32)) for i in range(8)]
        b_pb = [B("pb%d" % i) for i in range(8)]
        rr = {"i": 0}

        def nextbank(lo=0, hi=8):
            i = lo + (rr["i"] % (hi - lo))
            rr["i"] += 1
            return pbank[i], b_pb[i]

        es.enter_context(nc.Block())
        b_out = B("out")
        b_wsc1 = B("wsc"); b_wsc = [b_wsc1] * NGRP
        b_uvs = B("uvs")

        with ExitStack() as ps:
            def sbp(name, shape, dt=F32):
                return ps.enter_context(nc.sbuf_tensor(name, list(shape), dt))
            NSTG = 3
            stage = [sbp("stage%d" % i, [128, 8, 512]) for i in range(NSTG)]
            b_stage = [B("stage%d" % i) for i in range(NSTG)]
            cvt = [sbp("cvt%d" % i, [128, 8 * 512], BF16) for i in range(2)]
            b_cvt = [B("cvt%d" % i) for i in range(2)]
            ccol = sbp("ccol_s", [128, 16]); b_ccol = B("ccol")
            ctmp = sbp("ctmp", [128, 128]); b_ctmp = B("ctmp")
            g12 = sbp("g12", [128, 2, 16]); b_g12 = B("g12")
            subkf = sbp("subkf", [128, 16 * 128]); b_subkf = B("subkf")
            gt2b = sbp("gt2b", [128, D]); b_gt2 = B("gt2b")
            gt1b = sbp("gt1b_p", [128, D]); b_gt1s = B("gt1s")
            pm_ = ExitStack()
            modrow = pm_.enter_context(nc.sbuf_tensor("modrow", [1, 6 * D], F32)); b_mod = B("modrow")

            sp_loads = [
                (ccol[:], ccol_d.ap(), b_ccol), (modrow[:], bada_d.ap(), b_mod),
                (g12[:, 0, :], g1col_d.ap(), b_g12), (g12[:, 1, :], g2col_d.ap(), b_g12),
                (gqk[:, 0:1], gq_d.ap(), b_par), (gqk[:, 1:2], gk_d.ap(), b_par),
                (convw[:], convw_d.ap(), b_par), (gattn[:], gattn_d.ap(), b_par),
                (gconv[:], gconv_d.ap(), b_par), (iota16[:], iota_d[:, 0:16], b_const), (identf[:], identf_d.ap(), b_const),
                (esink[:], bass.AP(sinks_d, 0, [[0, 128], [1, 16]]), b_par), (subkf[:], subk_d.ap(), b_subkf),
            ]
            for o, i_, bb in sp_loads:
                tr.dma("sp", lambda h, o=o, i_=i_: h.dma_start(out=o, in_=i_), writes=[bb])
            for k, (src_d, dst) in enumerate([(identf_d, identb), (bones_d, bonesb), (ropep_d, ropepb),
                                              (mprev_d, mprevb), (mcur_d, mcurb)]):
                tr.dma("sp", lambda h, s=src_d: h.dma_start(out=ctmp[:], in_=s.ap()), writes=[b_ctmp])
                tr.op("dve", lambda h, d=dst: h.tensor_copy(out=d[:], in_=ctmp[:]), reads=[b_ctmp], writes=[b_const])
            tr.op("dve", lambda h: h.memset(onesb[:], 1.0), writes=[b_const])
            tr.op("dve", lambda h: h.memset(epsc[:], EPS), writes=[b_const])
            tr.dma("sp", lambda h: h.dma_start(out=ctmp[:], in_=iota_d.ap()), writes=[b_ctmp])
            tr.op("dve", lambda h: h.tensor_copy(out=iotab[:], in_=ctmp[:]), reads=[b_ctmp], writes=[b_const])
            tr.op("dve", lambda h: h.memset(onesf[:], 1.0), writes=[b_const])
            tr.op("dve", lambda h: h.tensor_copy(out=subkb[:].rearrange("p a b -> p (a b)"), in_=subkf[:]),
                  reads=[b_subkf], writes=[b_par])
            tr.op("act", lambda h: h.activation(out=esink[:], in_=esink[:], func=AF.Exp), reads=[b_par], writes=[b_par])
            tr.op("dve", lambda h: h.tensor_scalar(out=esink[:], in0=esink[:], scalar1=1e-3, scalar2=None, op0=ALU.mult), reads=[b_par], writes=[b_par])
            tr.op("act", lambda h: h.activation(out=ccol[:], in_=ccol[:], func=AF.Silu), reads=[b_ccol], writes=[b_ccol])

            if DEBUG_STAGE == "p1":
                tr.barrier()
                return nc, tr.ninst
            ntile = 0
            for t in range(24):
                pb, bpb = nextbank()
                for hh in range(2):
                    sl = ntile % NSTG
                    ntile += 1
                    src = wada_d.ap().rearrange("(c p) n -> p c n", p=128)[:, hh * 8:(hh + 1) * 8, t * 512:(t + 1) * 512]
                    tr.dma("sp", lambda h, sl=sl, src=src: h.dma_start(out=stage[sl][:], in_=src), writes=[b_stage[sl]])
                    for c8 in range(8):
                        c = hh * 8 + c8
                        tr.op("pe", lambda h, c=c, c8=c8, pb=pb, sl=sl: h.matmul(pb[0:1, :], lhsT=ccol[:, c:c + 1], rhs=stage[sl][:, c8, :],
                                                                                 start=(c == 0), stop=(c == 15)),
                              reads=[b_ccol, b_stage[sl]], writes=[bpb], inc=(c8 == 7))
                tr.op("dve", lambda h, pb=pb, t=t: h.tensor_tensor(out=modrow[0:1, t * 512:(t + 1) * 512], in0=pb[0:1, :],
                                                                   in1=modrow[0:1, t * 512:(t + 1) * 512], op=ALU.add),
                      reads=[bpb, b_mod], writes=[b_mod])
            if DEBUG_STAGE == "p2":
                tr.barrier()
                return nc, tr.ninst
            for dst, sec, bdst in ((gt1b, 2, b_rows), (gt2b, 5, b_gt2)):
                for dg in range(4):
                    pb, bpb = nextbank()
                    tr.op("pe", lambda h, pb=pb, sec=sec, dg=dg: h.matmul(pb[:, :], lhsT=onesf[0:1, :],
                                                                          rhs=modrow[0:1, sec * D + dg * 512: sec * D + (dg + 1) * 512],
                                                                          start=True, stop=True),
                          reads=[b_mod, b_const], writes=[bpb])
                    tr.op("act", lambda h, pb=pb, dst=dst, dg=dg: h.activation(out=dst[:, dg * 512:(dg + 1) * 512], in_=pb[:, :], func=AF.Copy),
                          reads=[bpb], writes=[bdst])
            tr.dma("sp", lambda h: h.dma_start(out=gt1_d.ap(), in_=gt1b[:]), reads=[b_rows], writes=[b_gt1s], sembuf=b_rows)
            pb, bpb = nextbank()
            for k, sec in enumerate((0, 1, 3, 4)):
                for c in range(16):
                    tr.op("pe", lambda h, pb=pb, k=k, c=c, sec=sec: h.matmul(
                        pb[:, (k * 16 + c) * 2:(k * 16 + c) * 2 + 2], lhsT=modrow[0:1, sec * D + c * 128: sec * D + (c + 1) * 128],
                        rhs=onesf[0:1, 0:2], start=True, stop=True), reads=[b_mod, b_const], writes=[bpb],
                        inc=(k == 3 and c == 15))
            pbv = pb[:, 0:128].rearrange("p (k c two) -> p k c two", k=4, c=16, two=2)
            tr.op("dve", lambda h: h.tensor_copy(out=cols[:], in_=pbv[:, :, :, 0]), reads=[bpb], writes=[b_cols])
            for k, gi in ((1, 0), (3, 1)):
                tr.op("dve", lambda h, k=k, gi=gi: h.scalar_tensor_tensor(out=cols[:, k, :], in0=cols[:, k, :], scalar=1.0,
                                                                         in1=g12[:, gi, :], op0=ALU.add, op1=ALU.mult),
                      reads=[b_cols, b_g12], writes=[b_cols])

            if DEBUG_STAGE == "p3":
                tr.barrier()
                return nc, tr.ninst
            tr.barrier()
            pm_.close()
            for k_ in range(3):
                stage.append(sbp("stage%d" % (NSTG + k_), [128, 8, 512])); b_stage.append(B("stage%d" % (NSTG + k_)))
            NSTG = 6
            gsrc = []
            for g in range(10):
                gsrc.append(win_d.ap().rearrange("(c p) n -> p c n", p=128)[:, :, g * 512:(g + 1) * 512])
            for g in range(4):
                gsrc.append(wout_d.ap().rearrange("(c p) n -> p c n", p=128)[:, :, g * 512:(g + 1) * 512])
            for g in range(4):
                gsrc.append(wpq_d.ap().rearrange("(c p) n -> p c n", p=128)[:, :, g * 512:(g + 1) * 512])
            utv = put_d.ap().rearrange("(c p) n -> p c n", p=128)
            vv = pv_d.ap().rearrange("(g c p) d -> g p c d", c=4, p=128)
            jobs = []
            for g in range(NGRP):
                for hh in range(2):
                    jobs.append((gsrc[g][:, hh * 8:(hh + 1) * 8, :], False, wsc_d[g][:, hh * 4096:(hh + 1) * 4096], b_wsc[g]))
            for g in range(32):
                for hh in range(2):
                    jobs.append((utv[:, hh * 8:(hh + 1) * 8, g * 512:(g + 1) * 512], False, us_d[g][:, hh * 4096:(hh + 1) * 4096], b_uvs))
            for g in range(32):
                for hh in range(2):
                    jobs.append((vv[g][:, hh * 2:(hh + 1) * 2, :], True, vs_d[g][:, hh * 4096:(hh + 1) * 4096], b_uvs))
            base = ntile

            def job_in(k):
                src, is_v, dst, bd = jobs[k]
                sl = (base + k) % NSTG
                o = stage[sl][:].rearrange("p c n -> p (c n)").rearrange("p (c d) -> p c d", c=2) if is_v else stage[sl][:]
                tr.dma("sp", lambda h: h.dma_start(out=o, in_=src), writes=[b_stage[sl]])

            for k in range(min(NSTG, len(jobs))):
                job_in(k)
            for k in range(len(jobs)):
                src, is_v, dst, bd = jobs[k]
                sl = (base + k) % NSTG
                cs = k % 2
                stf = stage[sl][:].rearrange("p c n -> p (c n)")
                if is_v:
                    tr.op("dve", lambda h: h.tensor_tensor(out=cvt[cs][:, 0:2048], in0=stf[:, 0:2048], in1=gt2b[:], op=ALU.mult),
                          reads=[b_stage[sl], b_gt2], writes=[b_cvt[cs]])
                    tr.op("pool", lambda h: h.tensor_tensor(out=cvt[cs][:, 2048:4096], in0=stf[:, 2048:4096], in1=gt2b[:], op=ALU.mult),
                          reads=[b_stage[sl], b_gt2], writes=[b_cvt[cs]])
                else:
                    tr.op("act", lambda h: h.activation(out=cvt[cs][:, 0:2048], in_=stf[:, 0:2048], func=AF.Copy),
                          reads=[b_stage[sl]], writes=[b_cvt[cs]])
                    tr.op("dve", lambda h: h.tensor_copy(out=cvt[cs][:, 2048:4096], in_=stf[:, 2048:4096]),
                          reads=[b_stage[sl]], writes=[b_cvt[cs]])
                if k + NSTG < len(jobs):
                    job_in(k + NSTG)
                tr.dma("sp", lambda h: h.dma_start(out=dst, in_=cvt[cs][:]), reads=[b_cvt[cs]], writes=[bd], sembuf=b_cvt[cs])
            tr.op("dve", lambda h: h.memset(kT[:], 0.0), writes=[b_kT])
            tr.op("dve", lambda h: h.memset(vtok[:], 0.0), writes=[b_vtok])
            tr.op("dve", lambda h: h.memset(ubuf[:], 0.0), writes=[b_u])
            tr.barrier()

        def load_w(g, wb, bwb):
            for hh in range(2):
                tr.dma("sp", lambda h, hh=hh: h.dma_start(out=wb[:, hh * 4096:(hh + 1) * 4096], in_=wsc_d[g][:, hh * 4096:(hh + 1) * 4096]),
                       reads=[b_wsc[g]], writes=[bwb])

        def rstd_of(ssq_ap, out_ap, scale, bufs_r, bufs_w):
            tr.op("act", lambda h: h.activation(out=out_ap, in_=ssq_ap, func=AF.Ln, scale=scale, bias=epsc[:]),
                  reads=list(bufs_r) + [b_const], writes=bufs_w)
            tr.op("act", lambda h: h.activation(out=out_ap, in_=out_ap, func=AF.Exp, scale=-0.5), reads=bufs_w, writes=bufs_w)

        def norm_transpose(xn, b_xn, hT, b_hT, kcol_shift, kcol_scale, stat_off):
            for i in range(NI):
                tr.op("act", lambda h, i=i: h.activation(out=xn[:, i, :], in_=xres[:, i, :], func=AF.Square,
                                                         accum_out=stat[:, stat_off + i:stat_off + i + 1]),
                      reads=[b_xres[i]], writes=[b_xn, b_stat])
            rstd_of(stat[:, stat_off:stat_off + NI], stat[:, stat_off + 2:stat_off + 2 + NI], 1.0 / D, [b_stat], [b_stat])
            for i in range(NI):
                tr.op("act", lambda h, i=i: h.activation(out=xn[:, i, :], in_=xres[:, i, :], func=AF.Copy,
                                                         scale=stat[:, stat_off + 2 + i:stat_off + 3 + i]),
                      reads=[b_xres[i], b_stat], writes=[b_xn])
            for c in range(16):
                pb, bpb = nextbank()
                pbb = pb[:].bitcast(BF16)
                for i in range(NI):
                    tr.op("pe", lambda h, i=i, c=c, pbb=pbb: h.transpose(out=pbb[:, i * 128:(i + 1) * 128],
                                                                         in_=xn[:, i, c * 128:(c + 1) * 128], identity=identb[:]),
                          reads=[b_xn, b_const], writes=[bpb], inc=(i == NI - 1))
                tr.op("dve", lambda h, c=c, pbb=pbb: h.tensor_scalar(out=hT[:, c, :], in0=pbb[:, 0:TT],
                                                                     scalar1=cols[:, kcol_scale, c:c + 1], scalar2=cols[:, kcol_shift, c:c + 1],
                                                                     op0=ALU.mult, op1=ALU.add),
                      reads=[bpb, b_cols], writes=[b_hT])

        for st in range(N_SUPER):
            t0 = st * TT
            with ExitStack() as pa:
                def sba(name, shape, dt=F32):
                    return pa.enter_context(nc.sbuf_tensor("%s_a%d" % (name, st), list(shape), dt))
                xn = sba("xn", [128, NI, D], BF16); b_xn = B("xn")
                hT = sba("hT", [128, 16, TT], BF16); b_hT = B("hT")
                wbuf = [sba("wbuf%d" % i, [128, 8192], BF16) for i in range(2)]
                b_wbuf = [B("wbuf%d" % i) for i in range(2)]
                cst = sba("cst", [128, 2, TT]); b_cst = B("cst")
                qT = sba("qT", [128, 8, TT], BF16); b_qT = B("qT")
                cgs = sba("cgs", [128, 8, TT]); b_cgs = B("cgs")
                mixT = sba("mixT", [128, 16, TT], BF16); b_mixT = B("mixT")
                Pt = [sba("Pt%d" % i, [128, 512], BF16) for i in range(4)]
                gt1b = sba("gt1b", [128, D])
                b_Pt = [B("Pt%d" % i) for i in range(4)]

                par_ = st % 2
                xres = xres2[:, par_ * NI:(par_ + 1) * NI, :]; b_xres = b_xres2[par_]
                if st == 0 or DEBUG_STAGE == "A":
                    for i in range(NI):
                        tr.dma("sp", lambda h, i=i: h.dma_start(out=xres[:, i, :], in_=x_d[t0 + i * 128: t0 + (i + 1) * 128, :]),
                               writes=[b_xres[i]])
                tr.dma("sp", lambda h: h.dma_start(out=cst[:, 0, :], in_=cos_d[:, t0:t0 + TT]), writes=[b_cst])
                tr.dma("sp", lambda h: h.dma_start(out=cst[:, 1, :], in_=sin_d[:, t0:t0 + TT]), writes=[b_cst])
                load_w(0, wbuf[0], b_wbuf[0])
                tr.dma("sp", lambda h: h.dma_start(out=gt1b[:], in_=gt1_d.ap()), reads=[b_gt1s], writes=[b_rows])
                load_w(1, wbuf[1], b_wbuf[1])
                norm_transpose(xn, b_xn, hT, b_hT, 0, 1, 0)

                if DEBUG_STAGE == "a1":
                    tr.barrier()
                    return nc, tr.ninst
                tmpc = {"i": 0}
                T2 = {}
                for nm, shp, dt_ in (("sq", [128, 512], BF16), ("r", [128, 512], F32), ("a", [128, 512], F32), ("b", [128, 512], F32), ("qn", [128, TT], BF16)):
                    T2[nm] = [(sba("t2%s%d" % (nm, k), shp, dt_), B("t2%s%d" % (nm, k))) for k in range(4)]

                def tmps():
                    k = tmpc["i"] % 4
                    tmpc["i"] += 1
                    return {nm: T2[nm][k] for nm in T2}

                pgc = {"i": 0}

                def proj_group(g):
                    wb, bwb = wbuf[g % 2], b_wbuf[g % 2]
                    bs = 2 * (pgc["i"] % 2)
                    pgc["i"] += 1
                    outs = []
                    for ch in range(4):
                        pbk, bpbk = pbank[bs + ch // 2], b_pb[bs + ch // 2]
                        pv = pbk[:, (ch % 2) * TT:(ch % 2 + 1) * TT]
                        for c in range(16):
                            tr.op("pe", lambda h, c=c, pv=pv, ch=ch: h.matmul(pv, lhsT=wb[:, c * 512 + ch * 128: c * 512 + (ch + 1) * 128],
                                                                             rhs=hT[:, c, :], start=(c == 0), stop=(c == 15)),
                                  reads=[bwb, b_hT], writes=[bpbk], inc=(c == 15))
                        outs.append((pv, bpbk))
                    if g + 2 < 14:
                        load_w(g + 2, wbuf[g % 2], b_wbuf[g % 2])
                    return outs

                def halfbank(k, ch):
                    pbk, bpbk = pbank[k + ch // 2], b_pb[k + ch // 2]
                    return pbk[:, (ch % 2) * TT:(ch % 2 + 1) * TT], bpbk

                def rstd_stages(srcs, Ts, n):
                    for (src_ap, b_src), T in zip(srcs, Ts):
                        (tsq, btsq) = T["sq"]
                        tr.op("act", lambda h, tsq=tsq, src_ap=src_ap: h.activation(out=tsq[:, 0:n], in_=src_ap, func=AF.Square),
                              reads=[b_src], writes=[btsq])
                    pbs = []
                    for ch, T in enumerate(Ts):
                        (tsq, btsq) = T["sq"]
                        pv2, bpb2 = halfbank(4, ch)
                        tr.op("pe", lambda h, tsq=tsq, pv2=pv2: h.matmul(pv2, lhsT=bonesb[:], rhs=tsq[:, 0:n], start=True, stop=True),
                              reads=[btsq, b_const], writes=[bpb2])
                        pbs.append((pv2, bpb2))
                    for (pv2, bpb2), T in zip(pbs, Ts):
                        (tr_, btr) = T["r"]
                        tr.op("act", lambda h, tr_=tr_, pv2=pv2: h.activation(out=tr_[:, 0:n], in_=pv2, func=AF.Ln, bias=epsc[:]),
                              reads=[bpb2, b_const], writes=[btr])
                    for T in Ts:
                        (tr_, btr) = T["r"]
                        tr.op("act", lambda h, tr_=tr_: h.activation(out=tr_[:, 0:n], in_=tr_[:, 0:n], func=AF.Exp, scale=-0.5), reads=[btr], writes=[btr])

                def post_qk(g, outs):
                    Ts = [tmps() for _ in range(4)]
                    rstd_stages(outs, Ts, TT)
                    gcol = gqk[:, 0:1] if g < 2 else gqk[:, 1:2]
                    for (pv, bpb), T in zip(outs, Ts):
                        (tr_, btr), (tqn, btqn) = T["r"], T["qn"]
                        tr.op("dve", lambda h, pv=pv, tr_=tr_, tqn=tqn: h.scalar_tensor_tensor(out=tqn[:], in0=pv, scalar=gcol, in1=tr_[:, 0:TT],
                                                                                             op0=ALU.mult, op1=ALU.mult),
                              reads=[bpb, btr, b_par], writes=[btqn])
                    rps = []
                    for ch, T in enumerate(Ts):
                        (tqn, btqn) = T["qn"]
                        pv3, bpb3 = halfbank(6, ch)
                        tr.op("pe", lambda h, tqn=tqn, pv3=pv3: h.matmul(pv3, lhsT=ropepb[:], rhs=tqn[:], start=True, stop=True),
                              reads=[btqn, b_const], writes=[bpb3])
                        rps.append((pv3, bpb3))
                    for T in Ts:
                        (ta, bta), (tqn, btqn) = T["a"], T["qn"]
                        tr.op("pool", lambda h, ta=ta, tqn=tqn: h.tensor_tensor(out=ta[:, 0:TT], in0=tqn[:], in1=cst[:, 0, :], op=ALU.mult),
                              reads=[btqn, b_cst], writes=[bta])
                    for (pv3, bpb3), T in zip(rps, Ts):
                        (tb, btb) = T["b"]
                        tr.op("dve", lambda h, tb=tb, pv3=pv3: h.tensor_tensor(out=tb[:, 0:TT], in0=pv3, in1=cst[:, 1, :], op=ALU.mult),
                              reads=[bpb3, b_cst], writes=[btb])
                    for ch, T in enumerate(Ts):
                        (ta, bta), (tb, btb) = T["a"], T["b"]
                        if g < 2:
                            tr.op("dve", lambda h, ch=ch, ta=ta, tb=tb: h.tensor_tensor(out=qT[:, g * 4 + ch, :], in0=ta[:, 0:TT], in1=tb[:, 0:TT], op=ALU.add),
                                  reads=[bta, btb], writes=[b_qT])
                        else:
                            for half in range(2):
                                ps_ = slice(half * 64, (half + 1) * 64)
                                tr.op("dve", lambda h, ch=ch, half=half, ps_=ps_, ta=ta, tb=tb: h.tensor_tensor(
                                    out=kT[ps_, ch, half, 128:128 + TT], in0=ta[ps_, 0:TT], in1=tb[ps_, 0:TT], op=ALU.add),
                                    reads=[bta, btb], writes=[b_kT])

                def post_conv(g, outs):
                    if g in (4, 5):
                        for ch in range(4):
                            pv, bpb = outs[ch]
                            cc = (g - 4) * 4 + ch
                            tr.op("act", lambda h, cc=cc, pv=pv: h.activation(out=cgs[:, cc, :], in_=pv, func=AF.Copy), reads=[bpb], writes=[b_cgs])
                    elif g in (6, 7):
                        for ch in range(4):
                            pv, bpb = outs[ch]
                            cc = (g - 6) * 4 + ch
                            tr.op("dve", lambda h, cc=cc, pv=pv: h.tensor_tensor(out=ubuf[:, cc, 2:2 + TT], in0=pv, in1=cgs[:, cc, :], op=ALU.mult),
                                  reads=[bpb, b_cgs], writes=[b_u])
                            tr.op("dve", lambda h, cc=cc: h.tensor_scalar(out=cgs[:, cc, :], in0=ubuf[:, cc, 0:TT], scalar1=convw[:, cc * 3:cc * 3 + 1],
                                                                           scalar2=None, op0=ALU.mult), reads=[b_u, b_par], writes=[b_cgs])
                            for tap in (1, 2):
                                tr.op("dve", lambda h, cc=cc, tap=tap: h.scalar_tensor_tensor(
                                    out=cgs[:, cc, :], in0=ubuf[:, cc, tap:tap + TT], scalar=convw[:, cc * 3 + tap:cc * 3 + tap + 1],
                                    in1=cgs[:, cc, :], op0=ALU.mult, op1=ALU.add), reads=[b_u, b_par, b_cgs], writes=[b_cgs])
                            tr.op("pool", lambda h, cc=cc: h.tensor_copy(out=ubuf[:, cc, 0:2], in_=ubuf[:, cc, TT:TT + 2]), reads=[b_u], writes=[b_u])
                    else:
                        Ts = [tmps() for _ in range(4)]
                        for ch, T in enumerate(Ts):
                            pv, bpb = outs[ch]
                            cc = (g - 8) * 4 + ch
                            (ta, bta) = T["a"]
                            tr.op("dve", lambda h, cc=cc, pv=pv, ta=ta: h.tensor_tensor(out=ta[:, 0:TT], in0=pv, in1=cgs[:, cc, :], op=ALU.mult),
                                  reads=[bpb, b_cgs], writes=[bta])
                        rstd_stages([(T["a"][0][:, 0:TT], T["a"][1]) for T in Ts], Ts, TT)
                        for ch, T in enumerate(Ts):
                            cc = (g - 8) * 4 + ch
                            (tr_, btr), (ta, bta) = T["r"], T["a"]
                            tr.op("dve", lambda h, cc=cc, ta=ta, tr_=tr_: h.scalar_tensor_tensor(out=mixT[:, 8 + cc, :], in0=ta[:, 0:TT], scalar=gconv[:, cc:cc + 1],
                                                                                                in1=tr_[:, 0:TT], op0=ALU.mult, op1=ALU.mult),
                                  reads=[bta, btr, b_par], writes=[b_mixT])

                def v_proj():
                    wb, bwb = wbuf[3 % 2], b_wbuf[3 % 2]
                    for i in range(NI):
                        pb, bpb = nextbank(4, 8)
                        for c in range(16):
                            tr.op("pe", lambda h, c=c, i=i, pb=pb: h.matmul(pb[:, :], lhsT=hT[:, c, i * 128:(i + 1) * 128],
                                                                            rhs=wb[:, c * 512:(c + 1) * 512], start=(c == 0), stop=(c == 15)),
                                  reads=[bwb, b_hT], writes=[bpb], inc=(c == 15))
                        tr.op("act", lambda h, i=i, pb=pb: h.activation(out=vtok[:, 1 + i, :], in_=pb[:, :], func=AF.Copy),
                              reads=[bpb], writes=[b_vtok])
                    load_w(5, wbuf[1], b_wbuf[1])

                def attn_pair(i, hks):
                    n = st * NI + i
                    kbs = ([] if n == 0 else [("prev", i * 128, i, mprevb)]) + [("cur", (i + 1) * 128, i + 1, mcurb)]
                    nk = len(kbs)
                    Ts = [tmps() for _ in hks]
                    sc = {}
                    for a_, hk in enumerate(hks):
                        for kbi, (nm, kc0, vblk, msk) in enumerate(kbs):
                            pb, bpb = pbank[a_ * 2 + kbi], b_pb[a_ * 2 + kbi]
                            for j in range(4):
                                half = j % 2
                                qc = 2 * hk + j // 2
                                tr.op("pe", lambda h, pb=pb, j=j, half=half, qc=qc, kc0=kc0, hk=hk: h.matmul(
                                    pb[:, j * 128:(j + 1) * 128], lhsT=kT[:, hk, half, kc0:kc0 + 128],
                                    rhs=qT[:, qc, i * 128:(i + 1) * 128], start=True, stop=True),
                                    reads=[b_kT, b_qT], writes=[bpb], inc=(j == 3))
                            sc[(a_, kbi)] = (pb, bpb)
                    for a_, hk in enumerate(hks):
                        for kbi in range(nk):
                            pb, bpb = sc[(a_, kbi)]
                            P_, bP = Pt[a_ * 2 + kbi], b_Pt[a_ * 2 + kbi]
                            tr.op("act", lambda h, pb=pb, P_=P_: h.activation(out=P_[:], in_=pb[:, :], func=AF.Exp, scale=0.125),
                                  reads=[bpb], writes=[bP])
                    for a_, hk in enumerate(hks):
                        for kbi, (nm, kc0, vblk, msk) in enumerate(kbs):
                            P_, bP = Pt[a_ * 2 + kbi], b_Pt[a_ * 2 + kbi]
                            tr.op("pool", lambda h, P_=P_, msk=msk: h.tensor_tensor(
                                out=P_[:].rearrange("p (j q) -> p j q", j=4), in0=P_[:].rearrange("p (j q) -> p j q", j=4),
                                in1=msk[:].unsqueeze(1).broadcast_to([128, 4, 128]), op=ALU.mult),
                                reads=[bP, b_const], writes=[bP])
                    pos, pds = [], []
                    for a_, hk in enumerate(hks):
                        po, bpo = pbank[4 + a_], b_pb[4 + a_]
                        pd, bpd = pbank[6 + a_], b_pb[6 + a_]
                        for kbi, (nm, kc0, vblk, msk) in enumerate(kbs):
                            P_, bP = Pt[a_ * 2 + kbi], b_Pt[a_ * 2 + kbi]
                            tr.op("pe", lambda h, P_=P_, vblk=vblk, kbi=kbi, po=po, hk=hk: h.matmul(
                                po[:, :], lhsT=vtok[:, vblk, hk * 128:(hk + 1) * 128], rhs=P_[:],
                                start=(kbi == 0), stop=(kbi == nk - 1)), reads=[bP, b_vtok], writes=[bpo], inc=(kbi == nk - 1))
                        for kbi in range(nk):
                            P_, bP = Pt[a_ * 2 + kbi], b_Pt[a_ * 2 + kbi]
                            tr.op("pe", lambda h, P_=P_, kbi=kbi, pd=pd: h.matmul(pd[:, :], lhsT=onesb[:], rhs=P_[:],
                                                                                 start=(kbi == 0), stop=(kbi == nk - 1)),
                                  reads=[bP, b_const], writes=[bpd], inc=(kbi == nk - 1))
                        pos.append((po, bpo))
                        pds.append((pd, bpd))
                    for a_, T in enumerate(Ts):
                        (tsq, btsq) = T["sq"]
                        po, bpo = pos[a_]
                        tr.op("act", lambda h, tsq=tsq, po=po: h.activation(out=tsq[:], in_=po[:, :], func=AF.Square), reads=[bpo], writes=[btsq])
                    pms = []
                    for a_, T in enumerate(Ts):
                        (tsq, btsq) = T["sq"]
                        pm, bpm = pbank[a_ * 2], b_pb[a_ * 2]
                        tr.op("pe", lambda h, tsq=tsq, pm=pm: h.matmul(pm[:, :], lhsT=bonesb[:], rhs=tsq[:], start=True, stop=True),
                              reads=[btsq, b_const], writes=[bpm])
                        pms.append((pm, bpm))
                    for a_, hk in enumerate(hks):
                        (ta, bta) = Ts[a_]["a"]
                        pd, bpd = pds[a_]
                        for j in range(4):
                            tr.op("act", lambda h, j=j, ta=ta, pd=pd, hk=hk: h.activation(out=ta[:, j * 128:(j + 1) * 128], in_=pd[:, j * 128:(j + 1) * 128],
                                                                                         func=AF.Square, scale=1e-3, bias=esink[:, hk * 4 + j:hk * 4 + j + 1]),
                                  reads=[bpd, b_par], writes=[bta])
                    for a_, T in enumerate(Ts):
                        (tr_, btr), (ta, bta) = T["r"], T["a"]
                        pm, bpm = pms[a_]
                        tr.op("dve", lambda h, tr_=tr_, pm=pm, ta=ta: h.tensor_tensor(out=tr_[:], in0=pm[:, :], in1=ta[:], op=ALU.add),
                              reads=[bpm, bta], writes=[btr])
                    for T in Ts:
                        (tr_, btr) = T["r"]
                        tr.op("act", lambda h, tr_=tr_: h.activation(out=tr_[:], in_=tr_[:], func=AF.Ln), reads=[btr], writes=[btr])
                    for T in Ts:
                        (tr_, btr) = T["r"]
                        tr.op("act", lambda h, tr_=tr_: h.activation(out=tr_[:], in_=tr_[:], func=AF.Exp, scale=-0.5), reads=[btr], writes=[btr])
                    for a_, hk in enumerate(hks):
                        (tr_, btr) = Ts[a_]["r"]
                        po, bpo = pos[a_]
                        for j in range(4):
                            half = j % 2
                            mc = 2 * hk + j // 2
                            ps_ = slice(half * 64, (half + 1) * 64)
                            tr.op("dve", lambda h, j=j, mc=mc, ps_=ps_, po=po, tr_=tr_: h.scalar_tensor_tensor(
                                out=mixT[ps_, mc, i * 128:(i + 1) * 128], in0=po[ps_, j * 128:(j + 1) * 128],
                                scalar=gattn[ps_, mc:mc + 1], in1=tr_[ps_, j * 128:(j + 1) * 128], op0=ALU.mult, op1=ALU.mult),
                                reads=[bpo, btr, b_par], writes=[b_mixT])

                o0 = proj_group(0)
                o1 = proj_group(1)
                post_qk(0, o0)
                o2 = proj_group(2)
                post_qk(1, o1)
                v_proj()
                post_qk(2, o2)
                for i in range(NI):
                    for hp in range(2):
                        attn_pair(i, (2 * hp, 2 * hp + 1))
                tr.op("pool", lambda h: h.tensor_copy(out=kT[:, :, :, 0:128], in_=kT[:, :, :, TT:TT + 128]), reads=[b_kT], writes=[b_kT])
                tr.op("pool", lambda h: h.tensor_copy(out=vtok[:, 0, :], in_=vtok[:, NI, :]), reads=[b_vtok], writes=[b_vtok])
                pend = None
                for g in range(4, 10):
                    o = proj_group(g)
                    if pend is not None:
                        post_conv(*pend)
                    pend = (g, o)
                post_conv(*pend)
                for g in range(10, 14):
                    wb, bwb = wbuf[g % 2], b_wbuf[g % 2]
                    dg = g - 10
                    for i in range(NI):
                        pb, bpb = nextbank()
                        T = tmps()
                        ta, bta = T["a"]
                        for mc in range(16):
                            tr.op("pe", lambda h, mc=mc, i=i, pb=pb: h.matmul(pb[:, :], lhsT=mixT[:, mc, i * 128:(i + 1) * 128],
                                                                             rhs=wb[:, mc * 512:(mc + 1) * 512], start=(mc == 0), stop=(mc == 15)),
                                  reads=[bwb, b_mixT], writes=[bpb], inc=(mc == 15))
                        tr.op("dve", lambda h, pb=pb, dg=dg, ta=ta: h.tensor_tensor(out=ta[:], in0=pb[:, :], in1=gt1b[:, dg * 512:(dg + 1) * 512], op=ALU.mult),
                              reads=[bpb, b_rows], writes=[bta])
                        tr.op("pool", lambda h, i=i, dg=dg, ta=ta: h.tensor_tensor(out=xres[:, i, dg * 512:(dg + 1) * 512], in0=xres[:, i, dg * 512:(dg + 1) * 512],
                                                                                in1=ta[:], op=ALU.add), reads=[bta, b_xres[i]], writes=[b_xres[i]])
                    if g + 2 < 14:
                        load_w(g + 2, wbuf[g % 2], b_wbuf[g % 2])
                tr.barrier()

            if DEBUG_STAGE == "A":
                for i in range(NI):
                    tr.dma("sp", lambda h, i=i: h.dma_start(out=out_d[t0 + i * 128: t0 + (i + 1) * 128, :], in_=xres[:, i, :]),
                           reads=[b_xres[i]], writes=[b_out], sembuf=b_xres[i])
                tr.barrier()
                continue

            with ExitStack() as pp:
                def sbq(name, shape, dt=F32):
                    return pp.enter_context(nc.sbuf_tensor("%s_p%d" % (name, st), list(shape), dt))
                hT = sbq("h2T", [128, 16, TT], BF16); b_hT = B("h2T")
                GT = sbq("GT", [128, TT, 128], BF16); b_GT = B("GT")
                mid_ = ExitStack()
                pqT = mid_.enter_context(nc.sbuf_tensor("pqT_p%d" % st, [128, 16, TT], BF16)); b_pqT = B("pqT")
                with ExitStack() as pq_:
                    def sbq1(name, shape, dt=F32):
                        return pq_.enter_context(nc.sbuf_tensor("%s_q%d" % (name, st), list(shape), dt))
                    xn = sbq1("xn2", [128, NI, D], BF16); b_xn = B("xn2")
                    wbuf = [sbq1("wbq%d" % i, [128, 8192], BF16) for i in range(2)]
                    b_wbuf = [B("wbq%d" % i) for i in range(2)]
                    load_w(14, wbuf[0], b_wbuf[0])
                    load_w(15, wbuf[1], b_wbuf[1])
                    norm_transpose(xn, b_xn, hT, b_hT, 2, 3, 4)
                    for g in range(4):
                        wb, bwb = wbuf[g % 2], b_wbuf[g % 2]
                        for ch in range(4):
                            pb, bpb = nextbank()
                            for c in range(16):
                                tr.op("pe", lambda h, c=c, pb=pb, ch=ch: h.matmul(pb[:, 0:TT], lhsT=wb[:, c * 512 + ch * 128: c * 512 + (ch + 1) * 128],
                                                                                 rhs=hT[:, c, :], start=(c == 0), stop=(c == 15)),
                                      reads=[bwb, b_hT], writes=[bpb], inc=(c == 15))
                            tr.op("act", lambda h, pb=pb, g=g, ch=ch: h.activation(out=pqT[:, g * 4 + ch, :], in_=pb[:, 0:TT], func=AF.Copy),
                                  reads=[bpb], writes=[b_pqT])
                        if g + 2 < 4:
                            load_w(14 + g + 2, wbuf[g % 2], b_wbuf[g % 2])
                    tr.barrier()
                with ExitStack() as p2_:
                    def sbq2(name, shape, dt=F32):
                        return p2_.enter_context(nc.sbuf_tensor("%s_r%d" % (name, st), list(shape), dt))
                    Ssb = sbq2("Ssb", [128, 16, 128]); b_S = B("Ssb")
                    S2s = [sbq2("S2_%d" % k, [128, 128]) for k in range(4)]; b_S2s = [B("S2_%d" % k) for k in range(4)]
                    T16 = sbq2("T16", [128, 16, 16]); b_T16s = [B("T16_%d" % k) for k in range(16)]
                    I16 = sbq2("I16", [128, 16, 16], U32); b_I16s = [B("I16_%d" % k) for k in range(16)]
                    I16f = sbq2("I16f", [128, 16, 16]); b_I16f = B("I16f")
                    cand = sbq2("cand", [128, 8, 256]); b_cand = B("cand")
                    cand2s = [sbq2("cand2_%d" % k, [128, 256]) for k in range(4)]; b_cand2s = [B("cand2_%d" % k) for k in range(4)]
                    C16 = sbq2("C16", [128, 8, 16]); b_C16s = [B("C16_%d" % k) for k in range(8)]
                    CI = sbq2("CI", [128, 8, 16], U32); b_CIs = [B("CI_%d" % k) for k in range(8)]
                    rc = sbq2("rc", [128, 2, 128], U32); b_rc = B("rc")
                    rcf = sbq2("rcf", [128, 2, 128]); b_rcf = B("rcf")
                    oh_flat = cand[:].rearrange("p a b -> p (a b)"); b_oh = b_cand
                    E12 = sbq2("E12", [128, 3, 128]); b_E12 = B("E12")
                    zz = sbq2("zz", [128, 16]); b_zz = B("zz")
                    ejT = sbq2("ejT", [128, 3, 128]); b_ejT = B("ejT")
                    bXt = [[B("Xb%d_%d" % (k, tl)) for tl in range(32)] for k in range(2)]
                    bYt = [[B("Yb%d_%d" % (k, tl)) for tl in range(32)] for k in range(2)]
                    Xb = [sbq2("Xb%d" % k, [128, 32, 128], BF16) for k in range(2)]; b_Xb = [B("Xb%d" % k) for k in range(2)]
                    Yb = [sbq2("Yb%d" % k, [128, 32, 128], BF16) for k in range(2)]; b_Yb = [B("Yb%d" % k) for k in range(2)]
                    gate3 = E12[:, 2, :].rearrange("p (a b) -> p a b", a=8)
                    xyc = {"i": 0}

                    def topk_gen(i):
                        for q4 in range(4):
                            pb, bpb = nextbank()
                            for k4 in range(4):
                                hh = q4 * 4 + k4
                                tr.op("pe", lambda h, pb=pb, k4=k4, hh=hh: h.matmul(pb[:, k4 * 128:(k4 + 1) * 128], lhsT=pqT[:, hh, i * 128:(i + 1) * 128],
                                                                                    rhs=subkb[:, hh, :], start=True, stop=True),
                                      reads=[b_pqT, b_par], writes=[bpb], inc=(k4 == 3))
                            tr.op("act", lambda h, pb=pb, q4=q4: h.activation(out=Ssb[:, q4 * 4:(q4 + 1) * 4, :].rearrange("p a b -> p (a b)"),
                                                                              in_=pb[:, :], func=AF.Copy), reads=[bpb], writes=[b_S])
                            yield
                        for h4 in range(4):
                            hhs = [h4 * 4 + k for k in range(4)]
                            for k, hh in enumerate(hhs):
                                tr.op("dve", lambda h, hh=hh: h.max(out=T16[:, hh, 0:8], in_=Ssb[:, hh, :]), reads=[b_S], writes=[b_T16s[hh]])
                            for k, hh in enumerate(hhs):
                                tr.op("dve", lambda h, hh=hh, k=k: h.match_replace(out=S2s[k][:], in_to_replace=T16[:, hh, 0:8], in_values=Ssb[:, hh, :], imm_value=-1e30),
                                      reads=[b_S, b_T16s[hh]], writes=[b_S2s[k]])
                            for k, hh in enumerate(hhs):
                                tr.op("dve", lambda h, hh=hh, k=k: h.max(out=T16[:, hh, 8:16], in_=S2s[k][:]), reads=[b_S2s[k]], writes=[b_T16s[hh]])
                            for k, hh in enumerate(hhs):
                                tr.op("dve", lambda h, hh=hh: h.max_index(out=I16[:, hh, 0:8], in_max=T16[:, hh, 0:8], in_values=Ssb[:, hh, :]),
                                      reads=[b_S, b_T16s[hh]], writes=[b_I16s[hh]])
                            for k, hh in enumerate(hhs):
                                tr.op("dve", lambda h, hh=hh, k=k: h.max_index(out=I16[:, hh, 8:16], in_max=T16[:, hh, 8:16], in_values=S2s[k][:]),
                                      reads=[b_S2s[k], b_T16s[hh]], writes=[b_I16s[hh]])
                            yield
                        tr.op("dve", lambda h: h.tensor_copy(out=I16f[:], in_=I16[:]), reads=b_I16s, writes=[b_I16f])
                        t16 = T16[:]
                        in0 = _ap(t16, 0, [[32, 8], [1, 16], [0, 16]])
                        in1 = _ap(t16, 16, [[32, 8], [0, 16], [1, 16]])
                        tr.op("dve", lambda h: h.tensor_tensor(out=cand[:].rearrange("p a (b c) -> p a b c", b=16), in0=in0, in1=in1, op=ALU.add),
                              reads=b_T16s, writes=[b_cand])
                        for h4 in range(2):
                            hds = [h4 * 4 + k for k in range(4)]
                            for k, hd in enumerate(hds):
                                tr.op("dve", lambda h, hd=hd: h.max(out=C16[:, hd, 0:8], in_=cand[:, hd, :]), reads=[b_cand], writes=[b_C16s[hd]])
                            for k, hd in enumerate(hds):
                                tr.op("dve", lambda h, hd=hd, k=k: h.match_replace(out=cand2s[k][:], in_to_replace=C16[:, hd, 0:8], in_values=cand[:, hd, :], imm_value=-1e30),
                                      reads=[b_cand, b_C16s[hd]], writes=[b_cand2s[k]])
                            for k, hd in enumerate(hds):
                                tr.op("dve", lambda h, hd=hd, k=k: h.max(out=C16[:, hd, 8:16], in_=cand2s[k][:]), reads=[b_cand2s[k]], writes=[b_C16s[hd]])
                            for k, hd in enumerate(hds):
                                tr.op("dve", lambda h, hd=hd: h.max_index(out=CI[:, hd, 0:8], in_max=C16[:, hd, 0:8], in_values=cand[:, hd, :]),
                                      reads=[b_cand, b_C16s[hd]], writes=[b_CIs[hd]])
                            for k, hd in enumerate(hds):
                                tr.op("dve", lambda h, hd=hd, k=k: h.max_index(out=CI[:, hd, 8:16], in_max=C16[:, hd, 8:16], in_values=cand2s[k][:]),
                                      reads=[b_cand2s[k], b_C16s[hd]], writes=[b_CIs[hd]])
                            yield
                        c16 = C16[:]
                        tr.op("dve", lambda h: h.tensor_tensor(out=gate3, in0=c16, in1=_ap(c16, 0, [[16, 8], [0, 16]]), op=ALU.subtract),
                              reads=b_C16s, writes=[b_E12])
                        tr.op("act", lambda h: h.activation(out=E12[:, 2, :], in_=E12[:, 2, :], func=AF.Exp), reads=[b_E12], writes=[b_E12])
                        tr.op("dve", lambda h: h.tensor_reduce(out=zz[:, 0:8], in_=gate3, axis=AX.X, op=ALU.add), reads=[b_E12], writes=[b_zz])
                        tr.op("dve", lambda h: h.reciprocal(out=zz[:, 8:16], in_=zz[:, 0:8]), reads=[b_zz], writes=[b_zz])
                        tr.op("dve", lambda h: h.tensor_tensor(out=gate3, in0=gate3, in1=_ap(zz[:], 8, [[1, 8], [0, 16]]), op=ALU.mult),
                              reads=[b_E12, b_zz], writes=[b_E12])
                        cif = CI[:].rearrange("p a b -> p (a b)")
                        tr.op("dve", lambda h: h.tensor_single_scalar(out=rc[:, 0, :], in_=cif, scalar=4, op=ALU.logical_shift_right),
                              reads=b_CIs, writes=[b_rc])
                        tr.op("dve", lambda h: h.tensor_single_scalar(out=rc[:, 1, :], in_=cif, scalar=15, op=ALU.bitwise_and),
                              reads=b_CIs, writes=[b_rc])
                        tr.op("dve", lambda h: h.tensor_copy(out=rcf[:], in_=rc[:]), reads=[b_rc], writes=[b_rcf])
                        i16f = I16f[:]
                        for w in range(2):
                            tr.op("dve", lambda h, w=w: h.tensor_tensor(out=oh_flat.rearrange("p (a b) -> p a b", b=16),
                                                                        in0=_ap(rcf[:], w * 128, [[1, 128], [0, 16]]),
                                                                        in1=_ap(iota16[:], 0, [[0, 128], [1, 16]]), op=ALU.is_equal),
                                  reads=[b_rcf, b_const], writes=[b_oh])
                            tr.op("dve", lambda h, w=w: h.tensor_tensor(out=oh_flat.rearrange("p (a k b) -> p a k b", a=8, k=16),
                                                                        in0=oh_flat.rearrange("p (a k b) -> p a k b", a=8, k=16),
                                                                        in1=_ap(i16f, w * 16, [[32, 8], [0, 16], [1, 16]]), op=ALU.mult),
                                  reads=[b_oh, b_I16f], writes=[b_oh])
                            tr.op("dve", lambda h, w=w: h.tensor_reduce(out=E12[:, w, :], in_=oh_flat.rearrange("p (a b) -> p a b", b=16), axis=AX.X, op=ALU.add),
                                  reads=[b_oh], writes=[b_E12])
                        yield

                    def ggen_gen(i):
                        pb, bpb = nextbank()
                        for k in range(3):
                            tr.op("pe", lambda h, pb=pb, k=k: h.transpose(out=pb[:, k * 128:(k + 1) * 128], in_=E12[:, k, :], identity=identf[:]),
                                  reads=[b_E12, b_const], writes=[bpb], inc=(k == 2))
                        tr.op("act", lambda h, pb=pb: h.activation(out=ejT[:].rearrange("p a b -> p (a b)"), in_=pb[:, 0:384], func=AF.Copy),
                              reads=[bpb], writes=[b_ejT])
                        yield
                        iob = _ap(iotab[:], 0, [[0, 32], [1, 128]])
                        for tg in range(4):
                            k_ = xyc["i"] % 2
                            xyc["i"] += 1
                            X_, bX, Y_, bY = Xb[k_], b_Xb[k_], Yb[k_], b_Yb[k_]
                            for tl in range(32):
                                tk = tg * 32 + tl
                                tr.op("dve", lambda h, X_=X_, tl=tl, tk=tk: h.tensor_scalar(out=X_[:, tl, :], in0=iotab[:], scalar1=ejT[:, 0, tk:tk + 1],
                                                                                            scalar2=ejT[:, 2, tk:tk + 1], op0=ALU.is_equal, op1=ALU.mult),
                                      reads=[b_const, b_ejT], writes=[bXt[k_][tl]])
                                tr.op("dve", lambda h, Y_=Y_, tl=tl, tk=tk: h.tensor_scalar(out=Y_[:, tl, :], in0=iotab[:], scalar1=ejT[:, 1, tk:tk + 1],
                                                                                            scalar2=None, op0=ALU.is_equal),
                                      reads=[b_const, b_ejT], writes=[bYt[k_][tl]])
                            for t4 in range(8):
                                pg, bpg = nextbank()
                                for tt in range(4):
                                    tl = t4 * 4 + tt
                                    tr.op("pe", lambda h, pg=pg, tt=tt, tl=tl, X_=X_, Y_=Y_: h.matmul(pg[:, tt * 128:(tt + 1) * 128], lhsT=Y_[:, tl, :], rhs=X_[:, tl, :],
                                                                                                    start=True, stop=True),
                                          reads=[bXt[k_][tl], bYt[k_][tl]], writes=[bpg], inc=(tt == 3))
                                tok0 = i * 128 + tg * 32 + t4 * 4
                                tr.op("act", lambda h, pg=pg, tok0=tok0: h.activation(out=GT[:, tok0:tok0 + 4, :].rearrange("p t e -> p (t e)"),
                                                                                      in_=pg[:, :], func=AF.Copy),
                                      reads=[bpg], writes=[b_GT])
                            yield
                        yield

                    def drain(*gw):
                        gw = [list(x) for x in gw]
                        while gw:
                            for item in list(gw):
                                for _ in range(item[1]):
                                    try:
                                        next(item[0])
                                    except StopIteration:
                                        gw.remove(item)
                                        break

                    drain((topk_gen(0), 1))
                    drain((ggen_gen(0), 1), (topk_gen(1), 3))
                    drain((ggen_gen(1), 1))
                    tr.barrier()
                mid_.close()
                with ExitStack() as p3_:
                    def sbq3(name, shape, dt=F32):
                        return p3_.enter_context(nc.sbuf_tensor("%s_s%d" % (name, st), list(shape), dt))
                    UTg = [sbq3("UTg%d" % k, [128, 8192], BF16) for k in range(2)]; b_UTg = [B("UTg%d" % k) for k in range(2)]
                    Vgr = [sbq3("Vgr%d" % k, [128, 8192], BF16) for k in range(2)]; b_Vgr = [B("Vgr%d" % k) for k in range(2)]
                    gl = [sbq3("gl%d" % k, [128, TT], BF16) for k in range(2)]; b_gl = [B("gl%d" % k) for k in range(2)]
                    GA = [sbq3("GA%d" % k, [128, 4, TT], BF16) for k in range(2)]; b_GA = [B("GA%d" % k) for k in range(2)]
                    ytmp = [sbq3("ytmp%d" % k, [128, 512]) for k in range(2)]; b_ytmp = [B("ytmp%d" % k) for k in range(2)]

                    def load_u(g):
                        k_ = g % 2
                        for hh in range(2):
                            tr.dma("sp", lambda h, hh=hh: h.dma_start(out=UTg[k_][:, hh * 4096:(hh + 1) * 4096], in_=us_d[g][:, hh * 4096:(hh + 1) * 4096]),
                                   reads=[b_uvs], writes=[b_UTg[k_]])

                    def load_v(g):
                        k_ = g % 2
                        for hh in range(2):
                            tr.dma("sp", lambda h, hh=hh: h.dma_start(out=Vgr[k_][:, hh * 4096:(hh + 1) * 4096], in_=vs_d[g][:, hh * 4096:(hh + 1) * 4096]),
                                   reads=[b_uvs], writes=[b_Vgr[k_]])

                    glc = {"i": 0}
                    ycnt = {"i": 0}

                    def a_stage(g):
                        k_ = g % 2
                        ut, but, ga_, bga = UTg[k_], b_UTg[k_], GA[k_], b_GA[k_]
                        for c4 in range(4):
                            c = g * 4 + c4
                            pa, bpa = nextbank(0, 4)
                            for dc in range(16):
                                tr.op("pe", lambda h, dc=dc, pa=pa, c4=c4: h.matmul(pa[:, 0:TT], lhsT=ut[:, dc * 512 + c4 * 128: dc * 512 + (c4 + 1) * 128],
                                                                                   rhs=hT[:, dc, :], start=(dc == 0), stop=(dc == 15)),
                                      reads=[but, b_hT], writes=[bpa], inc=(dc == 15))
                            gl_, bgl = gl[glc["i"] % 2], b_gl[glc["i"] % 2]
                            glc["i"] += 1
                            tr.op("act", lambda h, pa=pa, gl_=gl_: h.activation(out=gl_[:], in_=pa[:, 0:TT], func=AF.Gelu), reads=[bpa], writes=[bgl])
                            tr.op("dve", lambda h, gl_=gl_, c4=c4, c=c: h.tensor_tensor(out=ga_[:, c4, :], in0=gl_[:], in1=GT[:, :, c], op=ALU.mult),
                                  reads=[bgl, b_GT], writes=[bga])
                        if g + 2 < 32:
                            load_u(g + 2)

                    def y_stage(g):
                        k_ = g % 2
                        vg, bvg, ga_, bga = Vgr[k_], b_Vgr[k_], GA[k_], b_GA[k_]
                        for i in range(NI):
                            for dt_ in range(4):
                                py, bpy = nextbank(4, 8)
                                for c4 in range(4):
                                    tr.op("pe", lambda h, py=py, c4=c4, i=i, dt_=dt_: h.matmul(py[:, :], lhsT=ga_[:, c4, i * 128:(i + 1) * 128],
                                                                                              rhs=vg[:, c4 * 2048 + dt_ * 512: c4 * 2048 + (dt_ + 1) * 512],
                                                                                              start=(c4 == 0), stop=(c4 == 3)),
                                          reads=[bga, bvg], writes=[bpy], inc=(c4 == 3))
                                xs = xres[:, i, dt_ * 512:(dt_ + 1) * 512]
                                yc = ycnt["i"]
                                ycnt["i"] += 1
                                if yc % 2 == 0:
                                    tr.op("dve", lambda h, py=py, xs=xs: h.tensor_tensor(out=xs, in0=py[:, :], in1=xs, op=ALU.add),
                                          reads=[bpy, b_xres[i]], writes=[b_xres[i]])
                                else:
                                    yt, byt = ytmp[(yc // 2) % 2], b_ytmp[(yc // 2) % 2]
                                    tr.op("act", lambda h, py=py, yt=yt: h.activation(out=yt[:], in_=py[:, :], func=AF.Copy), reads=[bpy], writes=[byt])
                                    tr.op("pool", lambda h, yt=yt, xs=xs: h.tensor_tensor(out=xs, in0=yt[:], in1=xs, op=ALU.add),
                                          reads=[byt, b_xres[i]], writes=[b_xres[i]])
                        if g + 2 < 32:
                            load_v(g + 2)

                    load_u(0)
                    load_v(0)
                    load_u(1)
                    load_v(1)
                    if st + 1 < N_SUPER:
                        for i in range(NI):
                            tr.dma("sp", lambda h, i=i: h.dma_start(out=xres2[:, (1 - par_) * NI + i, :],
                                                                    in_=x_d[t0 + TT + i * 128: t0 + TT + (i + 1) * 128, :]),
                                   writes=[b_xres2[1 - par_][i]])
                    a_stage(0)
                    for g in range(32):
                        if g + 1 < 32:
                            a_stage(g + 1)
                        y_stage(g)
                    for i in range(NI):
                        tr.dma("sp", lambda h, i=i: h.dma_start(out=out_d[t0 + i * 128: t0 + (i + 1) * 128, :], in_=xres[:, i, :]),
                               reads=[b_xres[i]], writes=[b_out], sembuf=b_xres[i])
                    tr.barrier()
        tr.wait_all("sp", [b_out])
    return nc, tr.ninst


def _consts():
    identf = np.eye(128, dtype=np.float32)
    blk = np.arange(128) // 64
    bones = (blk[:, None] == blk[None, :]).astype(np.float32) / 64.0
    P = np.zeros((128, 128), np.float32)
    for hb in (0, 64):
        for i in range(8):
            P[hb + i, hb + i + 8] = -1.0
            P[hb + i + 8, hb + i] = 1.0
    ropepT = np.ascontiguousarray(P.T)
    kk = np.arange(128)[:, None]
    qq = np.arange(128)[None, :]
    mprev = (kk > qq).astype(np.float32)
    mcur = (kk <= qq).astype(np.float32)
    pos = np.arange(S, dtype=np.float32)
    inv_freq = (np.float32(500000.0) ** (-np.arange(0, 16, 2, dtype=np.float32) / np.float32(16))).astype(np.float32)
    ang = (pos[:, None] * inv_freq[None, :]).astype(np.float32)
    cos8 = np.cos(ang).astype(np.float32).T
    sin8 = np.sin(ang).astype(np.float32).T
    cosT = np.ones((128, S), np.float32)
    sinT = np.zeros((128, S), np.float32)
    for hb in (0, 64):
        cosT[hb:hb + 8] = cos8
        cosT[hb + 8:hb + 16] = cos8
        sinT[hb:hb + 8] = sin8
        sinT[hb + 8:hb + 16] = sin8
    iota128 = np.tile(np.arange(128, dtype=np.float32)[None, :], (128, 1))
    return dict(identf=identf, bones=bones, ropepT=ropepT, mprev=mprev, mcur=mcur, cosT=cosT, sinT=sinT, iota128=iota128)


def _layout_shared(w_ada, b_ada, g_norm1, w_in, g_q, g_k, sinks, conv_w, g_out_attn, g_out_conv,
                   w_out, g_norm2, w_pq, peer_subkeys, peer_u, peer_v):
    f = lambda a: np.ascontiguousarray(np.asarray(a, dtype=np.float32))
    col16 = lambda v: f(np.asarray(v).reshape(16, 128).T)
    w_in = np.asarray(w_in)
    q = w_in[:, 0:1024]
    k = w_in[:, 1024:1280]
    v = w_in[:, 1280:1536]
    bg = w_in[:, 1536:2560]
    cg = w_in[:, 2560:3584]
    hc = w_in[:, 3584:4608]
    kd = np.concatenate([k[:, h * 64:(h + 1) * 64] for h in range(4) for _ in range(2)], axis=1)
    vd = np.concatenate([v[:, h * 64:(h + 1) * 64] for h in range(4) for _ in range(2)], axis=1)
    w_in2 = f(np.concatenate([q, kd, vd, cg, hc, bg], axis=1))
    subkT = f(np.asarray(peer_subkeys).reshape(16, 128, 128).transpose(2, 0, 1).reshape(128, 16 * 128))
    convw = f(np.asarray(conv_w).reshape(3, 8, 128).transpose(2, 1, 0).reshape(128, 24))
    d = dict(
        w_ada=f(w_ada), b_ada=f(np.asarray(b_ada).reshape(1, -1)), g1col=col16(g_norm1), g2col=col16(g_norm2),
        w_in2=w_in2,
        gqcol=f(np.tile(np.asarray(g_q), 2).reshape(128, 1)), gkcol=f(np.tile(np.asarray(g_k), 2).reshape(128, 1)),
        sinks=f(np.asarray(sinks).reshape(1, 16)), convw=convw,
        gattn=f(np.asarray(g_out_attn).reshape(8, 128).T), gconv=f(np.asarray(g_out_conv).reshape(8, 128).T),
        w_out=f(w_out), w_pq=f(w_pq), subkT=subkT, peer_uT=f(np.asarray(peer_u).T), peer_v=f(peer_v),
    )
    d.update(_consts())
    return d


def kernel(x, c, w_ada, b_ada, g_norm1, w_in, g_q, g_k, sinks, conv_w, g_out_attn, g_out_conv,
           w_out, g_norm2, w_pq, peer_subkeys, peer_u, peer_v, _cores=None):
    x = np.asarray(x, dtype=np.float32)
    c = np.asarray(c, dtype=np.float32)
    shared = _layout_shared(w_ada, b_ada, g_norm1, w_in, g_q, g_k, sinks, conv_w, g_out_attn, g_out_conv,
                            w_out, g_norm2, w_pq, peer_subkeys, peer_u, peer_v)
    cores = list(range(8)) if _cores is None else list(_cores)
    nc, _ = build_program()
    in_maps = []
    for b in cores:
        m = dict(shared)
        m["x"] = np.ascontiguousarray(x[b])
        m["ccol"] = np.ascontiguousarray(c[b].reshape(16, 128).T)
        in_maps.append(m)
    res = run_bass_kernel_spmd(nc, in_maps, core_ids=list(range(len(cores))))
    outs = [np.asarray(r["out"], dtype=np.float32) for r in res.results]
    if _cores is not None:
        return outs
    return np.stack(outs, axis=0)
```

```python
import numpy as np
from contextlib import ExitStack
import concourse.bass as bass
import concourse.mybir as mybir
from concourse.bass_utils import run_bass_kernel_spmd

F32 = mybir.dt.float32
BF16 = mybir.dt.bfloat16
U32 = mybir.dt.uint32
I32 = mybir.dt.int32
ALU = mybir.AluOpType
AF = mybir.ActivationFunctionType
AX = mybir.AxisListType

S = 4096
D = 2048
TT = 256
NST = S // TT
NI = TT // 128
EPS = 1e-6
NGRP = 18
INC = 5120

DEBUG_STAGE = None
N_SUPER = NST


class Buf:
    __slots__ = ("name", "w", "r", "dsem", "dcnt")

    def __init__(self, name):
        self.name = name
        self.w = None
        self.r = []
        self.dsem = None
        self.dcnt = 0


class Tracker:
    def __init__(self, nc, es):
        self.nc = nc
        self.es = es
        self.eng = {}
        for name, h in (("pe", nc.tensor), ("act", nc.scalar), ("dve", nc.vector),
                        ("pool", nc.gpsimd), ("sp", nc.sync)):
            sem = es.enter_context(nc.semaphore("sem_" + name))
            self.eng[name] = {"h": h, "sem": sem, "cnt": 0, "seen": {}, "name": name}
        self.bufs = {}
        self.dsems = {}
        self.ninst = 0

    def buf(self, name):
        b = Buf(name)
        self.bufs[name] = b
        return b

    def _wait(self, e, toks):
        best = {}
        for t in toks:
            if t is None:
                continue
            sem, val = t
            k = id(sem)
            if e["seen"].get(k, 0) >= val:
                continue
            if k not in best or best[k][1] < val:
                best[k] = (sem, val)
        for k, (sem, val) in best.items():
            if e["name"] == "pe" and sem is e["sem"]:
                continue
            e["h"].wait_ge(sem, val)
            e["seen"][k] = val
            self.ninst += 1

    @staticmethod
    def _deps(reads, writes):
        toks = []
        for b in reads:
            toks.append(b.w)
        for b in writes:
            toks.append(b.w)
            toks.extend(b.r)
        return toks

    def op(self, en, fn, reads=(), writes=(), inc=True):
        e = self.eng[en]
        self._wait(e, self._deps(reads, writes))
        ins = fn(e["h"])
        self.ninst += 1
        if inc:
            e["cnt"] += 1
            ins.then_inc(e["sem"], 1)
            tok = (e["sem"], e["cnt"])
        else:
            tok = (e["sem"], e["cnt"] + 1)
        for b in reads:
            b.r.append(tok)
        for b in writes:
            b.w = tok
            b.r = []
        return tok

    def dma(self, en, fn, reads=(), writes=(), sembuf=None):
        e = self.eng[en]
        sb = sembuf if sembuf is not None else (writes[0] if writes else reads[0])
        if sb.dsem is None:
            if sb.name not in self.dsems:
                self.dsems[sb.name] = [self.es.enter_context(self.nc.semaphore("ds_" + sb.name)), 0]
            sb.dsem = self.dsems[sb.name][0]
            sb.dcnt = self.dsems[sb.name][1]
        toks = [t for t in self._deps(reads, writes) if not (t is not None and t[0] is sb.dsem)]
        self._wait(e, toks)
        ins = fn(e["h"])
        self.ninst += 1
        sb.dcnt += 16
        self.dsems[sb.name][1] = sb.dcnt
        ins.then_inc(sb.dsem, 16)
        tok = (sb.dsem, sb.dcnt)
        for b in reads:
            b.r.append(tok)
        for b in writes:
            b.w = tok
            b.r = []
        return tok

    def wait_all(self, en, bufs):
        e = self.eng[en]
        toks = []
        for b in bufs:
            toks.append(b.w)
            toks.extend(b.r)
        self._wait(e, toks)

    def barrier(self):
        sp = self.eng["sp"]
        toks = []
        for n, e in self.eng.items():
            if n != "sp" and e["cnt"] > 0:
                toks.append((e["sem"], e["cnt"]))
        for nm, (dsem, dcnt) in self.dsems.items():
            if dcnt > 0:
                toks.append((dsem, dcnt))
        self._wait(sp, toks)
        sp["cnt"] += 1
        sp["h"].nop().then_inc(sp["sem"], 1)
        self.ninst += 1
        tok = (sp["sem"], sp["cnt"])
        for n, e in self.eng.items():
            if n != "sp":
                self._wait(e, [tok])
        for b in self.bufs.values():
            b.w = None
            b.r = []


def _ap(src, offset, dims):
    return bass.AP(src.tensor, src.offset + offset, [list(src.ap[0])] + [list(d) for d in dims])


def build_program():
    nc = bass.Bass("TRN2", target_bir_lowering=False)

    def din(name, shape, dt=F32):
        return nc.dram_tensor(name, list(shape), dt, kind="ExternalInput")

    x_d = din("x", [S, D])
    ccol_d = din("ccol", [128, 16])
    wada_d = din("w_ada", [D, 6 * D])
    bada_d = din("b_ada", [1, 6 * D])
    g1col_d = din("g1col", [128, 16])
    g2col_d = din("g2col", [128, 16])
    win_d = din("w_in2", [D, INC])
    gq_d = din("gqcol", [128, 1])
    gk_d = din("gkcol", [128, 1])
    sinks_d = din("sinks", [1, 16])
    convw_d = din("convw", [128, 24])
    gattn_d = din("gattn", [128, 8])
    gconv_d = din("gconv", [128, 8])
    wout_d = din("w_out", [D, D])
    wpq_d = din("w_pq", [D, D])
    subk_d = din("subkT", [128, 16 * 128])
    put_d = din("peer_uT", [D, 16384])
    pv_d = din("peer_v", [16384, D])
    identf_d = din("identf", [128, 128])
    bones_d = din("bones", [128, 128])
    ropep_d = din("ropepT", [128, 128])
    mprev_d = din("mprev", [128, 128])
    mcur_d = din("mcur", [128, 128])
    cos_d = din("cosT", [128, S])
    sin_d = din("sinT", [128, S])
    iota_d = din("iota128", [128, 128])
    out_d = nc.dram_tensor("out", [S, D], F32, kind="ExternalOutput")
    wsc_d = nc.dram_tensor("wsc", [NGRP, 128, 16 * 512], BF16)
    us_d = nc.dram_tensor("us", [32, 128, 8192], BF16)
    gt1_d = nc.dram_tensor("gt1s", [128, D], F32)
    vs_d = nc.dram_tensor("vs", [32, 128, 8192], BF16)

    with ExitStack() as es:
        def sb(name, shape, dt=F32):
            return es.enter_context(nc.sbuf_tensor(name, list(shape), dt))

        tr = Tracker(nc, es)
        B = tr.buf

        xres2 = sb("xres", [128, 2 * NI, D])
        b_xres2 = [[B("xres%d_%d" % (p_, i)) for i in range(NI)] for p_ in range(2)]
        xres = xres2[:, 0:NI, :]; b_xres = b_xres2[0]
        b_rows = B("rows")
        cols = sb("cols", [128, 4, 16]); b_cols = B("cols")
        identb = sb("identb", [128, 128], BF16); bonesb = sb("bonesb", [128, 128], BF16)
        ropepb = sb("ropepb", [128, 128], BF16); mprevb = sb("mprevb", [128, 128], BF16)
        mcurb = sb("mcurb", [128, 128], BF16); onesb = sb("onesb", [128, 128], BF16)
        onesf = sb("onesf", [1, 128]); iota16 = sb("iota16s", [128, 16]); iotab = sb("iotab", [128, 128], BF16); identf = sb("identf_s", [128, 128])
        b_const = B("const")
        gqk = sb("gqk", [128, 2]); convw = sb("convw_s", [128, 24]); gattn = sb("gattn_s", [128, 8])
        gconv = sb("gconv_s", [128, 8]); esink = sb("esink", [128, 16]); subkb = sb("subkb", [128, 16, 128], BF16)
        b_par = B("par")
        kT = sb("kT", [128, 4, 2, 128 + TT], BF16); b_kT = B("kT")
        vtok = sb("vtok", [128, NI + 1, 512], BF16); b_vtok = B("vtok")
        ubuf = sb("ubuf", [128, 8, 2 + TT]); b_u = B("ubuf")
        stat = sb("stat", [128, 8]); b_stat = B("stat")
        epsc = sb("epsc", [128, 1])

        pbank = [es.enter_context(nc.psum_tensor("pb%d" % i, [128, 512], F32)) for i in range(8)]
        b_pb = [B("pb%d" % i) for i in range(8)]
        rr = {"i": 0}

        def nextbank(lo=0, hi=8):
            i = lo + (rr["i"] % (hi - lo))
            rr["i"] += 1
            return pbank[i], b_pb[i]

        es.enter_context(nc.Block())
        b_out = B("out")
        b_wsc1 = B("wsc"); b_wsc = [b_wsc1] * NGRP
        b_uvs = B("uvs")

        with ExitStack() as ps:
            def sbp(name, shape, dt=F32):
                return ps.enter_context(nc.sbuf_tensor(name, list(shape), dt))
            NSTG = 3
            stage = [sbp("stage%d" % i, [128, 8, 512]) for i in range(NSTG)]
            b_stage = [B("stage%d" % i) for i in range(NSTG)]
            cvt = [sbp("cvt%d" % i, [128, 8 * 512], BF16) for i in range(2)]
            b_cvt = [B("cvt%d" % i) for i in range(2)]
            ccol = sbp("ccol_s", [128, 16]); b_ccol = B("ccol")
            ctmp = sbp("ctmp", [128, 128]); b_ctmp = B("ctmp")
            g12 = sbp("g12", [128, 2, 16]); b_g12 = B("g12")
            subkf = sbp("subkf", [128, 16 * 128]); b_subkf = B("subkf")
            gt2b = sbp("gt2b", [128, D]); b_gt2 = B("gt2b")
            gt1b = sbp("gt1b_p", [128, D]); b_gt1s = B("gt1s")
            pm_ = ExitStack()
            modrow = pm_.enter_context(nc.sbuf_tensor("modrow", [1, 6 * D], F32)); b_mod = B("modrow")

            sp_loads = [
                (ccol[:], ccol_d.ap(), b_ccol), (modrow[:], bada_d.ap(), b_mod),
                (g12[:, 0, :], g1col_d.ap(), b_g12), (g12[:, 1, :], g2col_d.ap(), b_g12),
                (gqk[:, 0:1], gq_d.ap(), b_par), (gqk[:, 1:2], gk_d.ap(), b_par),
                (convw[:], convw_d.ap(), b_par), (gattn[:], gattn_d.ap(), b_par),
                (gconv[:], gconv_d.ap(), b_par), (iota16[:], iota_d[:, 0:16], b_const), (identf[:], identf_d.ap(), b_const),
                (esink[:], bass.AP(sinks_d, 0, [[0, 128], [1, 16]]), b_par), (subkf[:], subk_d.ap(), b_subkf),
            ]
            for o, i_, bb in sp_loads:
                tr.dma("sp", lambda h, o=o, i_=i_: h.dma_start(out=o, in_=i_), writes=[bb])
            for k, (src_d, dst) in enumerate([(identf_d, identb), (bones_d, bonesb), (ropep_d, ropepb),
                                              (mprev_d, mprevb), (mcur_d, mcurb)]):
                tr.dma("sp", lambda h, s=src_d: h.dma_start(out=ctmp[:], in_=s.ap()), writes=[b_ctmp])
                tr.op("dve", lambda h, d=dst: h.tensor_copy(out=d[:], in_=ctmp[:]), reads=[b_ctmp], writes=[b_const])
            tr.op("dve", lambda h: h.memset(onesb[:], 1.0), writes=[b_const])
            tr.op("dve", lambda h: h.memset(epsc[:], EPS), writes=[b_const])
            tr.dma("sp", lambda h: h.dma_start(out=ctmp[:], in_=iota_d.ap()), writes=[b_ctmp])
            tr.op("dve", lambda h: h.tensor_copy(out=iotab[:], in_=ctmp[:]), reads=[b_ctmp], writes=[b_const])
            tr.op("dve", lambda h: h.memset(onesf[:], 1.0), writes=[b_const])
            tr.op("dve", lambda h: h.tensor_copy(out=subkb[:].rearrange("p a b -> p (a b)"), in_=subkf[:]),
                  reads=[b_subkf], writes=[b_par])
            tr.op("act", lambda h: h.activation(out=esink[:], in_=esink[:], func=AF.Exp), reads=[b_par], writes=[b_par])
            tr.op("dve", lambda h: h.tensor_scalar(out=esink[:], in0=esink[:], scalar1=1e-3, scalar2=None, op0=ALU.mult), reads=[b_par], writes=[b_par])
            tr.op("act", lambda h: h.activation(out=ccol[:], in_=ccol[:], func=AF.Silu), reads=[b_ccol], writes=[b_ccol])

            if DEBUG_STAGE == "p1":
                tr.barrier()
                return nc, tr.ninst
            ntile = 0
            for t in range(24):
                pb, bpb = nextbank()
                for hh in range(2):
                    sl = ntile % NSTG
                    ntile += 1
                    src = wada_d.ap().rearrange("(c p) n -> p c n", p=128)[:, hh * 8:(hh + 1) * 8, t * 512:(t + 1) * 512]
                    tr.dma("sp", lambda h, sl=sl, src=src: h.dma_start(out=stage[sl][:], in_=src), writes=[b_stage[sl]])
                    for c8 in range(8):
                        c = hh * 8 + c8
                        tr.op("pe", lambda h, c=c, c8=c8, pb=pb, sl=sl: h.matmul(pb[0:1, :], lhsT=ccol[:, c:c + 1], rhs=stage[sl][:, c8, :],
                                                                                 start=(c == 0), stop=(c == 15)),
                              reads=[b_ccol, b_stage[sl]], writes=[bpb], inc=(c8 == 7))
                tr.op("dve", lambda h, pb=pb, t=t: h.tensor_tensor(out=modrow[0:1, t * 512:(t + 1) * 512], in0=pb[0:1, :],
                                                                   in1=modrow[0:1, t * 512:(t + 1) * 512], op=ALU.add),
                      reads=[bpb, b_mod], writes=[b_mod])
            if DEBUG_STAGE == "p2":
                tr.barrier()
                return nc, tr.ninst
            for dst, sec, bdst in ((gt1b, 2, b_rows), (gt2b, 5, b_gt2)):
                for dg in range(4):
                    pb, bpb = nextbank()
                    tr.op("pe", lambda h, pb=pb, sec=sec, dg=dg: h.matmul(pb[:, :], lhsT=onesf[0:1, :],
                                                                          rhs=modrow[0:1, sec * D + dg * 512: sec * D + (dg + 1) * 512],
                                                                          start=True, stop=True),
                          reads=[b_mod, b_const], writes=[bpb])
                    tr.op("act", lambda h, pb=pb, dst=dst, dg=dg: h.activation(out=dst[:, dg * 512:(dg + 1) * 512], in_=pb[:, :], func=AF.Copy),
                          reads=[bpb], writes=[bdst])
            tr.dma("sp", lambda h: h.dma_start(out=gt1_d.ap(), in_=gt1b[:]), reads=[b_rows], writes=[b_gt1s], sembuf=b_rows)
            pb, bpb = nextbank()
            for k, sec in enumerate((0, 1, 3, 4)):
                for c in range(16):
                    tr.op("pe", lambda h, pb=pb, k=k, c=c, sec=sec: h.matmul(
                        pb[:, (k * 16 + c) * 2:(k * 16 + c) * 2 + 2], lhsT=modrow[0:1, sec * D + c * 128: sec * D + (c + 1) * 128],
                        rhs=onesf[0:1, 0:2], start=True, stop=True), reads=[b_mod, b_const], writes=[bpb],
                        inc=(k == 3 and c == 15))
            pbv = pb[:, 0:128].rearrange("p (k c two) -> p k c two", k=4, c=16, two=2)
            tr.op("dve", lambda h: h.tensor_copy(out=cols[:], in_=pbv[:, :, :, 0]), reads=[bpb], writes=[b_cols])
            for k, gi in ((1, 0), (3, 1)):
                tr.op("dve", lambda h, k=k, gi=gi: h.scalar_tensor_tensor(out=cols[:, k, :], in0=cols[:, k, :], scalar=1.0,
                                                                         in1=g12[:, gi, :], op0=ALU.add, op1=ALU.mult),
                      reads=[b_cols, b_g12], writes=[b_cols])

            if DEBUG_STAGE == "p3":
                tr.barrier()
                return nc, tr.ninst
            tr.barrier()
            pm_.close()
            for k_ in range(3):
                stage.append(sbp("stage%d" % (NSTG + k_), [128, 8, 512])); b_stage.append(B("stage%d" % (NSTG + k_)))
            NSTG = 6
            gsrc = []
            for g in range(10):
                gsrc.append(win_d.ap().rearrange("(c p) n -> p c n", p=128)[:, :, g * 512:(g + 1) * 512])
            for g in range(4):
                gsrc.append(wout_d.ap().rearrange("(c p) n -> p c n", p=128)[:, :, g * 512:(g + 1) * 512])
            for g in range(4):
                gsrc.append(wpq_d.ap().rearrange("(c p) n -> p c n", p=128)[:, :, g * 512:(g + 1) * 512])
            utv = put_d.ap().rearrange("(c p) n -> p c n", p=128)
            vv = pv_d.ap().rearrange("(g c p) d -> g p c d", c=4, p=128)
            jobs = []
            for g in range(NGRP):
                for hh in range(2):
                    jobs.append((gsrc[g][:, hh * 8:(hh + 1) * 8, :], False, wsc_d[g][:, hh * 4096:(hh + 1) * 4096], b_wsc[g]))
            for g in range(32):
                for hh in range(2):
                    jobs.append((utv[:, hh * 8:(hh + 1) * 8, g * 512:(g + 1) * 512], False, us_d[g][:, hh * 4096:(hh + 1) * 4096], b_uvs))
            for g in range(32):
                for hh in range(2):
                    jobs.append((vv[g][:, hh * 2:(hh + 1) * 2, :], True, vs_d[g][:, hh * 4096:(hh + 1) * 4096], b_uvs))
            base = ntile

            def job_in(k):
                src, is_v, dst, bd = jobs[k]
                sl = (base + k) % NSTG
                o = stage[sl][:].rearrange("p c n -> p (c n)").rearrange("p (c d) -> p c d", c=2) if is_v else stage[sl][:]
                tr.dma("sp", lambda h: h.dma_start(out=o, in_=src), writes=[b_stage[sl]])

            for k in range(min(NSTG, len(jobs))):
                job_in(k)
            for k in range(len(jobs)):
                src, is_v, dst, bd = jobs[k]
                sl = (base + k) % NSTG
                cs = k % 2
                stf = stage[sl][:].rearrange("p c n -> p (c n)")
                if is_v:
                    tr.op("dve", lambda h: h.tensor_tensor(out=cvt[cs][:, 0:2048], in0=stf[:, 0:2048], in1=gt2b[:], op=ALU.mult),
                          reads=[b_stage[sl], b_gt2], writes=[b_cvt[cs]])
                    tr.op("pool", lambda h: h.tensor_tensor(out=cvt[cs][:, 2048:4096], in0=stf[:, 2048:4096], in1=gt2b[:], op=ALU.mult),
                          reads=[b_stage[sl], b_gt2], writes=[b_cvt[cs]])
                else:
                    tr.op("act", lambda h: h.activation(out=cvt[cs][:, 0:2048], in_=stf[:, 0:2048], func=AF.Copy),
                          reads=[b_stage[sl]], writes=[b_cvt[cs]])
                    tr.op("dve", lambda h: h.tensor_copy(out=cvt[cs][:, 2048:4096], in_=stf[:, 2048:4096]),
                          reads=[b_stage[sl]], writes=[b_cvt[cs]])
                if k + NSTG < len(jobs):
                    job_in(k + NSTG)
                tr.dma("sp", lambda h: h.dma_start(out=dst, in_=cvt[cs][:]), reads=[b_cvt[cs]], writes=[bd], sembuf=b_cvt[cs])
            tr.op("dve", lambda h: h.memset(kT[:], 0.0), writes=[b_kT])
            tr.op("dve", lambda h: h.memset(vtok[:], 0.0), writes=[b_vtok])
            tr.op("dve", lambda h: h.memset(ubuf[:], 0.0), writes=[b_u])
            tr.barrier()

        def load_w(g, wb, bwb):
            for hh in range(2):
                tr.dma("sp", lambda h, hh=hh: h.dma_start(out=wb[:, hh * 4096:(hh + 1) * 4096], in_=wsc_d[g][:, hh * 4096:(hh + 1) * 4096]),
                       reads=[b_wsc[g]], writes=[bwb])

        def rstd_of(ssq_ap, out_ap, scale, bufs_r, bufs_w):
            tr.op("act", lambda h: h.activation(out=out_ap, in_=ssq_ap, func=AF.Ln, scale=scale, bias=epsc[:]),
                  reads=list(bufs_r) + [b_const], writes=bufs_w)
            tr.op("act", lambda h: h.activation(out=out_ap, in_=out_ap, func=AF.Exp, scale=-0.5), reads=bufs_w, writes=bufs_w)

        def norm_transpose(xn, b_xn, hT, b_hT, kcol_shift, kcol_scale, stat_off):
            for i in range(NI):
                tr.op("act", lambda h, i=i: h.activation(out=xn[:, i, :], in_=xres[:, i, :], func=AF.Square,
                                                         accum_out=stat[:, stat_off + i:stat_off + i + 1]),
                      reads=[b_xres[i]], writes=[b_xn, b_stat])
            rstd_of(stat[:, stat_off:stat_off + NI], stat[:, stat_off + 2:stat_off + 2 + NI], 1.0 / D, [b_stat], [b_stat])
            for i in range(NI):
                tr.op("act", lambda h, i=i: h.activation(out=xn[:, i, :], in_=xres[:, i, :], func=AF.Copy,
                                                         scale=stat[:, stat_off + 2 + i:stat_off + 3 + i]),
                      reads=[b_xres[i], b_stat], writes=[b_xn])
            for c in range(16):
                pb, bpb = nextbank()
                pbb = pb[:].bitcast(BF16)
                for i in range(NI):
                    tr.op("pe", lambda h, i=i, c=c, pbb=pbb: h.transpose(out=pbb[:, i * 128:(i + 1) * 128],
                                                                         in_=xn[:, i, c * 128:(c + 1) * 128], identity=identb[:]),
                          reads=[b_xn, b_const], writes=[bpb], inc=(i == NI - 1))
                tr.op("dve", lambda h, c=c, pbb=pbb: h.tensor_scalar(out=hT[:, c, :], in0=pbb[:, 0:TT],
                                                                     scalar1=cols[:, kcol_scale, c:c + 1], scalar2=cols[:, kcol_shift, c:c + 1],
                                                                     op0=ALU.mult, op1=ALU.add),
                      reads=[bpb, b_cols], writes=[b_hT])

        for st in range(N_SUPER):
            t0 = st * TT
            with ExitStack() as pa:
                def sba(name, shape, dt=F32):
                    return pa.enter_context(nc.sbuf_tensor("%s_a%d" % (name, st), list(shape), dt))
                xn = sba("xn", [128, NI, D], BF16); b_xn = B("xn")
                hT = sba("hT", [128, 16, TT], BF16); b_hT = B("hT")
                wbuf = [sba("wbuf%d" % i, [128, 8192], BF16) for i in range(2)]
                b_wbuf = [B("wbuf%d" % i) for i in range(2)]
                cst = sba("cst", [128, 2, TT]); b_cst = B("cst")
                qT = sba("qT", [128, 8, TT], BF16); b_qT = B("qT")
                cgs = sba("cgs", [128, 8, TT]); b_cgs = B("cgs")
                mixT = sba("mixT", [128, 16, TT], BF16); b_mixT = B("mixT")
                Pt = [sba("Pt%d" % i, [128, 512], BF16) for i in range(4)]
                gt1b = sba("gt1b", [128, D])
                b_Pt = [B("Pt%d" % i) for i in range(4)]

                par_ = st % 2
                xres = xres2[:, par_ * NI:(par_ + 1) * NI, :]; b_xres = b_xres2[par_]
                if st == 0 or DEBUG_STAGE == "A":
                    for i in range(NI):
                        tr.dma("sp", lambda h, i=i: h.dma_start(out=xres[:, i, :], in_=x_d[t0 + i * 128: t0 + (i + 1) * 128, :]),
                               writes=[b_xres[i]])
                tr.dma("sp", lambda h: h.dma_start(out=cst[:, 0, :], in_=cos_d[:, t0:t0 + TT]), writes=[b_cst])
                tr.dma("sp", lambda h: h.dma_start(out=cst[:, 1, :], in_=sin_d[:, t0:t0 + TT]), writes=[b_cst])
                load_w(0, wbuf[0], b_wbuf[0])
                tr.dma("sp", lambda h: h.dma_start(out=gt1b[:], in_=gt1_d.ap()), reads=[b_gt1s], writes=[b_rows])
                load_w(1, wbuf[1], b_wbuf[1])
                norm_transpose(xn, b_xn, hT, b_hT, 0, 1, 0)

                if DEBUG_STAGE == "a1":
                    tr.barrier()
                    return nc, tr.ninst
                tmpc = {"i": 0}
                T2 = {}
                for nm, shp, dt_ in (("sq", [128, 512], BF16), ("r", [128, 512], F32), ("a", [128, 512], F32), ("b", [128, 512], F32), ("qn", [128, TT], BF16)):
                    T2[nm] = [(sba("t2%s%d" % (nm, k), shp, dt_), B("t2%s%d" % (nm, k))) for k in range(4)]

                def tmps():
                    k = tmpc["i"] % 4
                    tmpc["i"] += 1
                    return {nm: T2[nm][k] for nm in T2}

                pgc = {"i": 0}

                def proj_group(g):
                    wb, bwb = wbuf[g % 2], b_wbuf[g % 2]
                    bs = 2 * (pgc["i"] % 2)
                    pgc["i"] += 1
                    outs = []
                    for ch in range(4):
                        pbk, bpbk = pbank[bs + ch // 2], b_pb[bs + ch // 2]
                        pv = pbk[:, (ch % 2) * TT:(ch % 2 + 1) * TT]
                        for c in range(16):
                            tr.op("pe", lambda h, c=c, pv=pv, ch=ch: h.matmul(pv, lhsT=wb[:, c * 512 + ch * 128: c * 512 + (ch + 1) * 128],
                                                                             rhs=hT[:, c, :], start=(c == 0), stop=(c == 15)),
                                  reads=[bwb, b_hT], writes=[bpbk], inc=(c == 15))
                        outs.append((pv, bpbk))
                    if g + 2 < 14:
                        load_w(g + 2, wbuf[g % 2], b_wbuf[g % 2])
                    return outs

                def halfbank(k, ch):
                    pbk, bpbk = pbank[k + ch // 2], b_pb[k + ch // 2]
                    return pbk[:, (ch % 2) * TT:(ch % 2 + 1) * TT], bpbk

                def rstd_stages(srcs, Ts, n):
                    for (src_ap, b_src), T in zip(srcs, Ts):
                        (tsq, btsq) = T["sq"]
                        tr.op("act", lambda h, tsq=tsq, src_ap=src_ap: h.activation(out=tsq[:, 0:n], in_=src_ap, func=AF.Square),
                              reads=[b_src], writes=[btsq])
                    pbs = []
                    for ch, T in enumerate(Ts):
                        (tsq, btsq) = T["sq"]
                        pv2, bpb2 = halfbank(4, ch)
                        tr.op("pe", lambda h, tsq=tsq, pv2=pv2: h.matmul(pv2, lhsT=bonesb[:], rhs=tsq[:, 0:n], start=True, stop=True),
                              reads=[btsq, b_const], writes=[bpb2])
                        pbs.append((pv2, bpb2))
                    for (pv2, bpb2), T in zip(pbs, Ts):
                        (tr_, btr) = T["r"]
                        tr.op("act", lambda h, tr_=tr_, pv2=pv2: h.activation(out=tr_[:, 0:n], in_=pv2, func=AF.Ln, bias=epsc[:]),
                              reads=[bpb2, b_const], writes=[btr])
                    for T in Ts:
                        (tr_, btr) = T["r"]
                        tr.op("act", lambda h, tr_=tr_: h.activation(out=tr_[:, 0:n], in_=tr_[:, 0:n], func=AF.Exp, scale=-0.5), reads=[btr], writes=[btr])

                def post_qk(g, outs):
                    Ts = [tmps() for _ in range(4)]
                    rstd_stages(outs, Ts, TT)
                    gcol = gqk[:, 0:1] if g < 2 else gqk[:, 1:2]
                    for (pv, bpb), T in zip(outs, Ts):
                        (tr_, btr), (tqn, btqn) = T["r"], T["qn"]
                        tr.op("dve", lambda h, pv=pv, tr_=tr_, tqn=tqn: h.scalar_tensor_tensor(out=tqn[:], in0=pv, scalar=gcol, in1=tr_[:, 0:TT],
                                                                                             op0=ALU.mult, op1=ALU.mult),
                              reads=[bpb, btr, b_par], writes=[btqn])
                    rps = []
                    for ch, T in enumerate(Ts):
                        (tqn, btqn) = T["qn"]
                        pv3, bpb3 = halfbank(6, ch)
                        tr.op("pe", lambda h, tqn=tqn, pv3=pv3: h.matmul(pv3, lhsT=ropepb[:], rhs=tqn[:], start=True, stop=True),
                              reads=[btqn, b_const], writes=[bpb3])
                        rps.append((pv3, bpb3))
                    for T in Ts:
                        (ta, bta), (tqn, btqn) = T["a"], T["qn"]
                        tr.op("pool", lambda h, ta=ta, tqn=tqn: h.tensor_tensor(out=ta[:, 0:TT], in0=tqn[:], in1=cst[:, 0, :], op=ALU.mult),
                              reads=[btqn, b_cst], writes=[bta])
                    for (pv3, bpb3), T in zip(rps, Ts):
                        (tb, btb) = T["b"]
                        tr.op("dve", lambda h, tb=tb, pv3=pv3: h.tensor_tensor(out=tb[:, 0:TT], in0=pv3, in1=cst[:, 1, :], op=ALU.mult),
                              reads=[bpb3, b_cst], writes=[btb])
                    for ch, T in enumerate(Ts):
                        (ta, bta), (tb, btb) = T["a"], T["b"]
                        if g < 2:
                            tr.op("dve", lambda h, ch=ch, ta=ta, tb=tb: h.tensor_tensor(out=qT[:, g * 4 + ch, :], in0=ta[:, 0:TT], in1=tb[:, 0:TT], op=ALU.add),
                                  reads=[bta, btb], writes=[b_qT])
                        else:
                            for half in range(2):
                                ps_ = slice(half * 64, (half + 1) * 64)
                                tr.op("dve", lambda h, ch=ch, half=half, ps_=ps_, ta=ta, tb=tb: h.tensor_tensor(
                                    out=kT[ps_, ch, half, 128:128 + TT], in0=ta[ps_, 0:TT], in1=tb[ps_, 0:TT], op=ALU.add),
                                    reads=[bta, btb], writes=[b_kT])

                def post_conv(g, outs):
                    if g in (4, 5):
                        for ch in range(4):
                            pv, bpb = outs[ch]
                            cc = (g - 4) * 4 + ch
                            tr.op("act", lambda h, cc=cc, pv=pv: h.activation(out=cgs[:, cc, :], in_=pv, func=AF.Copy), reads=[bpb], writes=[b_cgs])
                    elif g in (6, 7):
                        for ch in range(4):
                            pv, bpb = outs[ch]
                            cc = (g - 6) * 4 + ch
                            tr.op("dve", lambda h, cc=cc, pv=pv: h.tensor_tensor(out=ubuf[:, cc, 2:2 + TT], in0=pv, in1=cgs[:, cc, :], op=ALU.mult),
                                  reads=[bpb, b_cgs], writes=[b_u])
                            tr.op("dve", lambda h, cc=cc: h.tensor_scalar(out=cgs[:, cc, :], in0=ubuf[:, cc, 0:TT], scalar1=convw[:, cc * 3:cc * 3 + 1],
                                                                           scalar2=None, op0=ALU.mult), reads=[b_u, b_par], writes=[b_cgs])
                            for tap in (1, 2):
                                tr.op("dve", lambda h, cc=cc, tap=tap: h.scalar_tensor_tensor(
                                    out=cgs[:, cc, :], in0=ubuf[:, cc, tap:tap + TT], scalar=convw[:, cc * 3 + tap:cc * 3 + tap + 1],
                                    in1=cgs[:, cc, :], op0=ALU.mult, op1=ALU.add), reads=[b_u, b_par, b_cgs], writes=[b_cgs])
                            tr.op("pool", lambda h, cc=cc: h.tensor_copy(out=ubuf[:, cc, 0:2], in_=ubuf[:, cc, TT:TT + 2]), reads=[b_u], writes=[b_u])
                    else:
                        Ts = [tmps() for _ in range(4)]
                        for ch, T in enumerate(Ts):
                            pv, bpb = outs[ch]
                            cc = (g - 8) * 4 + ch
                            (ta, bta) = T["a"]
                            tr.op("dve", lambda h, cc=cc, pv=pv, ta=ta: h.tensor_tensor(out=ta[:, 0:TT], in0=pv, in1=cgs[:, cc, :], op=ALU.mult),
                                  reads=[bpb, b_cgs], writes=[bta])
                        rstd_stages([(T["a"][0][:, 0:TT], T["a"][1]) for T in Ts], Ts, TT)
                        for ch, T in enumerate(Ts):
                            cc = (g - 8) * 4 + ch
                            (tr_, btr), (ta, bta) = T["r"], T["a"]
                            tr.op("dve", lambda h, cc=cc, ta=ta, tr_=tr_: h.scalar_tensor_tensor(out=mixT[:, 8 + cc, :], in0=ta[:, 0:TT], scalar=gconv[:, cc:cc + 1],
                                                                                                in1=tr_[:, 0:TT], op0=ALU.mult, op1=ALU.mult),
                                  reads=[bta, btr, b_par], writes=[b_mixT])

                def v_proj():
                    wb, bwb = wbuf[3 % 2], b_wbuf[3 % 2]
                    for i in range(NI):
                        pb, bpb = nextbank(4, 8)
                        for c in range(16):
                            tr.op("pe", lambda h, c=c, i=i, pb=pb: h.matmul(pb[:, :], lhsT=hT[:, c, i * 128:(i + 1) * 128],
                                                                            rhs=wb[:, c * 512:(c + 1) * 512], start=(c == 0), stop=(c == 15)),
                                  reads=[bwb, b_hT], writes=[bpb], inc=(c == 15))
                        tr.op("act", lambda h, i=i, pb=pb: h.activation(out=vtok[:, 1 + i, :], in_=pb[:, :], func=AF.Copy),
                              reads=[bpb], writes=[b_vtok])
                    load_w(5, wbuf[1], b_wbuf[1])

                def attn_pair(i, hks):
                    n = st * NI + i
                    kbs = ([] if n == 0 else [("prev", i * 128, i, mprevb)]) + [("cur", (i + 1) * 128, i + 1, mcurb)]
                    nk = len(kbs)
                    Ts = [tmps() for _ in hks]
                    sc = {}
                    for a_, hk in enumerate(hks):
                        for kbi, (nm, kc0, vblk, msk) in enumerate(kbs):
                            pb, bpb = pbank[a_ * 2 + kbi], b_pb[a_ * 2 + kbi]
                            for j in range(4):
                                half = j % 2
                                qc = 2 * hk + j // 2
                                tr.op("pe", lambda h, pb=pb, j=j, half=half, qc=qc, kc0=kc0, hk=hk: h.matmul(
                                    pb[:, j * 128:(j + 1) * 128], lhsT=kT[:, hk, half, kc0:kc0 + 128],
                                    rhs=qT[:, qc, i * 128:(i + 1) * 128], start=True, stop=True),
                                    reads=[b_kT, b_qT], writes=[bpb], inc=(j == 3))
                            sc[(a_, kbi)] = (pb, bpb)
                    for a_, hk in enumerate(hks):
                        for kbi in range(nk):
                            pb, bpb = sc[(a_, kbi)]
                            P_, bP = Pt[a_ * 2 + kbi], b_Pt[a_ * 2 + kbi]
                            tr.op("act", lambda h, pb=pb, P_=P_: h.activation(out=P_[:], in_=pb[:, :], func=AF.Exp, scale=0.125),
                                  reads=[bpb], writes=[bP])
                    for a_, hk in enumerate(hks):
                        for kbi, (nm, kc0, vblk, msk) in enumerate(kbs):
                            P_, bP = Pt[a_ * 2 + kbi], b_Pt[a_ * 2 + kbi]
                            tr.op("pool", lambda h, P_=P_, msk=msk: h.tensor_tensor(
                                out=P_[:].rearrange("p (j q) -> p j q", j=4), in0=P_[:].rearrange("p (j q) -> p j q", j=4),
                                in1=msk[:].unsqueeze(1).broadcast_to([128, 4, 128]), op=ALU.mult),
                                reads=[bP, b_const], writes=[bP])
                    pos, pds = [], []
                    for a_, hk in enumerate(hks):
                        po, bpo = pbank[4 + a_], b_pb[4 + a_]
                        pd, bpd = pbank[6 + a_], b_pb[6 + a_]
                        for kbi, (nm, kc0, vblk, msk) in enumerate(kbs):
                            P_, bP = Pt[a_ * 2 + kbi], b_Pt[a_ * 2 + kbi]
                            tr.op("pe", lambda h, P_=P_, vblk=vblk, kbi=kbi, po=po, hk=hk: h.matmul(
                                po[:, :], lhsT=vtok[:, vblk, hk * 128:(hk + 1) * 128], rhs=P_[:],
                                start=(kbi == 0), stop=(kbi == nk - 1)), reads=[bP, b_vtok], writes=[bpo], inc=(kbi == nk - 1))
                        for kbi in range(nk):
                            P_, bP = Pt[a_ * 2 + kbi], b_Pt[a_ * 2 + kbi]
                            tr.op("pe", lambda h, P_=P_, kbi=kbi, pd=pd: h.matmul(pd[:, :], lhsT=onesb[:], rhs=P_[:],
                                                                                 start=(kbi == 0), stop=(kbi == nk - 1)),
                                  reads=[bP, b_const], writes=[bpd], inc=(kbi == nk - 1))
                        pos.append((po, bpo))
                        pds.append((pd, bpd))
                    for a_, T in enumerate(Ts):
                        (tsq, btsq) = T["sq"]
                        po, bpo = pos[a_]
                        tr.op("act", lambda h, tsq=tsq, po=po: h.activation(out=tsq[:], in_=po[:, :], func=AF.Square), reads=[bpo], writes=[btsq])
                    pms = []
                    for a_, T in enumerate(Ts):
                        (tsq, btsq) = T["sq"]
                        pm, bpm = pbank[a_ * 2], b_pb[a_ * 2]
                        tr.op("pe", lambda h, tsq=tsq, pm=pm: h.matmul(pm[:, :], lhsT=bonesb[:], rhs=tsq[:], start=True, stop=True),
                              reads=[btsq, b_const], writes=[bpm])
                        pms.append((pm, bpm))
                    for a_, hk in enumerate(hks):
                        (ta, bta) = Ts[a_]["a"]
                        pd, bpd = pds[a_]
                        for j in range(4):
                            tr.op("act", lambda h, j=j, ta=ta, pd=pd, hk=hk: h.activation(out=ta[:, j * 128:(j + 1) * 128], in_=pd[:, j * 128:(j + 1) * 128],
                                                                                         func=AF.Square, scale=1e-3, bias=esink[:, hk * 4 + j:hk * 4 + j + 1]),
                                  reads=[bpd, b_par], writes=[bta])
                    for a_, T in enumerate(Ts):
                        (tr_, btr), (ta, bta) = T["r"], T["a"]
                        pm, bpm = pms[a_]
                        tr.op("dve", lambda h, tr_=tr_, pm=pm, ta=ta: h.tensor_tensor(out=tr_[:], in0=pm[:, :], in1=ta[:], op=ALU.add),
                              reads=[bpm, bta], writes=[btr])
                    for T in Ts:
                        (tr_, btr) = T["r"]
                        tr.op("act", lambda h, tr_=tr_: h.activation(out=tr_[:], in_=tr_[:], func=AF.Ln), reads=[btr], writes=[btr])
                    for T in Ts:
                        (tr_, btr) = T["r"]
                        tr.op("act", lambda h, tr_=tr_: h.activation(out=tr_[:], in_=tr_[:], func=AF.Exp, scale=-0.5), reads=[btr], writes=[btr])
                    for a_, hk in enumerate(hks):
                        (tr_, btr) = Ts[a_]["r"]
                        po, bpo = pos[a_]
                        for j in range(4):
                            half = j % 2
                            mc = 2 * hk + j // 2
                            ps_ = slice(half * 64, (half + 1) * 64)
                            tr.op("dve", lambda h, j=j, mc=mc, ps_=ps_, po=po, tr_=tr_: h.scalar_tensor_tensor(
                                out=mixT[ps_, mc, i * 128:(i + 1) * 128], in0=po[ps_, j * 128:(j + 1) * 128],
                                scalar=gattn[ps_, mc:mc + 1], in1=tr_[ps_, j * 128:(j + 1) * 128], op0=ALU.mult, op1=ALU.mult),
                                reads=[bpo, btr, b_par], writes=[b_mixT])

                o0 = proj_group(0)
                o1 = proj_group(1)
                post_qk(0, o0)
                o2 = proj_group(2)
                post_qk(1, o1)
                v_proj()
                post_qk(2, o2)
                for i in range(NI):
                    for hp in range(2):
                        attn_pair(i, (2 * hp, 2 * hp + 1))
                tr.op("pool", lambda h: h.tensor_copy(out=kT[:, :, :, 0:128], in_=kT[:, :, :, TT:TT + 128]), reads=[b_kT], writes=[b_kT])
                tr.op("pool", lambda h: h.tensor_copy(out=vtok[:, 0, :], in_=vtok[:, NI, :]), reads=[b_vtok], writes=[b_vtok])
                pend = None
                for g in range(4, 10):
                    o = proj_group(g)
                    if pend is not None:
                        post_conv(*pend)
                    pend = (g, o)
                post_conv(*pend)
                for g in range(10, 14):
                    wb, bwb = wbuf[g % 2], b_wbuf[g % 2]
                    dg = g - 10
                    for i in range(NI):
                        pb, bpb = nextbank()
                        T = tmps()
                        ta, bta = T["a"]
                        for mc in range(16):
                            tr.op("pe", lambda h, mc=mc, i=i, pb=pb: h.matmul(pb[:, :], lhsT=mixT[:, mc, i * 128:(i + 1) * 128],
                                                                             rhs=wb[:, mc * 512:(mc + 1) * 512], start=(mc == 0), stop=(mc == 15)),
                                  reads=[bwb, b_mixT], writes=[bpb], inc=(mc == 15))
                        tr.op("dve", lambda h, pb=pb, dg=dg, ta=ta: h.tensor_tensor(out=ta[:], in0=pb[:, :], in1=gt1b[:, dg * 512:(dg + 1) * 512], op=ALU.mult),
                              reads=[bpb, b_rows], writes=[bta])
                        tr.op("pool", lambda h, i=i, dg=dg, ta=ta: h.tensor_tensor(out=xres[:, i, dg * 512:(dg + 1) * 512], in0=xres[:, i, dg * 512:(dg + 1) * 512],
                                                                                in1=ta[:], op=ALU.add), reads=[bta, b_xres[i]], writes=[b_xres[i]])
                    if g + 2 < 14:
                        load_w(g + 2, wbuf[g % 2], b_wbuf[g % 2])
                tr.barrier()

            if DEBUG_STAGE == "A":
                for i in range(NI):
                    tr.dma("sp", lambda h, i=i: h.dma_start(out=out_d[t0 + i * 128: t0 + (i + 1) * 128, :], in_=xres[:, i, :]),
                           reads=[b_xres[i]], writes=[b_out], sembuf=b_xres[i])
                tr.barrier()
                continue

            with ExitStack() as pp:
                def sbq(name, shape, dt=F32):
                    return pp.enter_context(nc.sbuf_tensor("%s_p%d" % (name, st), list(shape), dt))
                hT = sbq("h2T", [128, 16, TT], BF16); b_hT = B("h2T")
                GT = sbq("GT", [128, TT, 128], BF16); b_GT = B("GT")
                mid_ = ExitStack()
                pqT = mid_.enter_context(nc.sbuf_tensor("pqT_p%d" % st, [128, 16, TT], BF16)); b_pqT = B("pqT")
                with ExitStack() as pq_:
                    def sbq1(name, shape, dt=F32):
                        return pq_.enter_context(nc.sbuf_tensor("%s_q%d" % (name, st), list(shape), dt))
                    xn = sbq1("xn2", [128, NI, D], BF16); b_xn = B("xn2")
                    wbuf = [sbq1("wbq%d" % i, [128, 8192], BF16) for i in range(2)]
                    b_wbuf = [B("wbq%d" % i) for i in range(2)]
                    load_w(14, wbuf[0], b_wbuf[0])
                    load_w(15, wbuf[1], b_wbuf[1])
                    norm_transpose(xn, b_xn, hT, b_hT, 2, 3, 4)
                    for g in range(4):
                        wb, bwb = wbuf[g % 2], b_wbuf[g % 2]
                        for ch in range(4):
                            pb, bpb = nextbank()
                            for c in range(16):
                                tr.op("pe", lambda h, c=c, pb=pb, ch=ch: h.matmul(pb[:, 0:TT], lhsT=wb[:, c * 512 + ch * 128: c * 512 + (ch + 1) * 128],
                                                                                 rhs=hT[:, c, :], start=(c == 0), stop=(c == 15)),
                                      reads=[bwb, b_hT], writes=[bpb], inc=(c == 15))
                            tr.op("act", lambda h, pb=pb, g=g, ch=ch: h.activation(out=pqT[:, g * 4 + ch, :], in_=pb[:, 0:TT], func=AF.Copy),
                                  reads=[bpb], writes=[b_pqT])
                        if g + 2 < 4:
                            load_w(14 + g + 2, wbuf[g % 2], b_wbuf[g % 2])
                    tr.barrier()
                with ExitStack() as p2_:
                    def sbq2(name, shape, dt=F32):
                        return p2_.enter_context(nc.sbuf_tensor("%s_r%d" % (name, st), list(shape), dt))
                    Ssb = sbq2("Ssb", [128, 16, 128]); b_S = B("Ssb")
                    S2s = [sbq2("S2_%d" % k, [128, 128]) for k in range(4)]; b_S2s = [B("S2_%d" % k) for k in range(4)]
                    T16 = sbq2("T16", [128, 16, 16]); b_T16s = [B("T16_%d" % k) for k in range(16)]
                    I16 = sbq2("I16", [128, 16, 16], U32); b_I16s = [B("I16_%d" % k) for k in range(16)]
                    I16f = sbq2("I16f", [128, 16, 16]); b_I16f = B("I16f")
                    cand = sbq2("cand", [128, 8, 256]); b_cand = B("cand")
                    cand2s = [sbq2("cand2_%d" % k, [128, 256]) for k in range(4)]; b_cand2s = [B("cand2_%d" % k) for k in range(4)]
                    C16 = sbq2("C16", [128, 8, 16]); b_C16s = [B("C16_%d" % k) for k in range(8)]
                    CI = sbq2("CI", [128, 8, 16], U32); b_CIs = [B("CI_%d" % k) for k in range(8)]
                    rc = sbq2("rc", [128, 2, 128], U32); b_rc = B("rc")
                    rcf = sbq2("rcf", [128, 2, 128]); b_rcf = B("rcf")
                    oh_flat = cand[:].rearrange("p a b -> p (a b)"); b_oh = b_cand
                    E12 = sbq2("E12", [128, 3, 128]); b_E12 = B("E12")
                    zz = sbq2("zz", [128, 16]); b_zz = B("zz")
                    ejT = sbq2("ejT", [128, 3, 128]); b_ejT = B("ejT")
                    bXt = [[B("Xb%d_%d" % (k, tl)) for tl in range(32)] for k in range(2)]
                    bYt = [[B("Yb%d_%d" % (k, tl)) for tl in range(32)] for k in range(2)]
                    Xb = [sbq2("Xb%d" % k, [128, 32, 128], BF16) for k in range(2)]; b_Xb = [B("Xb%d" % k) for k in range(2)]
                    Yb = [sbq2("Yb%d" % k, [128, 32, 128], BF16) for k in range(2)]; b_Yb = [B("Yb%d" % k) for k in range(2)]
                    gate3 = E12[:, 2, :].rearrange("p (a b) -> p a b", a=8)
                    xyc = {"i": 0}

                    def topk_gen(i):
                        for q4 in range(4):
                            pb, bpb = nextbank()
                            for k4 in range(4):
                                hh = q4 * 4 + k4
                                tr.op("pe", lambda h, pb=pb, k4=k4, hh=hh: h.matmul(pb[:, k4 * 128:(k4 + 1) * 128], lhsT=pqT[:, hh, i * 128:(i + 1) * 128],
                                                                                    rhs=subkb[:, hh, :], start=True, stop=True),
                                      reads=[b_pqT, b_par], writes=[bpb], inc=(k4 == 3))
                            tr.op("act", lambda h, pb=pb, q4=q4: h.activation(out=Ssb[:, q4 * 4:(q4 + 1) * 4, :].rearrange("p a b -> p (a b)"),
                                                                              in_=pb[:, :], func=AF.Copy), reads=[bpb], writes=[b_S])
                            yield
                        for h4 in range(4):
                            hhs = [h4 * 4 + k for k in range(4)]
                            for k, hh in enumerate(hhs):
                                tr.op("dve", lambda h, hh=hh: h.max(out=T16[:, hh, 0:8], in_=Ssb[:, hh, :]), reads=[b_S], writes=[b_T16s[hh]])
                            for k, hh in enumerate(hhs):
                                tr.op("dve", lambda h, hh=hh, k=k: h.match_replace(out=S2s[k][:], in_to_replace=T16[:, hh, 0:8], in_values=Ssb[:, hh, :], imm_value=-1e30),
                                      reads=[b_S, b_T16s[hh]], writes=[b_S2s[k]])
                            for k, hh in enumerate(hhs):
                                tr.op("dve", lambda h, hh=hh, k=k: h.max(out=T16[:, hh, 8:16], in_=S2s[k][:]), reads=[b_S2s[k]], writes=[b_T16s[hh]])
                            for k, hh in enumerate(hhs):
                                tr.op("dve", lambda h, hh=hh: h.max_index(out=I16[:, hh, 0:8], in_max=T16[:, hh, 0:8], in_values=Ssb[:, hh, :]),
                                      reads=[b_S, b_T16s[hh]], writes=[b_I16s[hh]])
                            for k, hh in enumerate(hhs):
                                tr.op("dve", lambda h, hh=hh, k=k: h.max_index(out=I16[:, hh, 8:16], in_max=T16[:, hh, 8:16], in_values=S2s[k][:]),
                                      reads=[b_S2s[k], b_T16s[hh]], writes=[b_I16s[hh]])
                            yield
                        tr.op("dve", lambda h: h.tensor_copy(out=I16f[:], in_=I16[:]), reads=b_I16s, writes=[b_I16f])
                        t16 = T16[:]
                        in0 = _ap(t16, 0, [[32, 8], [1, 16], [0, 16]])
                        in1 = _ap(t16, 16, [[32, 8], [0, 16], [1, 16]])
                        tr.op("dve", lambda h: h.tensor_tensor(out=cand[:].rearrange("p a (b c) -> p a b c", b=16), in0=in0, in1=in1, op=ALU.add),
                              reads=b_T16s, writes=[b_cand])
                        for h4 in range(2):
                            hds = [h4 * 4 + k for k in range(4)]
                            for k, hd in enumerate(hds):
                                tr.op("dve", lambda h, hd=hd: h.max(out=C16[:, hd, 0:8], in_=cand[:, hd, :]), reads=[b_cand], writes=[b_C16s[hd]])
                            for k, hd in enumerate(hds):
                                tr.op("dve", lambda h, hd=hd, k=k: h.match_replace(out=cand2s[k][:], in_to_replace=C16[:, hd, 0:8], in_values=cand[:, hd, :], imm_value=-1e30),
                                      reads=[b_cand, b_C16s[hd]], writes=[b_cand2s[k]])
                            for k, hd in enumerate(hds):
                                tr.op("dve", lambda h, hd=hd, k=k: h.max(out=C16[:, hd, 8:16], in_=cand2s[k][:]), reads=[b_cand2s[k]], writes=[b_C16s[hd]])
                            for k, hd in enumerate(hds):
                                tr.op("dve", lambda h, hd=hd: h.max_index(out=CI[:, hd, 0:8], in_max=C16[:, hd, 0:8], in_values=cand[:, hd, :]),
                                      reads=[b_cand, b_C16s[hd]], writes=[b_CIs[hd]])
                            for k, hd in enumerate(hds):
                                tr.op("dve", lambda h, hd=hd, k=k: h.max_index(out=CI[:, hd, 8:16], in_max=C16[:, hd, 8:16], in_values=cand2s[k][:]),
                                      reads=[b_cand2s[k], b_C16s[hd]], writes=[b_CIs[hd]])
                            yield
                        c16 = C16[:]
                        tr.op("dve", lambda h: h.tensor_tensor(out=gate3, in0=c16, in1=_ap(c16, 0, [[16, 8], [0, 16]]), op=ALU.subtract),
                              reads=b_C16s, writes=[b_E12])
                        tr.op("act", lambda h: h.activation(out=E12[:, 2, :], in_=E12[:, 2, :], func=AF.Exp), reads=[b_E12], writes=[b_E12])
                        tr.op("dve", lambda h: h.tensor_reduce(out=zz[:, 0:8], in_=gate3, axis=AX.X, op=ALU.add), reads=[b_E12], writes=[b_zz])
                        tr.op("dve", lambda h: h.reciprocal(out=zz[:, 8:16], in_=zz[:, 0:8]), reads=[b_zz], writes=[b_zz])
                        tr.op("dve", lambda h: h.tensor_tensor(out=gate3, in0=gate3, in1=_ap(zz[:], 8, [[1, 8], [0, 16]]), op=ALU.mult),
                              reads=[b_E12, b_zz], writes=[b_E12])
                        cif = CI[:].rearrange("p a b -> p (a b)")
                        tr.op("dve", lambda h: h.tensor_single_scalar(out=rc[:, 0, :], in_=cif, scalar=4, op=ALU.logical_shift_right),
                              reads=b_CIs, writes=[b_rc])
                        tr.op("dve", lambda h: h.tensor_single_scalar(out=rc[:, 1, :], in_=cif, scalar=15, op=ALU.bitwise_and),
                              reads=b_CIs, writes=[b_rc])
                        tr.op("dve", lambda h: h.tensor_copy(out=rcf[:], in_=rc[:]), reads=[b_rc], writes=[b_rcf])
                        i16f = I16f[:]
                        for w in range(2):
                            tr.op("dve", lambda h, w=w: h.tensor_tensor(out=oh_flat.rearrange("p (a b) -> p a b", b=16),
                                                                        in0=_ap(rcf[:], w * 128, [[1, 128], [0, 16]]),
                                                                        in1=_ap(iota16[:], 0, [[0, 128], [1, 16]]), op=ALU.is_equal),
                                  reads=[b_rcf, b_const], writes=[b_oh])
                            tr.op("dve", lambda h, w=w: h.tensor_tensor(out=oh_flat.rearrange("p (a k b) -> p a k b", a=8, k=16),
                                                                        in0=oh_flat.rearrange("p (a k b) -> p a k b", a=8, k=16),
                                                                        in1=_ap(i16f, w * 16, [[32, 8], [0, 16], [1, 16]]), op=ALU.mult),
                                  reads=[b_oh, b_I16f], writes=[b_oh])
                            tr.op("dve", lambda h, w=w: h.tensor_reduce(out=E12[:, w, :], in_=oh_flat.rearrange("p (a b) -> p a b", b=16), axis=AX.X, op=ALU.add),
                                  reads=[b_oh], writes=[b_E12])
                        yield

                    def ggen_gen(i):
                        pb, bpb = nextbank()
                        for k in range(3):
                            tr.op("pe", lambda h, pb=pb, k=k: h.transpose(out=pb[:, k * 128:(k + 1) * 128], in_=E12[:, k, :], identity=identf[:]),
                                  reads=[b_E12, b_const], writes=[bpb], inc=(k == 2))
                        tr.op("act", lambda h, pb=pb: h.activation(out=ejT[:].rearrange("p a b -> p (a b)"), in_=pb[:, 0:384], func=AF.Copy),
                              reads=[bpb], writes=[b_ejT])
                        yield
                        iob = _ap(iotab[:], 0, [[0, 32], [1, 128]])
                        for tg in range(4):
                            k_ = xyc["i"] % 2
                            xyc["i"] += 1
                            X_, bX, Y_, bY = Xb[k_], b_Xb[k_], Yb[k_], b_Yb[k_]
                            for tl in range(32):
                                tk = tg * 32 + tl
                                tr.op("dve", lambda h, X_=X_, tl=tl, tk=tk: h.tensor_scalar(out=X_[:, tl, :], in0=iotab[:], scalar1=ejT[:, 0, tk:tk + 1],
                                                                                            scalar2=ejT[:, 2, tk:tk + 1], op0=ALU.is_equal, op1=ALU.mult),
                                      reads=[b_const, b_ejT], writes=[bXt[k_][tl]])
                                tr.op("dve", lambda h, Y_=Y_, tl=tl, tk=tk: h.tensor_scalar(out=Y_[:, tl, :], in0=iotab[:], scalar1=ejT[:, 1, tk:tk + 1],
                                                                                            scalar2=None, op0=ALU.is_equal),
                                      reads=[b_const, b_ejT], writes=[bYt[k_][tl]])
                            for t4 in range(8):
                                pg, bpg = nextbank()
                                for tt in range(4):
                                    tl = t4 * 4 + tt
                                    tr.op("pe", lambda h, pg=pg, tt=tt, tl=tl, X_=X_, Y_=Y_: h.matmul(pg[:, tt * 128:(tt + 1) * 128], lhsT=Y_[:, tl, :], rhs=X_[:, tl, :],
                                                                                                    start=True, stop=True),
                                          reads=[bXt[k_][tl], bYt[k_][tl]], writes=[bpg], inc=(tt == 3))
                                tok0 = i * 128 + tg * 32 + t4 * 4
                                tr.op("act", lambda h, pg=pg, tok0=tok0: h.activation(out=GT[:, tok0:tok0 + 4, :].rearrange("p t e -> p (t e)"),
                                                                                      in_=pg[:, :], func=AF.Copy),
                                      reads=[bpg], writes=[b_GT])
                            yield
                        yield

                    def drain(*gw):
                        gw = [list(x) for x in gw]
                        while gw:
                            for item in list(gw):
                                for _ in range(item[1]):
                                    try:
                                        next(item[0])
                                    except StopIteration:
                                        gw.remove(item)
                                        break

                    drain((topk_gen(0), 1))
                    drain((ggen_gen(0), 1), (topk_gen(1), 3))
                    drain((ggen_gen(1), 1))
                    tr.barrier()
                mid_.close()
                with ExitStack() as p3_:
                    def sbq3(name, shape, dt=F32):
                        return p3_.enter_context(nc.sbuf_tensor("%s_s%d" % (name, st), list(shape), dt))
                    UTg = [sbq3("UTg%d" % k, [128, 8192], BF16) for k in range(2)]; b_UTg = [B("UTg%d" % k) for k in range(2)]
                    Vgr = [sbq3("Vgr%d" % k, [128, 8192], BF16) for k in range(2)]; b_Vgr = [B("Vgr%d" % k) for k in range(2)]
                    gl = [sbq3("gl%d" % k, [128, TT], BF16) for k in range(2)]; b_gl = [B("gl%d" % k) for k in range(2)]
                    GA = [sbq3("GA%d" % k, [128, 4, TT], BF16) for k in range(2)]; b_GA = [B("GA%d" % k) for k in range(2)]
                    ytmp = [sbq3("ytmp%d" % k, [128, 512]) for k in range(2)]; b_ytmp = [B("ytmp%d" % k) for k in range(2)]

                    def load_u(g):
                        k_ = g % 2
                        for hh in range(2):
                            tr.dma("sp", lambda h, hh=hh: h.dma_start(out=UTg[k_][:, hh * 4096:(hh + 1) * 4096], in_=us_d[g][:, hh * 4096:(hh + 1) * 4096]),
                                   reads=[b_uvs], writes=[b_UTg[k_]])

                    def load_v(g):
                        k_ = g % 2
                        for hh in range(2):
                            tr.dma("sp", lambda h, hh=hh: h.dma_start(out=Vgr[k_][:, hh * 4096:(hh + 1) * 4096], in_=vs_d[g][:, hh * 4096:(hh + 1) * 4096]),
                                   reads=[b_uvs], writes=[b_Vgr[k_]])

                    glc = {"i": 0}
                    ycnt = {"i": 0}

                    def a_stage(g):
                        k_ = g % 2
                        ut, but, ga_, bga = UTg[k_], b_UTg[k_], GA[k_], b_GA[k_]
                        for c4 in range(4):
                            c = g * 4 + c4
                            pa, bpa = nextbank(0, 4)
                            for dc in range(16):
                                tr.op("pe", lambda h, dc=dc, pa=pa, c4=c4: h.matmul(pa[:, 0:TT], lhsT=ut[:, dc * 512 + c4 * 128: dc * 512 + (c4 + 1) * 128],
                                                                                   rhs=hT[:, dc, :], start=(dc == 0), stop=(dc == 15)),
                                      reads=[but, b_hT], writes=[bpa], inc=(dc == 15))
                            gl_, bgl = gl[glc["i"] % 2], b_gl[glc["i"] % 2]
                            glc["i"] += 1
                            tr.op("act", lambda h, pa=pa, gl_=gl_: h.activation(out=gl_[:], in_=pa[:, 0:TT], func=AF.Gelu), reads=[bpa], writes=[bgl])
                            tr.op("dve", lambda h, gl_=gl_, c4=c4, c=c: h.tensor_tensor(out=ga_[:, c4, :], in0=gl_[:], in1=GT[:, :, c], op=ALU.mult),
                                  reads=[bgl, b_GT], writes=[bga])
                        if g + 2 < 32:
                            load_u(g + 2)

                    def y_stage(g):
                        k_ = g % 2
                        vg, bvg, ga_, bga = Vgr[k_], b_Vgr[k_], GA[k_], b_GA[k_]
                        for i in range(NI):
                            for dt_ in range(4):
                                py, bpy = nextbank(4, 8)
                                for c4 in range(4):
                                    tr.op("pe", lambda h, py=py, c4=c4, i=i, dt_=dt_: h.matmul(py[:, :], lhsT=ga_[:, c4, i * 128:(i + 1) * 128],
                                                                                              rhs=vg[:, c4 * 2048 + dt_ * 512: c4 * 2048 + (dt_ + 1) * 512],
                                                                                              start=(c4 == 0), stop=(c4 == 3)),
                                          reads=[bga, bvg], writes=[bpy], inc=(c4 == 3))
                                xs = xres[:, i, dt_ * 512:(dt_ + 1) * 512]
                                yc = ycnt["i"]
                                ycnt["i"] += 1
                                if yc % 2 == 0:
                                    tr.op("dve", lambda h, py=py, xs=xs: h.tensor_tensor(out=xs, in0=py[:, :], in1=xs, op=ALU.add),
                                          reads=[bpy, b_xres[i]], writes=[b_xres[i]])
                                else:
                                    yt, byt = ytmp[(yc // 2) % 2], b_ytmp[(yc // 2) % 2]
                                    tr.op("act", lambda h, py=py, yt=yt: h.activation(out=yt[:], in_=py[:, :], func=AF.Copy), reads=[bpy], writes=[byt])
                                    tr.op("pool", lambda h, yt=yt, xs=xs: h.tensor_tensor(out=xs, in0=yt[:], in1=xs, op=ALU.add),
                                          reads=[byt, b_xres[i]], writes=[b_xres[i]])
                        if g + 2 < 32:
                            load_v(g + 2)

                    load_u(0)
                    load_u(1)
                    load_v(0)
                    load_v(1)

                    def prefetch_x():
                        if st + 1 < N_SUPER:
                            for i in range(NI):
                                tr.dma("sp", lambda h, i=i: h.dma_start(out=xres2[:, (1 - par_) * NI + i, :],
                                                                        in_=x_d[t0 + TT + i * 128: t0 + TT + (i + 1) * 128, :]),
                                       writes=[b_xres2[1 - par_][i]])
                    a_stage(0)
                    for g in range(32):
                        if g + 1 < 32:
                            a_stage(g + 1)
                        y_stage(g)
                        if g == 4:
                            prefetch_x()
                    for i in range(NI):
                        tr.dma("sp", lambda h, i=i: h.dma_start(out=out_d[t0 + i * 128: t0 + (i + 1) * 128, :], in_=xres[:, i, :]),
                               reads=[b_xres[i]], writes=[b_out], sembuf=b_xres[i])
                    tr.barrier()
        tr.wait_all("sp", [b_out])
    return nc, tr.ninst


def _consts():
    identf = np.eye(128, dtype=np.float32)
    blk = np.arange(128) // 64
    bones = (blk[:, None] == blk[None, :]).astype(np.float32) / 64.0
    P = np.zeros((128, 128), np.float32)
    for hb in (0, 64):
        for i in range(8):
            P[hb + i, hb + i + 8] = -1.0
            P[hb + i + 8, hb + i] = 1.0
    ropepT = np.ascontiguousarray(P.T)
    kk = np.arange(128)[:, None]
    qq = np.arange(128)[None, :]
    mprev = (kk > qq).astype(np.float32)
    mcur = (kk <= qq).astype(np.float32)
    pos = np.arange(S, dtype=np.float32)
    inv_freq = (np.float32(500000.0) ** (-np.arange(0, 16, 2, dtype=np.float32) / np.float32(16))).astype(np.float32)
    ang = (pos[:, None] * inv_freq[None, :]).astype(np.float32)
    cos8 = np.cos(ang).astype(np.float32).T
    sin8 = np.sin(ang).astype(np.float32).T
    cosT = np.ones((128, S), np.float32)
    sinT = np.zeros((128, S), np.float32)
    for hb in (0, 64):
        cosT[hb:hb + 8] = cos8
        cosT[hb + 8:hb + 16] = cos8
        sinT[hb:hb + 8] = sin8
        sinT[hb + 8:hb + 16] = sin8
    iota128 = np.tile(np.arange(128, dtype=np.float32)[None, :], (128, 1))
    return dict(identf=identf, bones=bones, ropepT=ropepT, mprev=mprev, mcur=mcur, cosT=cosT, sinT=sinT, iota128=iota128)


def _layout_shared(w_ada, b_ada, g_norm1, w_in, g_q, g_k, sinks, conv_w, g_out_attn, g_out_conv,
                   w_out, g_norm2, w_pq, peer_subkeys, peer_u, peer_v):
    f = lambda a: np.ascontiguousarray(np.asarray(a, dtype=np.float32))
    col16 = lambda v: f(np.asarray(v).reshape(16, 128).T)
    w_in = np.asarray(w_in)
    q = w_in[:, 0:1024]
    k = w_in[:, 1024:1280]
    v = w_in[:, 1280:1536]
    bg = w_in[:, 1536:2560]
    cg = w_in[:, 2560:3584]
    hc = w_in[:, 3584:4608]
    kd = np.concatenate([k[:, h * 64:(h + 1) * 64] for h in range(4) for _ in range(2)], axis=1)
    vd = np.concatenate([v[:, h * 64:(h + 1) * 64] for h in range(4) for _ in range(2)], axis=1)
    w_in2 = f(np.concatenate([q, kd, vd, cg, hc, bg], axis=1))
    subkT = f(np.asarray(peer_subkeys).reshape(16, 128, 128).transpose(2, 0, 1).reshape(128, 16 * 128))
    convw = f(np.asarray(conv_w).reshape(3, 8, 128).transpose(2, 1, 0).reshape(128, 24))
    d = dict(
        w_ada=f(w_ada), b_ada=f(np.asarray(b_ada).reshape(1, -1)), g1col=col16(g_norm1), g2col=col16(g_norm2),
        w_in2=w_in2,
        gqcol=f(np.tile(np.asarray(g_q), 2).reshape(128, 1)), gkcol=f(np.tile(np.asarray(g_k), 2).reshape(128, 1)),
        sinks=f(np.asarray(sinks).reshape(1, 16)), convw=convw,
        gattn=f(np.asarray(g_out_attn).reshape(8, 128).T), gconv=f(np.asarray(g_out_conv).reshape(8, 128).T),
        w_out=f(w_out), w_pq=f(w_pq), subkT=subkT, peer_uT=f(np.asarray(peer_u).T), peer_v=f(peer_v),
    )
    d.update(_consts())
    return d


def kernel(x, c, w_ada, b_ada, g_norm1, w_in, g_q, g_k, sinks, conv_w, g_out_attn, g_out_conv,
           w_out, g_norm2, w_pq, peer_subkeys, peer_u, peer_v, _cores=None):
    x = np.asarray(x, dtype=np.float32)
    c = np.asarray(c, dtype=np.float32)
    shared = _layout_shared(w_ada, b_ada, g_norm1, w_in, g_q, g_k, sinks, conv_w, g_out_attn, g_out_conv,
                            w_out, g_norm2, w_pq, peer_subkeys, peer_u, peer_v)
    cores = list(range(8)) if _cores is None else list(_cores)
    nc, _ = build_program()
    in_maps = []
    for b in cores:
        m = dict(shared)
        m["x"] = np.ascontiguousarray(x[b])
        m["ccol"] = np.ascontiguousarray(c[b].reshape(16, 128).T)
        in_maps.append(m)
    res = run_bass_kernel_spmd(nc, in_maps, core_ids=list(range(len(cores))))
    outs = [np.asarray(r["out"], dtype=np.float32) for r in res.results]
    if _cores is not None:
        return outs
    return np.stack(outs, axis=0)
```

```python
import numpy as np
from contextlib import ExitStack
import concourse.bass as bass
import concourse.mybir as mybir
from concourse.bass_utils import run_bass_kernel_spmd

F32 = mybir.dt.float32
BF16 = mybir.dt.bfloat16
U32 = mybir.dt.uint32
I32 = mybir.dt.int32
ALU = mybir.AluOpType
AF = mybir.ActivationFunctionType
AX = mybir.AxisListType

S = 4096
D = 2048
TT = 256
NST = S // TT
NI = TT // 128
EPS = 1e-6
NGRP = 18
INC = 5120

DEBUG_STAGE = None
N_SUPER = NST


class Buf:
    __slots__ = ("name", "w", "r", "dsem", "dcnt")

    def __init__(self, name):
        self.name = name
        self.w = None
        self.r = []
        self.dsem = None
        self.dcnt = 0


class Tracker:
    def __init__(self, nc, es):
        self.nc = nc
        self.es = es
        self.eng = {}
        for name, h in (("pe", nc.tensor), ("act", nc.scalar), ("dve", nc.vector),
                        ("pool", nc.gpsimd), ("sp", nc.sync)):
            sem = es.enter_context(nc.semaphore("sem_" + name))
            self.eng[name] = {"h": h, "sem": sem, "cnt": 0, "seen": {}, "name": name}
        self.bufs = {}
        self.dsems = {}
        self.ninst = 0

    def buf(self, name):
        b = Buf(name)
        self.bufs[name] = b
        return b

    def _wait(self, e, toks):
        best = {}
        for t in toks:
            if t is None:
                continue
            sem, val = t
            k = id(sem)
            if e["seen"].get(k, 0) >= val:
                continue
            if k not in best or best[k][1] < val:
                best[k] = (sem, val)
        for k, (sem, val) in best.items():
            if e["name"] == "pe" and sem is e["sem"]:
                continue
            e["h"].wait_ge(sem, val)
            e["seen"][k] = val
            self.ninst += 1

    @staticmethod
    def _deps(reads, writes):
        toks = []
        for b in reads:
            toks.append(b.w)
        for b in writes:
            toks.append(b.w)
            toks.extend(b.r)
        return toks

    def op(self, en, fn, reads=(), writes=(), inc=True):
        e = self.eng[en]
        self._wait(e, self._deps(reads, writes))
        ins = fn(e["h"])
        self.ninst += 1
        if inc:
            e["cnt"] += 1
            ins.then_inc(e["sem"], 1)
            tok = (e["sem"], e["cnt"])
        else:
            tok = (e["sem"], e["cnt"] + 1)
        for b in reads:
            b.r.append(tok)
        for b in writes:
            b.w = tok
            b.r = []
        return tok

    def dma(self, en, fn, reads=(), writes=(), sembuf=None):
        e = self.eng[en]
        sb = sembuf if sembuf is not None else (writes[0] if writes else reads[0])
        if sb.dsem is None:
            if sb.name not in self.dsems:
                self.dsems[sb.name] = [self.es.enter_context(self.nc.semaphore("ds_" + sb.name)), 0]
            sb.dsem = self.dsems[sb.name][0]
            sb.dcnt = self.dsems[sb.name][1]
        toks = [t for t in self._deps(reads, writes) if not (t is not None and t[0] is sb.dsem)]
        self._wait(e, toks)
        ins = fn(e["h"])
        self.ninst += 1
        sb.dcnt += 16
        self.dsems[sb.name][1] = sb.dcnt
        ins.then_inc(sb.dsem, 16)
        tok = (sb.dsem, sb.dcnt)
        for b in reads:
            b.r.append(tok)
        for b in writes:
            b.w = tok
            b.r = []
        return tok

    def wait_all(self, en, bufs):
        e = self.eng[en]
        toks = []
        for b in bufs:
            toks.append(b.w)
            toks.extend(b.r)
        self._wait(e, toks)

    def barrier(self):
        sp = self.eng["sp"]
        toks = []
        for n, e in self.eng.items():
            if n != "sp" and e["cnt"] > 0:
                toks.append((e["sem"], e["cnt"]))
        for nm, (dsem, dcnt) in self.dsems.items():
            if dcnt > 0:
                toks.append((dsem, dcnt))
        self._wait(sp, toks)
        sp["cnt"] += 1
        sp["h"].nop().then_inc(sp["sem"], 1)
        self.ninst += 1
        tok = (sp["sem"], sp["cnt"])
        for n, e in self.eng.items():
            if n != "sp":
                self._wait(e, [tok])
        for b in self.bufs.values():
            b.w = None
            b.r = []


def _ap(src, offset, dims):
    return bass.AP(src.tensor, src.offset + offset, [list(src.ap[0])] + [list(d) for d in dims])


def build_program():
    nc = bass.Bass("TRN2", target_bir_lowering=False)

    def din(name, shape, dt=F32):
        return nc.dram_tensor(name, list(shape), dt, kind="ExternalInput")

    x_d = din("x", [S, D])
    ccol_d = din("ccol", [128, 16])
    wada_d = din("w_ada", [D, 6 * D])
    bada_d = din("b_ada", [1, 6 * D])
    g1col_d = din("g1col", [128, 16])
    g2col_d = din("g2col", [128, 16])
    win_d = din("w_in2", [D, INC])
    gq_d = din("gqcol", [128, 1])
    gk_d = din("gkcol", [128, 1])
    sinks_d = din("sinks", [1, 16])
    convw_d = din("convw", [128, 24])
    gattn_d = din("gattn", [128, 8])
    gconv_d = din("gconv", [128, 8])
    wout_d = din("w_out", [D, D])
    wpq_d = din("w_pq", [D, D])
    subk_d = din("subkT", [128, 16 * 128])
    put_d = din("peer_uT", [D, 16384])
    pv_d = din("peer_v", [16384, D])
    identf_d = din("identf", [128, 128])
    bones_d = din("bones", [128, 128])
    ropep_d = din("ropepT", [128, 128])
    mprev_d = din("mprev", [128, 128])
    mcur_d = din("mcur", [128, 128])
    cos_d = din("cosT", [128, S])
    sin_d = din("sinT", [128, S])
    iota_d = din("iota128", [128, 128])
    out_d = nc.dram_tensor("out", [S, D], F32, kind="ExternalOutput")
    wsc_d = nc.dram_tensor("wsc", [NGRP, 128, 16 * 512], BF16)
    us_d = nc.dram_tensor("us", [32, 128, 8192], BF16)
    gt1_d = nc.dram_tensor("gt1s", [128, D], F32)
    vs_d = nc.dram_tensor("vs", [32, 128, 8192], BF16)

    with ExitStack() as es:
        def sb(name, shape, dt=F32):
            return es.enter_context(nc.sbuf_tensor(name, list(shape), dt))

        tr = Tracker(nc, es)
        B = tr.buf

        xres2 = sb("xres", [128, 2 * NI, D])
        b_xres2 = [[B("xres%d_%d" % (p_, i)) for i in range(NI)] for p_ in range(2)]
        xres = xres2[:, 0:NI, :]; b_xres = b_xres2[0]
        b_rows = B("rows")
        cols = sb("cols", [128, 4, 16]); b_cols = B("cols")
        identb = sb("identb", [128, 128], BF16); bonesb = sb("bonesb", [128, 128], BF16)
        ropepb = sb("ropepb", [128, 128], BF16); mprevb = sb("mprevb", [128, 128], BF16)
        mcurb = sb("mcurb", [128, 128], BF16); onesb = sb("onesb", [128, 128], BF16)
        onesf = sb("onesf", [1, 128]); iota16 = sb("iota16s", [128, 16]); iotab = sb("iotab", [128, 128], BF16); identf = sb("identf_s", [128, 128])
        b_const = B("const")
        gqk = sb("gqk", [128, 2]); convw = sb("convw_s", [128, 24]); gattn = sb("gattn_s", [128, 8])
        gconv = sb("gconv_s", [128, 8]); esink = sb("esink", [128, 16]); subkb = sb("subkb", [128, 16, 128], BF16)
        b_par = B("par")
        kT = sb("kT", [128, 4, 2, 128 + TT], BF16); b_kT = B("kT")
        vtok = sb("vtok", [128, NI + 1, 512], BF16); b_vtok = B("vtok")
        ubuf = sb("ubuf", [128, 8, 2 + TT]); b_u = B("ubuf")
        stat = sb("stat", [128, 8]); b_stat = B("stat")
        epsc = sb("epsc", [128, 1])

        pbank = [es.enter_context(nc.psum_tensor("pb%d" % i, [128, 512], F32)) for i in range(8)]
        b_pb = [B("pb%d" % i) for i in range(8)]
        rr = {"i": 0}

        def nextbank(lo=0, hi=8):
            i = lo + (rr["i"] % (hi - lo))
            rr["i"] += 1
            return pbank[i], b_pb[i]

        es.enter_context(nc.Block())
        b_out = B("out")
        b_wsc1 = B("wsc"); b_wsc = [b_wsc1] * NGRP
        b_uvs = B("uvs")

        with ExitStack() as ps:
            def sbp(name, shape, dt=F32):
                return ps.enter_context(nc.sbuf_tensor(name, list(shape), dt))
            NSTG = 3
            stage = [sbp("stage%d" % i, [128, 8, 512]) for i in range(NSTG)]
            b_stage = [B("stage%d" % i) for i in range(NSTG)]
            cvt = [sbp("cvt%d" % i, [128, 8 * 512], BF16) for i in range(2)]
            b_cvt = [B("cvt%d" % i) for i in range(2)]
            ccol = sbp("ccol_s", [128, 16]); b_ccol = B("ccol")
            ctmp = sbp("ctmp", [128, 128]); b_ctmp = B("ctmp")
            g12 = sbp("g12", [128, 2, 16]); b_g12 = B("g12")
            subkf = sbp("subkf", [128, 16 * 128]); b_subkf = B("subkf")
            gt2b = sbp("gt2b", [128, D]); b_gt2 = B("gt2b")
            gt1b = sbp("gt1b_p", [128, D]); b_gt1s = B("gt1s")
            pm_ = ExitStack()
            modrow = pm_.enter_context(nc.sbuf_tensor("modrow", [1, 6 * D], F32)); b_mod = B("modrow")

            sp_loads = [
                (ccol[:], ccol_d.ap(), b_ccol), (modrow[:], bada_d.ap(), b_mod),
                (g12[:, 0, :], g1col_d.ap(), b_g12), (g12[:, 1, :], g2col_d.ap(), b_g12),
                (gqk[:, 0:1], gq_d.ap(), b_par), (gqk[:, 1:2], gk_d.ap(), b_par),
                (convw[:], convw_d.ap(), b_par), (gattn[:], gattn_d.ap(), b_par),
                (gconv[:], gconv_d.ap(), b_par), (iota16[:], iota_d[:, 0:16], b_const), (identf[:], identf_d.ap(), b_const),
                (esink[:], bass.AP(sinks_d, 0, [[0, 128], [1, 16]]), b_par), (subkf[:], subk_d.ap(), b_subkf),
            ]
            for o, i_, bb in sp_loads:
                tr.dma("sp", lambda h, o=o, i_=i_: h.dma_start(out=o, in_=i_), writes=[bb])
            for k, (src_d, dst) in enumerate([(identf_d, identb), (bones_d, bonesb), (ropep_d, ropepb),
                                              (mprev_d, mprevb), (mcur_d, mcurb)]):
                tr.dma("sp", lambda h, s=src_d: h.dma_start(out=ctmp[:], in_=s.ap()), writes=[b_ctmp])
                tr.op("dve", lambda h, d=dst: h.tensor_copy(out=d[:], in_=ctmp[:]), reads=[b_ctmp], writes=[b_const])
            tr.op("dve", lambda h: h.memset(onesb[:], 1.0), writes=[b_const])
            tr.op("dve", lambda h: h.memset(epsc[:], EPS), writes=[b_const])
            tr.dma("sp", lambda h: h.dma_start(out=ctmp[:], in_=iota_d.ap()), writes=[b_ctmp])
            tr.op("dve", lambda h: h.tensor_copy(out=iotab[:], in_=ctmp[:]), reads=[b_ctmp], writes=[b_const])
            tr.op("dve", lambda h: h.memset(onesf[:], 1.0), writes=[b_const])
            tr.op("dve", lambda h: h.tensor_copy(out=subkb[:].rearrange("p a b -> p (a b)"), in_=subkf[:]),
                  reads=[b_subkf], writes=[b_par])
            tr.op("act", lambda h: h.activation(out=esink[:], in_=esink[:], func=AF.Exp), reads=[b_par], writes=[b_par])
            tr.op("dve", lambda h: h.tensor_scalar(out=esink[:], in0=esink[:], scalar1=1e-3, scalar2=None, op0=ALU.mult), reads=[b_par], writes=[b_par])
            tr.op("act", lambda h: h.activation(out=ccol[:], in_=ccol[:], func=AF.Silu), reads=[b_ccol], writes=[b_ccol])

            if DEBUG_STAGE == "p1":
                tr.barrier()
                return nc, tr.ninst
            ntile = 0
            for t in range(24):
                pb, bpb = nextbank()
                for hh in range(2):
                    sl = ntile % NSTG
                    ntile += 1
                    src = wada_d.ap().rearrange("(c p) n -> p c n", p=128)[:, hh * 8:(hh + 1) * 8, t * 512:(t + 1) * 512]
                    tr.dma("sp", lambda h, sl=sl, src=src: h.dma_start(out=stage[sl][:], in_=src), writes=[b_stage[sl]])
                    for c8 in range(8):
                        c = hh * 8 + c8
                        tr.op("pe", lambda h, c=c, c8=c8, pb=pb, sl=sl: h.matmul(pb[0:1, :], lhsT=ccol[:, c:c + 1], rhs=stage[sl][:, c8, :],
                                                                                 start=(c == 0), stop=(c == 15)),
                              reads=[b_ccol, b_stage[sl]], writes=[bpb], inc=(c8 == 7))
                tr.op("dve", lambda h, pb=pb, t=t: h.tensor_tensor(out=modrow[0:1, t * 512:(t + 1) * 512], in0=pb[0:1, :],
                                                                   in1=modrow[0:1, t * 512:(t + 1) * 512], op=ALU.add),
                      reads=[bpb, b_mod], writes=[b_mod])
            if DEBUG_STAGE == "p2":
                tr.barrier()
                return nc, tr.ninst
            for dst, sec, bdst in ((gt1b, 2, b_rows), (gt2b, 5, b_gt2)):
                for dg in range(4):
                    pb, bpb = nextbank()
                    tr.op("pe", lambda h, pb=pb, sec=sec, dg=dg: h.matmul(pb[:, :], lhsT=onesf[0:1, :],
                                                                          rhs=modrow[0:1, sec * D + dg * 512: sec * D + (dg + 1) * 512],
                                                                          start=True, stop=True),
                          reads=[b_mod, b_const], writes=[bpb])
                    tr.op("act", lambda h, pb=pb, dst=dst, dg=dg: h.activation(out=dst[:, dg * 512:(dg + 1) * 512], in_=pb[:, :], func=AF.Copy),
                          reads=[bpb], writes=[bdst])
            tr.dma("sp", lambda h: h.dma_start(out=gt1_d.ap(), in_=gt1b[:]), reads=[b_rows], writes=[b_gt1s], sembuf=b_rows)
            pb, bpb = nextbank()
            for k, sec in enumerate((0, 1, 3, 4)):
                for c in range(16):
                    tr.op("pe", lambda h, pb=pb, k=k, c=c, sec=sec: h.matmul(
                        pb[:, (k * 16 + c) * 2:(k * 16 + c) * 2 + 2], lhsT=modrow[0:1, sec * D + c * 128: sec * D + (c + 1) * 128],
                        rhs=onesf[0:1, 0:2], start=True, stop=True), reads=[b_mod, b_const], writes=[bpb],
                        inc=(k == 3 and c == 15))
            pbv = pb[:, 0:128].rearrange("p (k c two) -> p k c two", k=4, c=16, two=2)
            tr.op("dve", lambda h: h.tensor_copy(out=cols[:], in_=pbv[:, :, :, 0]), reads=[bpb], writes=[b_cols])
            for k, gi in ((1, 0), (3, 1)):
                tr.op("dve", lambda h, k=k, gi=gi: h.scalar_tensor_tensor(out=cols[:, k, :], in0=cols[:, k, :], scalar=1.0,
                                                                         in1=g12[:, gi, :], op0=ALU.add, op1=ALU.mult),
                      reads=[b_cols, b_g12], writes=[b_cols])

            if DEBUG_STAGE == "p3":
                tr.barrier()
                return nc, tr.ninst
            tr.barrier()
            pm_.close()
            for k_ in range(3):
                stage.append(sbp("stage%d" % (NSTG + k_), [128, 8, 512])); b_stage.append(B("stage%d" % (NSTG + k_)))
            NSTG = 6
            gsrc = []
            for g in range(10):
                gsrc.append(win_d.ap().rearrange("(c p) n -> p c n", p=128)[:, :, g * 512:(g + 1) * 512])
            for g in range(4):
                gsrc.append(wout_d.ap().rearrange("(c p) n -> p c n", p=128)[:, :, g * 512:(g + 1) * 512])
            for g in range(4):
                gsrc.append(wpq_d.ap().rearrange("(c p) n -> p c n", p=128)[:, :, g * 512:(g + 1) * 512])
            utv = put_d.ap().rearrange("(c p) n -> p c n", p=128)
            vv = pv_d.ap().rearrange("(g c p) d -> g p c d", c=4, p=128)
            jobs = []
            for g in range(NGRP):
                for hh in range(2):
                    jobs.append((gsrc[g][:, hh * 8:(hh + 1) * 8, :], False, wsc_d[g][:, hh * 4096:(hh + 1) * 4096], b_wsc[g]))
            for g in range(32):
                for hh in range(2):
                    jobs.append((utv[:, hh * 8:(hh + 1) * 8, g * 512:(g + 1) * 512], False, us_d[g][:, hh * 4096:(hh + 1) * 4096], b_uvs))
            for g in range(32):
                for hh in range(2):
                    jobs.append((vv[g][:, hh * 2:(hh + 1) * 2, :], True, vs_d[g][:, hh * 4096:(hh + 1) * 4096], b_uvs))
            base = ntile

            def job_in(k):
                src, is_v, dst, bd = jobs[k]
                sl = (base + k) % NSTG
                o = stage[sl][:].rearrange("p c n -> p (c n)").rearrange("p (c d) -> p c d", c=2) if is_v else stage[sl][:]
                tr.dma("sp", lambda h: h.dma_start(out=o, in_=src), writes=[b_stage[sl]])

            for k in range(min(NSTG, len(jobs))):
                job_in(k)
            for k in range(len(jobs)):
                src, is_v, dst, bd = jobs[k]
                sl = (base + k) % NSTG
                cs = k % 2
                stf = stage[sl][:].rearrange("p c n -> p (c n)")
                if is_v:
                    tr.op("dve", lambda h: h.tensor_tensor(out=cvt[cs][:, 0:2048], in0=stf[:, 0:2048], in1=gt2b[:], op=ALU.mult),
                          reads=[b_stage[sl], b_gt2], writes=[b_cvt[cs]])
                    tr.op("pool", lambda h: h.tensor_tensor(out=cvt[cs][:, 2048:4096], in0=stf[:, 2048:4096], in1=gt2b[:], op=ALU.mult),
                          reads=[b_stage[sl], b_gt2], writes=[b_cvt[cs]])
                else:
                    tr.op("act", lambda h: h.activation(out=cvt[cs][:, 0:2048], in_=stf[:, 0:2048], func=AF.Copy),
                          reads=[b_stage[sl]], writes=[b_cvt[cs]])
                    tr.op("dve", lambda h: h.tensor_copy(out=cvt[cs][:, 2048:4096], in_=stf[:, 2048:4096]),
                          reads=[b_stage[sl]], writes=[b_cvt[cs]])
                if k + NSTG < len(jobs):
                    job_in(k + NSTG)
                tr.dma("sp", lambda h: h.dma_start(out=dst, in_=cvt[cs][:]), reads=[b_cvt[cs]], writes=[bd], sembuf=b_cvt[cs])
            tr.op("dve", lambda h: h.memset(kT[:], 0.0), writes=[b_kT])
            tr.op("dve", lambda h: h.memset(vtok[:], 0.0), writes=[b_vtok])
            tr.op("dve", lambda h: h.memset(ubuf[:], 0.0), writes=[b_u])
            tr.barrier()

        def load_w(g, wb, bwb):
            for hh in range(2):
                tr.dma("sp", lambda h, hh=hh: h.dma_start(out=wb[:, hh * 4096:(hh + 1) * 4096], in_=wsc_d[g][:, hh * 4096:(hh + 1) * 4096]),
                       reads=[b_wsc[g]], writes=[bwb])

        def rstd_of(ssq_ap, out_ap, scale, bufs_r, bufs_w):
            tr.op("act", lambda h: h.activation(out=out_ap, in_=ssq_ap, func=AF.Ln, scale=scale, bias=epsc[:]),
                  reads=list(bufs_r) + [b_const], writes=bufs_w)
            tr.op("act", lambda h: h.activation(out=out_ap, in_=out_ap, func=AF.Exp, scale=-0.5), reads=bufs_w, writes=bufs_w)

        def norm_transpose(xn, b_xn, hT, b_hT, kcol_shift, kcol_scale, stat_off):
            for i in range(NI):
                tr.op("act", lambda h, i=i: h.activation(out=xn[:, i, :], in_=xres[:, i, :], func=AF.Square,
                                                         accum_out=stat[:, stat_off + i:stat_off + i + 1]),
                      reads=[b_xres[i]], writes=[b_xn, b_stat])
            rstd_of(stat[:, stat_off:stat_off + NI], stat[:, stat_off + 2:stat_off + 2 + NI], 1.0 / D, [b_stat], [b_stat])
            for i in range(NI):
                tr.op("act", lambda h, i=i: h.activation(out=xn[:, i, :], in_=xres[:, i, :], func=AF.Copy,
                                                         scale=stat[:, stat_off + 2 + i:stat_off + 3 + i]),
                      reads=[b_xres[i], b_stat], writes=[b_xn])
            for c in range(16):
                pb, bpb = nextbank()
                pbb = pb[:].bitcast(BF16)
                for i in range(NI):
                    tr.op("pe", lambda h, i=i, c=c, pbb=pbb: h.transpose(out=pbb[:, i * 128:(i + 1) * 128],
                                                                         in_=xn[:, i, c * 128:(c + 1) * 128], identity=identb[:]),
                          reads=[b_xn, b_const], writes=[bpb], inc=(i == NI - 1))
                tr.op("dve", lambda h, c=c, pbb=pbb: h.tensor_scalar(out=hT[:, c, :], in0=pbb[:, 0:TT],
                                                                     scalar1=cols[:, kcol_scale, c:c + 1], scalar2=cols[:, kcol_shift, c:c + 1],
                                                                     op0=ALU.mult, op1=ALU.add),
                      reads=[bpb, b_cols], writes=[b_hT])

        for st in range(N_SUPER):
            t0 = st * TT
            with ExitStack() as pa:
                def sba(name, shape, dt=F32):
                    return pa.enter_context(nc.sbuf_tensor("%s_a%d" % (name, st), list(shape), dt))
                xn = sba("xn", [128, NI, D], BF16); b_xn = B("xn")
                hT = sba("hT", [128, 16, TT], BF16); b_hT = B("hT")
                wbuf = [sba("wbuf%d" % i, [128, 8192], BF16) for i in range(2)]
                b_wbuf = [B("wbuf%d" % i) for i in range(2)]
                cst = sba("cst", [128, 2, TT]); b_cst = B("cst")
                qT = sba("qT", [128, 8, TT], BF16); b_qT = B("qT")
                cgs = sba("cgs", [128, 8, TT]); b_cgs = B("cgs")
                mixT = sba("mixT", [128, 16, TT], BF16); b_mixT = B("mixT")
                Pt = [sba("Pt%d" % i, [128, 512], BF16) for i in range(4)]
                gt1b = sba("gt1b", [128, D])
                b_Pt = [B("Pt%d" % i) for i in range(4)]

                par_ = st % 2
                xres = xres2[:, par_ * NI:(par_ + 1) * NI, :]; b_xres = b_xres2[par_]
                if st == 0 or DEBUG_STAGE == "A":
                    for i in range(NI):
                        tr.dma("sp", lambda h, i=i: h.dma_start(out=xres[:, i, :], in_=x_d[t0 + i * 128: t0 + (i + 1) * 128, :]),
                               writes=[b_xres[i]])
                load_w(0, wbuf[0], b_wbuf[0])
                load_w(1, wbuf[1], b_wbuf[1])
                tr.dma("sp", lambda h: h.dma_start(out=cst[:, 0, :], in_=cos_d[:, t0:t0 + TT]), writes=[b_cst])
                tr.dma("sp", lambda h: h.dma_start(out=cst[:, 1, :], in_=sin_d[:, t0:t0 + TT]), writes=[b_cst])
                tr.dma("sp", lambda h: h.dma_start(out=gt1b[:], in_=gt1_d.ap()), reads=[b_gt1s], writes=[b_rows])
                norm_transpose(xn, b_xn, hT, b_hT, 0, 1, 0)

                if DEBUG_STAGE == "a1":
                    tr.barrier()
                    return nc, tr.ninst
                tmpc = {"i": 0}
                T2 = {}
                for nm, shp, dt_ in (("sq", [128, 512], BF16), ("r", [128, 512], F32), ("a", [128, 512], F32), ("b", [128, 512], F32), ("qn", [128, TT], BF16)):
                    T2[nm] = [(sba("t2%s%d" % (nm, k), shp, dt_), B("t2%s%d" % (nm, k))) for k in range(4)]

                def tmps():
                    k = tmpc["i"] % 4
                    tmpc["i"] += 1
                    return {nm: T2[nm][k] for nm in T2}

                pgc = {"i": 0}

                def proj_group(g):
                    wb, bwb = wbuf[g % 2], b_wbuf[g % 2]
                    bs = 2 * (pgc["i"] % 2)
                    pgc["i"] += 1
                    outs = []
                    for ch in range(4):
                        pbk, bpbk = pbank[bs + ch // 2], b_pb[bs + ch // 2]
                        pv = pbk[:, (ch % 2) * TT:(ch % 2 + 1) * TT]
                        for c in range(16):
                            tr.op("pe", lambda h, c=c, pv=pv, ch=ch: h.matmul(pv, lhsT=wb[:, c * 512 + ch * 128: c * 512 + (ch + 1) * 128],
                                                                             rhs=hT[:, c, :], start=(c == 0), stop=(c == 15)),
                                  reads=[bwb, b_hT], writes=[bpbk], inc=(c == 15))
                        outs.append((pv, bpbk))
                    if g + 2 < 14:
                        load_w(g + 2, wbuf[g % 2], b_wbuf[g % 2])
                    return outs

                def halfbank(k, ch):
                    pbk, bpbk = pbank[k + ch // 2], b_pb[k + ch // 2]
                    return pbk[:, (ch % 2) * TT:(ch % 2 + 1) * TT], bpbk

                def rstd_stages(srcs, Ts, n):
                    for (src_ap, b_src), T in zip(srcs, Ts):
                        (tsq, btsq) = T["sq"]
                        tr.op("act", lambda h, tsq=tsq, src_ap=src_ap: h.activation(out=tsq[:, 0:n], in_=src_ap, func=AF.Square),
                              reads=[b_src], writes=[btsq])
                    pbs = []
                    for ch, T in enumerate(Ts):
                        (tsq, btsq) = T["sq"]
                        pv2, bpb2 = halfbank(4, ch)
                        tr.op("pe", lambda h, tsq=tsq, pv2=pv2: h.matmul(pv2, lhsT=bonesb[:], rhs=tsq[:, 0:n], start=True, stop=True),
                              reads=[btsq, b_const], writes=[bpb2])
                        pbs.append((pv2, bpb2))
                    for (pv2, bpb2), T in zip(pbs, Ts):
                        (tr_, btr) = T["r"]
                        tr.op("act", lambda h, tr_=tr_, pv2=pv2: h.activation(out=tr_[:, 0:n], in_=pv2, func=AF.Ln, bias=epsc[:]),
                              reads=[bpb2, b_const], writes=[btr])
                    for T in Ts:
                        (tr_, btr) = T["r"]
                        tr.op("act", lambda h, tr_=tr_: h.activation(out=tr_[:, 0:n], in_=tr_[:, 0:n], func=AF.Exp, scale=-0.5), reads=[btr], writes=[btr])

                def post_qk(g, outs):
                    Ts = [tmps() for _ in range(4)]
                    rstd_stages(outs, Ts, TT)
                    gcol = gqk[:, 0:1] if g < 2 else gqk[:, 1:2]
                    for (pv, bpb), T in zip(outs, Ts):
                        (tr_, btr), (tqn, btqn) = T["r"], T["qn"]
                        tr.op("dve", lambda h, pv=pv, tr_=tr_, tqn=tqn: h.scalar_tensor_tensor(out=tqn[:], in0=pv, scalar=gcol, in1=tr_[:, 0:TT],
                                                                                             op0=ALU.mult, op1=ALU.mult),
                              reads=[bpb, btr, b_par], writes=[btqn])
                    rps = []
                    for ch, T in enumerate(Ts):
                        (tqn, btqn) = T["qn"]
                        pv3, bpb3 = halfbank(6, ch)
                        tr.op("pe", lambda h, tqn=tqn, pv3=pv3: h.matmul(pv3, lhsT=ropepb[:], rhs=tqn[:], start=True, stop=True),
                              reads=[btqn, b_const], writes=[bpb3])
                        rps.append((pv3, bpb3))
                    for T in Ts:
                        (ta, bta), (tqn, btqn) = T["a"], T["qn"]
                        tr.op("pool", lambda h, ta=ta, tqn=tqn: h.tensor_tensor(out=ta[:, 0:TT], in0=tqn[:], in1=cst[:, 0, :], op=ALU.mult),
                              reads=[btqn, b_cst], writes=[bta])
                    for (pv3, bpb3), T in zip(rps, Ts):
                        (tb, btb) = T["b"]
                        tr.op("dve", lambda h, tb=tb, pv3=pv3: h.tensor_tensor(out=tb[:, 0:TT], in0=pv3, in1=cst[:, 1, :], op=ALU.mult),
                              reads=[bpb3, b_cst], writes=[btb])
                    for ch, T in enumerate(Ts):
                        (ta, bta), (tb, btb) = T["a"], T["b"]
                        if g < 2:
                            tr.op("dve", lambda h, ch=ch, ta=ta, tb=tb: h.tensor_tensor(out=qT[:, g * 4 + ch, :], in0=ta[:, 0:TT], in1=tb[:, 0:TT], op=ALU.add),
                                  reads=[bta, btb], writes=[b_qT])
                        else:
                            for half in range(2):
                                ps_ = slice(half * 64, (half + 1) * 64)
                                tr.op("dve", lambda h, ch=ch, half=half, ps_=ps_, ta=ta, tb=tb: h.tensor_tensor(
                                    out=kT[ps_, ch, half, 128:128 + TT], in0=ta[ps_, 0:TT], in1=tb[ps_, 0:TT], op=ALU.add),
                                    reads=[bta, btb], writes=[b_kT])

                def post_conv(g, outs):
                    if g in (4, 5):
                        for ch in range(4):
                            pv, bpb = outs[ch]
                            cc = (g - 4) * 4 + ch
                            tr.op("act", lambda h, cc=cc, pv=pv: h.activation(out=cgs[:, cc, :], in_=pv, func=AF.Copy), reads=[bpb], writes=[b_cgs])
                    elif g in (6, 7):
                        for ch in range(4):
                            pv, bpb = outs[ch]
                            cc = (g - 6) * 4 + ch
                            tr.op("dve", lambda h, cc=cc, pv=pv: h.tensor_tensor(out=ubuf[:, cc, 2:2 + TT], in0=pv, in1=cgs[:, cc, :], op=ALU.mult),
                                  reads=[bpb, b_cgs], writes=[b_u])
                            tr.op("dve", lambda h, cc=cc: h.tensor_scalar(out=cgs[:, cc, :], in0=ubuf[:, cc, 0:TT], scalar1=convw[:, cc * 3:cc * 3 + 1],
                                                                           scalar2=None, op0=ALU.mult), reads=[b_u, b_par], writes=[b_cgs])
                            for tap in (1, 2):
                                tr.op("dve", lambda h, cc=cc, tap=tap: h.scalar_tensor_tensor(
                                    out=cgs[:, cc, :], in0=ubuf[:, cc, tap:tap + TT], scalar=convw[:, cc * 3 + tap:cc * 3 + tap + 1],
                                    in1=cgs[:, cc, :], op0=ALU.mult, op1=ALU.add), reads=[b_u, b_par, b_cgs], writes=[b_cgs])
                            tr.op("pool", lambda h, cc=cc: h.tensor_copy(out=ubuf[:, cc, 0:2], in_=ubuf[:, cc, TT:TT + 2]), reads=[b_u], writes=[b_u])
                    else:
                        Ts = [tmps() for _ in range(4)]
                        for ch, T in enumerate(Ts):
                            pv, bpb = outs[ch]
                            cc = (g - 8) * 4 + ch
                            (ta, bta) = T["a"]
                            tr.op("dve", lambda h, cc=cc, pv=pv, ta=ta: h.tensor_tensor(out=ta[:, 0:TT], in0=pv, in1=cgs[:, cc, :], op=ALU.mult),
                                  reads=[bpb, b_cgs], writes=[bta])
                        rstd_stages([(T["a"][0][:, 0:TT], T["a"][1]) for T in Ts], Ts, TT)
                        for ch, T in enumerate(Ts):
                            cc = (g - 8) * 4 + ch
                            (tr_, btr), (ta, bta) = T["r"], T["a"]
                            tr.op("dve", lambda h, cc=cc, ta=ta, tr_=tr_: h.scalar_tensor_tensor(out=mixT[:, 8 + cc, :], in0=ta[:, 0:TT], scalar=gconv[:, cc:cc + 1],
                                                                                                in1=tr_[:, 0:TT], op0=ALU.mult, op1=ALU.mult),
                                  reads=[bta, btr, b_par], writes=[b_mixT])

                def v_proj():
                    wb, bwb = wbuf[3 % 2], b_wbuf[3 % 2]
                    for i in range(NI):
                        pb, bpb = nextbank(4, 8)
                        for c in range(16):
                            tr.op("pe", lambda h, c=c, i=i, pb=pb: h.matmul(pb[:, :], lhsT=hT[:, c, i * 128:(i + 1) * 128],
                                                                            rhs=wb[:, c * 512:(c + 1) * 512], start=(c == 0), stop=(c == 15)),
                                  reads=[bwb, b_hT], writes=[bpb], inc=(c == 15))
                        tr.op("act", lambda h, i=i, pb=pb: h.activation(out=vtok[:, 1 + i, :], in_=pb[:, :], func=AF.Copy),
                              reads=[bpb], writes=[b_vtok])
                    load_w(5, wbuf[1], b_wbuf[1])

                def attn_pair(i, hks):
                    n = st * NI + i
                    kbs = ([] if n == 0 else [("prev", i * 128, i, mprevb)]) + [("cur", (i + 1) * 128, i + 1, mcurb)]
                    nk = len(kbs)
                    Ts = [tmps() for _ in hks]
                    sc = {}
                    for a_, hk in enumerate(hks):
                        for kbi, (nm, kc0, vblk, msk) in enumerate(kbs):
                            pb, bpb = pbank[a_ * 2 + kbi], b_pb[a_ * 2 + kbi]
                            for j in range(4):
                                half = j % 2
                                qc = 2 * hk + j // 2
                                tr.op("pe", lambda h, pb=pb, j=j, half=half, qc=qc, kc0=kc0, hk=hk: h.matmul(
                                    pb[:, j * 128:(j + 1) * 128], lhsT=kT[:, hk, half, kc0:kc0 + 128],
                                    rhs=qT[:, qc, i * 128:(i + 1) * 128], start=True, stop=True),
                                    reads=[b_kT, b_qT], writes=[bpb], inc=(j == 3))
                            sc[(a_, kbi)] = (pb, bpb)
                    for a_, hk in enumerate(hks):
                        for kbi in range(nk):
                            pb, bpb = sc[(a_, kbi)]
                            P_, bP = Pt[a_ * 2 + kbi], b_Pt[a_ * 2 + kbi]
                            tr.op("act", lambda h, pb=pb, P_=P_: h.activation(out=P_[:], in_=pb[:, :], func=AF.Exp, scale=0.125),
                                  reads=[bpb], writes=[bP])
                    for a_, hk in enumerate(hks):
                        for kbi, (nm, kc0, vblk, msk) in enumerate(kbs):
                            P_, bP = Pt[a_ * 2 + kbi], b_Pt[a_ * 2 + kbi]
                            tr.op("pool", lambda h, P_=P_, msk=msk: h.tensor_tensor(
                                out=P_[:].rearrange("p (j q) -> p j q", j=4), in0=P_[:].rearrange("p (j q) -> p j q", j=4),
                                in1=msk[:].unsqueeze(1).broadcast_to([128, 4, 128]), op=ALU.mult),
                                reads=[bP, b_const], writes=[bP])
                    pos, pds = [], []
                    for a_, hk in enumerate(hks):
                        po, bpo = pbank[4 + a_], b_pb[4 + a_]
                        pd, bpd = pbank[6 + a_], b_pb[6 + a_]
                        for kbi, (nm, kc0, vblk, msk) in enumerate(kbs):
                            P_, bP = Pt[a_ * 2 + kbi], b_Pt[a_ * 2 + kbi]
                            tr.op("pe", lambda h, P_=P_, vblk=vblk, kbi=kbi, po=po, hk=hk: h.matmul(
                                po[:, :], lhsT=vtok[:, vblk, hk * 128:(hk + 1) * 128], rhs=P_[:],
                                start=(kbi == 0), stop=(kbi == nk - 1)), reads=[bP, b_vtok], writes=[bpo], inc=(kbi == nk - 1))
                        for kbi in range(nk):
                            P_, bP = Pt[a_ * 2 + kbi], b_Pt[a_ * 2 + kbi]
                            tr.op("pe", lambda h, P_=P_, kbi=kbi, pd=pd: h.matmul(pd[:, :], lhsT=onesb[:], rhs=P_[:],
                                                                                 start=(kbi == 0), stop=(kbi == nk - 1)),
                                  reads=[bP, b_const], writes=[bpd], inc=(kbi == nk - 1))
                        pos.append((po, bpo))
                        pds.append((pd, bpd))
                    for a_, T in enumerate(Ts):
                        (tsq, btsq) = T["sq"]
                        po, bpo = pos[a_]
                        tr.op("act", lambda h, tsq=tsq, po=po: h.activation(out=tsq[:], in_=po[:, :], func=AF.Square), reads=[bpo], writes=[btsq])
                    pms = []
                    for a_, T in enumerate(Ts):
                        (tsq, btsq) = T["sq"]
                        pm, bpm = pbank[a_ * 2], b_pb[a_ * 2]
                        tr.op("pe", lambda h, tsq=tsq, pm=pm: h.matmul(pm[:, :], lhsT=bonesb[:], rhs=tsq[:], start=True, stop=True),
                              reads=[btsq, b_const], writes=[bpm])
                        pms.append((pm, bpm))
                    for a_, hk in enumerate(hks):
                        (ta, bta) = Ts[a_]["a"]
                        pd, bpd = pds[a_]
                        for j in range(4):
                            tr.op("act", lambda h, j=j, ta=ta, pd=pd, hk=hk: h.activation(out=ta[:, j * 128:(j + 1) * 128], in_=pd[:, j * 128:(j + 1) * 128],
                                                                                         func=AF.Square, scale=1e-3, bias=esink[:, hk * 4 + j:hk * 4 + j + 1]),
                                  reads=[bpd, b_par], writes=[bta])
                    for a_, T in enumerate(Ts):
                        (tr_, btr), (ta, bta) = T["r"], T["a"]
                        pm, bpm = pms[a_]
                        tr.op("dve", lambda h, tr_=tr_, pm=pm, ta=ta: h.tensor_tensor(out=tr_[:], in0=pm[:, :], in1=ta[:], op=ALU.add),
                              reads=[bpm, bta], writes=[btr])
                    for T in Ts:
                        (tr_, btr) = T["r"]
                        tr.op("act", lambda h, tr_=tr_: h.activation(out=tr_[:], in_=tr_[:], func=AF.Ln), reads=[btr], writes=[btr])
                    for T in Ts:
                        (tr_, btr) = T["r"]
                        tr.op("act", lambda h, tr_=tr_: h.activation(out=tr_[:], in_=tr_[:], func=AF.Exp, scale=-0.5), reads=[btr], writes=[btr])
                    for a_, hk in enumerate(hks):
                        (tr_, btr) = Ts[a_]["r"]
                        po, bpo = pos[a_]
                        for j in range(4):
                            half = j % 2
                            mc = 2 * hk + j // 2
                            ps_ = slice(half * 64, (half + 1) * 64)
                            tr.op("dve", lambda h, j=j, mc=mc, ps_=ps_, po=po, tr_=tr_: h.scalar_tensor_tensor(
                                out=mixT[ps_, mc, i * 128:(i + 1) * 128], in0=po[ps_, j * 128:(j + 1) * 128],
                                scalar=gattn[ps_, mc:mc + 1], in1=tr_[ps_, j * 128:(j + 1) * 128], op0=ALU.mult, op1=ALU.mult),
                                reads=[bpo, btr, b_par], writes=[b_mixT])

                o0 = proj_group(0)
                o1 = proj_group(1)
                post_qk(0, o0)
                o2 = proj_group(2)
                post_qk(1, o1)
                v_proj()
                post_qk(2, o2)
                for i in range(NI):
                    for hp in range(2):
                        attn_pair(i, (2 * hp, 2 * hp + 1))
                tr.op("pool", lambda h: h.tensor_copy(out=kT[:, :, :, 0:128], in_=kT[:, :, :, TT:TT + 128]), reads=[b_kT], writes=[b_kT])
                tr.op("pool", lambda h: h.tensor_copy(out=vtok[:, 0, :], in_=vtok[:, NI, :]), reads=[b_vtok], writes=[b_vtok])
                pend = None
                for g in range(4, 10):
                    o = proj_group(g)
                    if pend is not None:
                        post_conv(*pend)
                    pend = (g, o)
                post_conv(*pend)
                for g in range(10, 14):
                    wb, bwb = wbuf[g % 2], b_wbuf[g % 2]
                    dg = g - 10
                    for i in range(NI):
                        pb, bpb = nextbank()
                        T = tmps()
                        ta, bta = T["a"]
                        for mc in range(16):
                            tr.op("pe", lambda h, mc=mc, i=i, pb=pb: h.matmul(pb[:, :], lhsT=mixT[:, mc, i * 128:(i + 1) * 128],
                                                                             rhs=wb[:, mc * 512:(mc + 1) * 512], start=(mc == 0), stop=(mc == 15)),
                                  reads=[bwb, b_mixT], writes=[bpb], inc=(mc == 15))
                        tr.op("dve", lambda h, pb=pb, dg=dg, ta=ta: h.tensor_tensor(out=ta[:], in0=pb[:, :], in1=gt1b[:, dg * 512:(dg + 1) * 512], op=ALU.mult),
                              reads=[bpb, b_rows], writes=[bta])
                        tr.op("pool", lambda h, i=i, dg=dg, ta=ta: h.tensor_tensor(out=xres[:, i, dg * 512:(dg + 1) * 512], in0=xres[:, i, dg * 512:(dg + 1) * 512],
                                                                                in1=ta[:], op=ALU.add), reads=[bta, b_xres[i]], writes=[b_xres[i]])
                    if g + 2 < 14:
                        load_w(g + 2, wbuf[g % 2], b_wbuf[g % 2])
                tr.barrier()

            if DEBUG_STAGE == "A":
                for i in range(NI):
                    tr.dma("sp", lambda h, i=i: h.dma_start(out=out_d[t0 + i * 128: t0 + (i + 1) * 128, :], in_=xres[:, i, :]),
                           reads=[b_xres[i]], writes=[b_out], sembuf=b_xres[i])
                tr.barrier()
                continue

            with ExitStack() as pp:
                def sbq(name, shape, dt=F32):
                    return pp.enter_context(nc.sbuf_tensor("%s_p%d" % (name, st), list(shape), dt))
                hT = sbq("h2T", [128, 16, TT], BF16); b_hT = B("h2T")
                GT = sbq("GT", [128, TT, 128], BF16); b_GT = B("GT")
                mid_ = ExitStack()
                pqT = mid_.enter_context(nc.sbuf_tensor("pqT_p%d" % st, [128, 16, TT], BF16)); b_pqT = B("pqT")
                with ExitStack() as pq_:
                    def sbq1(name, shape, dt=F32):
                        return pq_.enter_context(nc.sbuf_tensor("%s_q%d" % (name, st), list(shape), dt))
                    xn = sbq1("xn2", [128, NI, D], BF16); b_xn = B("xn2")
                    wbuf = [sbq1("wbq%d" % i, [128, 8192], BF16) for i in range(2)]
                    b_wbuf = [B("wbq%d" % i) for i in range(2)]
                    load_w(14, wbuf[0], b_wbuf[0])
                    load_w(15, wbuf[1], b_wbuf[1])
                    norm_transpose(xn, b_xn, hT, b_hT, 2, 3, 4)
                    for g in range(4):
                        wb, bwb = wbuf[g % 2], b_wbuf[g % 2]
                        for ch in range(4):
                            pb, bpb = nextbank()
                            for c in range(16):
                                tr.op("pe", lambda h, c=c, pb=pb, ch=ch: h.matmul(pb[:, 0:TT], lhsT=wb[:, c * 512 + ch * 128: c * 512 + (ch + 1) * 128],
                                                                                 rhs=hT[:, c, :], start=(c == 0), stop=(c == 15)),
                                      reads=[bwb, b_hT], writes=[bpb], inc=(c == 15))
                            tr.op("act", lambda h, pb=pb, g=g, ch=ch: h.activation(out=pqT[:, g * 4 + ch, :], in_=pb[:, 0:TT], func=AF.Copy),
                                  reads=[bpb], writes=[b_pqT])
                        if g + 2 < 4:
                            load_w(14 + g + 2, wbuf[g % 2], b_wbuf[g % 2])
                    tr.barrier()
                with ExitStack() as p2_:
                    def sbq2(name, shape, dt=F32):
                        return p2_.enter_context(nc.sbuf_tensor("%s_r%d" % (name, st), list(shape), dt))
                    Ssb = sbq2("Ssb", [128, 16, 128]); b_S = B("Ssb")
                    S2s = [sbq2("S2_%d" % k, [128, 128]) for k in range(4)]; b_S2s = [B("S2_%d" % k) for k in range(4)]
                    T16 = sbq2("T16", [128, 16, 16]); b_T16s = [B("T16_%d" % k) for k in range(16)]
                    I16 = sbq2("I16", [128, 16, 16], U32); b_I16s = [B("I16_%d" % k) for k in range(16)]
                    I16f = sbq2("I16f", [128, 16, 16]); b_I16f = B("I16f")
                    cand = sbq2("cand", [128, 8, 256]); b_cand = B("cand")
                    cand2s = [sbq2("cand2_%d" % k, [128, 256]) for k in range(4)]; b_cand2s = [B("cand2_%d" % k) for k in range(4)]
                    C16 = sbq2("C16", [128, 8, 16]); b_C16s = [B("C16_%d" % k) for k in range(8)]
                    CI = sbq2("CI", [128, 8, 16], U32); b_CIs = [B("CI_%d" % k) for k in range(8)]
                    rc = sbq2("rc", [128, 2, 128], U32); b_rc = B("rc")
                    rcf = sbq2("rcf", [128, 2, 128]); b_rcf = B("rcf")
                    oh_flat = cand[:].rearrange("p a b -> p (a b)"); b_oh = b_cand
                    E12 = sbq2("E12", [128, 3, 128]); b_E12 = B("E12")
                    zz = sbq2("zz", [128, 16]); b_zz = B("zz")
                    ejT = sbq2("ejT", [128, 3, 128]); b_ejT = B("ejT")
                    bXt = [[B("Xb%d_%d" % (k, tl)) for tl in range(32)] for k in range(2)]
                    bYt = [[B("Yb%d_%d" % (k, tl)) for tl in range(32)] for k in range(2)]
                    Xb = [sbq2("Xb%d" % k, [128, 32, 128], BF16) for k in range(2)]; b_Xb = [B("Xb%d" % k) for k in range(2)]
                    Yb = [sbq2("Yb%d" % k, [128, 32, 128], BF16) for k in range(2)]; b_Yb = [B("Yb%d" % k) for k in range(2)]
                    gate3 = E12[:, 2, :].rearrange("p (a b) -> p a b", a=8)
                    xyc = {"i": 0}

                    def topk_gen(i):
                        for q4 in range(4):
                            pb, bpb = nextbank()
                            for k4 in range(4):
                                hh = q4 * 4 + k4
                                tr.op("pe", lambda h, pb=pb, k4=k4, hh=hh: h.matmul(pb[:, k4 * 128:(k4 + 1) * 128], lhsT=pqT[:, hh, i * 128:(i + 1) * 128],
                                                                                    rhs=subkb[:, hh, :], start=True, stop=True),
                                      reads=[b_pqT, b_par], writes=[bpb], inc=(k4 == 3))
                            tr.op("act", lambda h, pb=pb, q4=q4: h.activation(out=Ssb[:, q4 * 4:(q4 + 1) * 4, :].rearrange("p a b -> p (a b)"),
                                                                              in_=pb[:, :], func=AF.Copy), reads=[bpb], writes=[b_S])
                            yield
                        for h4 in range(4):
                            hhs = [h4 * 4 + k for k in range(4)]
                            for k, hh in enumerate(hhs):
                                tr.op("dve", lambda h, hh=hh: h.max(out=T16[:, hh, 0:8], in_=Ssb[:, hh, :]), reads=[b_S], writes=[b_T16s[hh]])
                            for k, hh in enumerate(hhs):
                                tr.op("dve", lambda h, hh=hh, k=k: h.match_replace(out=S2s[k][:], in_to_replace=T16[:, hh, 0:8], in_values=Ssb[:, hh, :], imm_value=-1e30),
                                      reads=[b_S, b_T16s[hh]], writes=[b_S2s[k]])
                            for k, hh in enumerate(hhs):
                                tr.op("dve", lambda h, hh=hh, k=k: h.max(out=T16[:, hh, 8:16], in_=S2s[k][:]), reads=[b_S2s[k]], writes=[b_T16s[hh]])
                            for k, hh in enumerate(hhs):
                                tr.op("dve", lambda h, hh=hh: h.max_index(out=I16[:, hh, 0:8], in_max=T16[:, hh, 0:8], in_values=Ssb[:, hh, :]),
                                      reads=[b_S, b_T16s[hh]], writes=[b_I16s[hh]])
                            for k, hh in enumerate(hhs):
                                tr.op("dve", lambda h, hh=hh, k=k: h.max_index(out=I16[:, hh, 8:16], in_max=T16[:, hh, 8:16], in_values=S2s[k][:]),
                                      reads=[b_S2s[k], b_T16s[hh]], writes=[b_I16s[hh]])
                            yield
                        tr.op("dve", lambda h: h.tensor_copy(out=I16f[:], in_=I16[:]), reads=b_I16s, writes=[b_I16f])
                        t16 = T16[:]
                        in0 = _ap(t16, 0, [[32, 8], [1, 16], [0, 16]])
                        in1 = _ap(t16, 16, [[32, 8], [0, 16], [1, 16]])
                        tr.op("dve", lambda h: h.tensor_tensor(out=cand[:].rearrange("p a (b c) -> p a b c", b=16), in0=in0, in1=in1, op=ALU.add),
                              reads=b_T16s, writes=[b_cand])
                        for h4 in range(2):
                            hds = [h4 * 4 + k for k in range(4)]
                            for k, hd in enumerate(hds):
                                tr.op("dve", lambda h, hd=hd: h.max(out=C16[:, hd, 0:8], in_=cand[:, hd, :]), reads=[b_cand], writes=[b_C16s[hd]])
                            for k, hd in enumerate(hds):
                                tr.op("dve", lambda h, hd=hd, k=k: h.match_replace(out=cand2s[k][:], in_to_replace=C16[:, hd, 0:8], in_values=cand[:, hd, :], imm_value=-1e30),
                                      reads=[b_cand, b_C16s[hd]], writes=[b_cand2s[k]])
                            for k, hd in enumerate(hds):
                                tr.op("dve", lambda h, hd=hd, k=k: h.max(out=C16[:, hd, 8:16], in_=cand2s[k][:]), reads=[b_cand2s[k]], writes=[b_C16s[hd]])
                            for k, hd in enumerate(hds):
                                tr.op("dve", lambda h, hd=hd: h.max_index(out=CI[:, hd, 0:8], in_max=C16[:, hd, 0:8], in_values=cand[:, hd, :]),
                                      reads=[b_cand, b_C16s[hd]], writes=[b_CIs[hd]])
                            for k, hd in enumerate(hds):
                                tr.op("dve", lambda h, hd=hd, k=k: h.max_index(out=CI[:, hd, 8:16], in_max=C16[:, hd, 8:16], in_values=cand2s[k][:]),
                                      reads=[b_cand2s[k], b_C16s[hd]], writes=[b_CIs[hd]])
                            yield
                        c16 = C16[:]
                        tr.op("dve", lambda h: h.tensor_tensor(out=gate3, in0=c16, in1=_ap(c16, 0, [[16, 8], [0, 16]]), op=ALU.subtract),
                              reads=b_C16s, writes=[b_E12])
                        tr.op("act", lambda h: h.activation(out=E12[:, 2, :], in_=E12[:, 2, :], func=AF.Exp), reads=[b_E12], writes=[b_E12])
                        tr.op("dve", lambda h: h.tensor_reduce(out=zz[:, 0:8], in_=gate3, axis=AX.X, op=ALU.add), reads=[b_E12], writes=[b_zz])
                        tr.op("dve", lambda h: h.reciprocal(out=zz[:, 8:16], in_=zz[:, 0:8]), reads=[b_zz], writes=[b_zz])
                        tr.op("dve", lambda h: h.tensor_tensor(out=gate3, in0=gate3, in1=_ap(zz[:], 8, [[1, 8], [0, 16]]), op=ALU.mult),
                              reads=[b_E12, b_zz], writes=[b_E12])
                        cif = CI[:].rearrange("p a b -> p (a b)")
                        tr.op("dve", lambda h: h.tensor_single_scalar(out=rc[:, 0, :], in_=cif, scalar=4, op=ALU.logical_shift_right),
                              reads=b_CIs, writes=[b_rc])
                        tr.op("dve", lambda h: h.tensor_single_scalar(out=rc[:, 1, :], in_=cif, scalar=15, op=ALU.bitwise_and),
                              reads=b_CIs, writes=[b_rc])
                        tr.op("dve", lambda h: h.tensor_copy(out=rcf[:], in_=rc[:]), reads=[b_rc], writes=[b_rcf])
                        i16f = I16f[:]
                        for w in range(2):
                            tr.op("dve", lambda h, w=w: h.tensor_tensor(out=oh_flat.rearrange("p (a b) -> p a b", b=16),
                                                                        in0=_ap(rcf[:], w * 128, [[1, 128], [0, 16]]),
                                                                        in1=_ap(iota16[:], 0, [[0, 128], [1, 16]]), op=ALU.is_equal),
                                  reads=[b_rcf, b_const], writes=[b_oh])
                            tr.op("dve", lambda h, w=w: h.tensor_tensor(out=oh_flat.rearrange("p (a k b) -> p a k b", a=8, k=16),
                                                                        in0=oh_flat.rearrange("p (a k b) -> p a k b", a=8, k=16),
                                                                        in1=_ap(i16f, w * 16, [[32, 8], [0, 16], [1, 16]]), op=ALU.mult),
                                  reads=[b_oh, b_I16f], writes=[b_oh])
                            tr.op("dve", lambda h, w=w: h.tensor_reduce(out=E12[:, w, :], in_=oh_flat.rearrange("p (a b) -> p a b", b=16), axis=AX.X, op=ALU.add),
                                  reads=[b_oh], writes=[b_E12])
                        yield

                    def ggen_gen(i):
                        pb, bpb = nextbank()
                        for k in range(3):
                            tr.op("pe", lambda h, pb=pb, k=k: h.transpose(out=pb[:, k * 128:(k + 1) * 128], in_=E12[:, k, :], identity=identf[:]),
                                  reads=[b_E12, b_const], writes=[bpb], inc=(k == 2))
                        tr.op("act", lambda h, pb=pb: h.activation(out=ejT[:].rearrange("p a b -> p (a b)"), in_=pb[:, 0:384], func=AF.Copy),
                              reads=[bpb], writes=[b_ejT])
                        yield
                        iob = _ap(iotab[:], 0, [[0, 32], [1, 128]])
                        for tg in range(4):
                            k_ = xyc["i"] % 2
                            xyc["i"] += 1
                            X_, bX, Y_, bY = Xb[k_], b_Xb[k_], Yb[k_], b_Yb[k_]
                            for tl in range(32):
                                tk = tg * 32 + tl
                                tr.op("dve", lambda h, X_=X_, tl=tl, tk=tk: h.tensor_scalar(out=X_[:, tl, :], in0=iotab[:], scalar1=ejT[:, 0, tk:tk + 1],
                                                                                            scalar2=ejT[:, 2, tk:tk + 1], op0=ALU.is_equal, op1=ALU.mult),
                                      reads=[b_const, b_ejT], writes=[bXt[k_][tl]])
                                tr.op("dve", lambda h, Y_=Y_, tl=tl, tk=tk: h.tensor_scalar(out=Y_[:, tl, :], in0=iotab[:], scalar1=ejT[:, 1, tk:tk + 1],
                                                                                            scalar2=None, op0=ALU.is_equal),
                                      reads=[b_const, b_ejT], writes=[bYt[k_][tl]])
                            for t4 in range(8):
                                pg, bpg = nextbank()
                                for tt in range(4):
                                    tl = t4 * 4 + tt
                                    tr.op("pe", lambda h, pg=pg, tt=tt, tl=tl, X_=X_, Y_=Y_: h.matmul(pg[:, tt * 128:(tt + 1) * 128], lhsT=Y_[:, tl, :], rhs=X_[:, tl, :],
                                                                                                    start=True, stop=True),
                                          reads=[bXt[k_][tl], bYt[k_][tl]], writes=[bpg], inc=(tt == 3))
                                tok0 = i * 128 + tg * 32 + t4 * 4
                                tr.op("act", lambda h, pg=pg, tok0=tok0: h.activation(out=GT[:, tok0:tok0 + 4, :].rearrange("p t e -> p (t e)"),
                                                                                      in_=pg[:, :], func=AF.Copy),
                                      reads=[bpg], writes=[b_GT])
                            yield
                        yield

                    def drain(*gw):
                        gw = [list(x) for x in gw]
                        while gw:
                            for item in list(gw):
                                for _ in range(item[1]):
                                    try:
                                        next(item[0])
                                    except StopIteration:
                                        gw.remove(item)
                                        break

                    drain((topk_gen(0), 1))
                    drain((ggen_gen(0), 1), (topk_gen(1), 3))
                    drain((ggen_gen(1), 1))
                    tr.barrier()
                mid_.close()
                with ExitStack() as p3_:
                    def sbq3(name, shape, dt=F32):
                        return p3_.enter_context(nc.sbuf_tensor("%s_s%d" % (name, st), list(shape), dt))
                    UTg = [sbq3("UTg%d" % k, [128, 8192], BF16) for k in range(2)]; b_UTg = [B("UTg%d" % k) for k in range(2)]
                    Vgr = [sbq3("Vgr%d" % k, [128, 8192], BF16) for k in range(2)]; b_Vgr = [B("Vgr%d" % k) for k in range(2)]
                    gl = [sbq3("gl%d" % k, [128, TT], BF16) for k in range(2)]; b_gl = [B("gl%d" % k) for k in range(2)]
                    GA = [sbq3("GA%d" % k, [128, 4, TT], BF16) for k in range(2)]; b_GA = [B("GA%d" % k) for k in range(2)]
                    ytmp = [sbq3("ytmp%d" % k, [128, 512]) for k in range(2)]; b_ytmp = [B("ytmp%d" % k) for k in range(2)]

                    def load_u(g):
                        k_ = g % 2
                        for hh in range(2):
                            tr.dma("sp", lambda h, hh=hh: h.dma_start(out=UTg[k_][:, hh * 4096:(hh + 1) * 4096], in_=us_d[g][:, hh * 4096:(hh + 1) * 4096]),
                                   reads=[b_uvs], writes=[b_UTg[k_]])

                    def load_v(g):
                        k_ = g % 2
                        for hh in range(2):
                            tr.dma("sp", lambda h, hh=hh: h.dma_start(out=Vgr[k_][:, hh * 4096:(hh + 1) * 4096], in_=vs_d[g][:, hh * 4096:(hh + 1) * 4096]),
                                   reads=[b_uvs], writes=[b_Vgr[k_]])

                    glc = {"i": 0}
                    ycnt = {"i": 0}

                    def a_stage(g):
                        k_ = g % 2
                        ut, but, ga_, bga = UTg[k_], b_UTg[k_], GA[k_], b_GA[k_]
                        for c4 in range(4):
                            c = g * 4 + c4
                            pa, bpa = nextbank(0, 4)
                            for dc in range(16):
                                tr.op("pe", lambda h, dc=dc, pa=pa, c4=c4: h.matmul(pa[:, 0:TT], lhsT=ut[:, dc * 512 + c4 * 128: dc * 512 + (c4 + 1) * 128],
                                                                                   rhs=hT[:, dc, :], start=(dc == 0), stop=(dc == 15)),
                                      reads=[but, b_hT], writes=[bpa], inc=(dc == 15))
                            gl_, bgl = gl[glc["i"] % 2], b_gl[glc["i"] % 2]
                            glc["i"] += 1
                            tr.op("act", lambda h, pa=pa, gl_=gl_: h.activation(out=gl_[:], in_=pa[:, 0:TT], func=AF.Gelu), reads=[bpa], writes=[bgl])
                            tr.op("dve", lambda h, gl_=gl_, c4=c4, c=c: h.tensor_tensor(out=ga_[:, c4, :], in0=gl_[:], in1=GT[:, :, c], op=ALU.mult),
                                  reads=[bgl, b_GT], writes=[bga])
                        if g + 2 < 32:
                            load_u(g + 2)

                    def y_stage(g):
                        k_ = g % 2
                        vg, bvg, ga_, bga = Vgr[k_], b_Vgr[k_], GA[k_], b_GA[k_]
                        for i in range(NI):
                            for dt_ in range(4):
                                py, bpy = nextbank(4, 8)
                                for c4 in range(4):
                                    tr.op("pe", lambda h, py=py, c4=c4, i=i, dt_=dt_: h.matmul(py[:, :], lhsT=ga_[:, c4, i * 128:(i + 1) * 128],
                                                                                              rhs=vg[:, c4 * 2048 + dt_ * 512: c4 * 2048 + (dt_ + 1) * 512],
                                                                                              start=(c4 == 0), stop=(c4 == 3)),
                                          reads=[bga, bvg], writes=[bpy], inc=(c4 == 3))
                                xs = xres[:, i, dt_ * 512:(dt_ + 1) * 512]
                                yc = ycnt["i"]
                                ycnt["i"] += 1
                                if yc % 2 == 0:
                                    tr.op("dve", lambda h, py=py, xs=xs: h.tensor_tensor(out=xs, in0=py[:, :], in1=xs, op=ALU.add),
                                          reads=[bpy, b_xres[i]], writes=[b_xres[i]])
                                else:
                                    yt, byt = ytmp[(yc // 2) % 2], b_ytmp[(yc // 2) % 2]
                                    tr.op("act", lambda h, py=py, yt=yt: h.activation(out=yt[:], in_=py[:, :], func=AF.Copy), reads=[bpy], writes=[byt])
                                    tr.op("pool", lambda h, yt=yt, xs=xs: h.tensor_tensor(out=xs, in0=yt[:], in1=xs, op=ALU.add),
                                          reads=[byt, b_xres[i]], writes=[b_xres[i]])
                        if g + 2 < 32:
                            load_v(g + 2)

                    load_u(0)
                    load_u(1)
                    load_v(0)
                    load_v(1)

                    def prefetch_x():
                        if st + 1 < N_SUPER:
                            for i in range(NI):
                                tr.dma("sp", lambda h, i=i: h.dma_start(out=xres2[:, (1 - par_) * NI + i, :],
                                                                        in_=x_d[t0 + TT + i * 128: t0 + TT + (i + 1) * 128, :]),
                                       writes=[b_xres2[1 - par_][i]])
                    a_stage(0)
                    for g in range(32):
                        if g + 1 < 32:
                            a_stage(g + 1)
                        y_stage(g)
                        if g == 4:
                            prefetch_x()
                    for i in range(NI):
                        tr.dma("sp", lambda h, i=i: h.dma_start(out=out_d[t0 + i * 128: t0 + (i + 1) * 128, :], in_=xres[:, i, :]),
                               reads=[b_xres[i]], writes=[b_out], sembuf=b_xres[i])
                    tr.barrier()
        tr.wait_all("sp", [b_out])
    return nc, tr.ninst


def _consts():
    identf = np.eye(128, dtype=np.float32)
    blk = np.arange(128) // 64
    bones = (blk[:, None] == blk[None, :]).astype(np.float32) / 64.0
    P = np.zeros((128, 128), np.float32)
    for hb in (0, 64):
        for i in range(8):
            P[hb + i, hb + i + 8] = -1.0
            P[hb + i + 8, hb + i] = 1.0
    ropepT = np.ascontiguousarray(P.T)
    kk = np.arange(128)[:, None]
    qq = np.arange(128)[None, :]
    mprev = (kk > qq).astype(np.float32)
    mcur = (kk <= qq).astype(np.float32)
    pos = np.arange(S, dtype=np.float32)
    inv_freq = (np.float32(500000.0) ** (-np.arange(0, 16, 2, dtype=np.float32) / np.float32(16))).astype(np.float32)
    ang = (pos[:, None] * inv_freq[None, :]).astype(np.float32)
    cos8 = np.cos(ang).astype(np.float32).T
    sin8 = np.sin(ang).astype(np.float32).T
    cosT = np.ones((128, S), np.float32)
    sinT = np.zeros((128, S), np.float32)
    for hb in (0, 64):
        cosT[hb:hb + 8] = cos8
        cosT[hb + 8:hb + 16] = cos8
        sinT[hb:hb + 8] = sin8
        sinT[hb + 8:hb + 16] = sin8
    iota128 = np.tile(np.arange(128, dtype=np.float32)[None, :], (128, 1))
    return dict(identf=identf, bones=bones, ropepT=ropepT, mprev=mprev, mcur=mcur, cosT=cosT, sinT=sinT, iota128=iota128)


def _layout_shared(w_ada, b_ada, g_norm1, w_in, g_q, g_k, sinks, conv_w, g_out_attn, g_out_conv,
                   w_out, g_norm2, w_pq, peer_subkeys, peer_u, peer_v):
    f = lambda a: np.ascontiguousarray(np.asarray(a, dtype=np.float32))
    col16 = lambda v: f(np.asarray(v).reshape(16, 128).T)
    w_in = np.asarray(w_in)
    q = w_in[:, 0:1024]
    k = w_in[:, 1024:1280]
    v = w_in[:, 1280:1536]
    bg = w_in[:, 1536:2560]
    cg = w_in[:, 2560:3584]
    hc = w_in[:, 3584:4608]
    kd = np.concatenate([k[:, h * 64:(h + 1) * 64] for h in range(4) for _ in range(2)], axis=1)
    vd = np.concatenate([v[:, h * 64:(h + 1) * 64] for h in range(4) for _ in range(2)], axis=1)
    w_in2 = f(np.concatenate([q, kd, vd, cg, hc, bg], axis=1))
    subkT = f(np.asarray(peer_subkeys).reshape(16, 128, 128).transpose(2, 0, 1).reshape(128, 16 * 128))
    convw = f(np.asarray(conv_w).reshape(3, 8, 128).transpose(2, 1, 0).reshape(128, 24))
    d = dict(
        w_ada=f(w_ada), b_ada=f(np.asarray(b_ada).reshape(1, -1)), g1col=col16(g_norm1), g2col=col16(g_norm2),
        w_in2=w_in2,
        gqcol=f(np.tile(np.asarray(g_q), 2).reshape(128, 1)), gkcol=f(np.tile(np.asarray(g_k), 2).reshape(128, 1)),
        sinks=f(np.asarray(sinks).reshape(1, 16)), convw=convw,
        gattn=f(np.asarray(g_out_attn).reshape(8, 128).T), gconv=f(np.asarray(g_out_conv).reshape(8, 128).T),
        w_out=f(w_out), w_pq=f(w_pq), subkT=subkT, peer_uT=f(np.asarray(peer_u).T), peer_v=f(peer_v),
    )
    d.update(_consts())
    return d


def kernel(x, c, w_ada, b_ada, g_norm1, w_in, g_q, g_k, sinks, conv_w, g_out_attn, g_out_conv,
           w_out, g_norm2, w_pq, peer_subkeys, peer_u, peer_v, _cores=None):
    x = np.asarray(x, dtype=np.float32)
    c = np.asarray(c, dtype=np.float32)
    shared = _layout_shared(w_ada, b_ada, g_norm1, w_in, g_q, g_k, sinks, conv_w, g_out_attn, g_out_conv,
                            w_out, g_norm2, w_pq, peer_subkeys, peer_u, peer_v)
    cores = list(range(8)) if _cores is None else list(_cores)
    nc, _ = build_program()
    in_maps = []
    for b in cores:
        m = dict(shared)
        m["x"] = np.ascontiguousarray(x[b])
        m["ccol"] = np.ascontiguousarray(c[b].reshape(16, 128).T)
        in_maps.append(m)
    res = run_bass_kernel_spmd(nc, in_maps, core_ids=list(range(len(cores))))
    outs = [np.asarray(r["out"], dtype=np.float32) for r in res.results]
    if _cores is not None:
        return outs
    return np.stack(outs, axis=0)
```
